# Optimizing a Trainium2 kernel written in Bass

```python
import jax
import jax.numpy as jnp
from jax import lax
import numpy as np

D_MODEL = 1024
BATCH = 4
SEQ = 8192
DEPTH = 2

GRID_W = 64
CTX_LEN = 256
MIX_WIDTH = D_MODEL
M_HEADS = 4
M_DV = MIX_WIDTH // (2 * M_HEADS)
M_DK = M_DV // 2
M_CHUNK = 128
A_HEADS = 8
A_KV_HEADS = 2
A_GROUP = A_HEADS // A_KV_HEADS
A_DH = (MIX_WIDTH - M_HEADS * M_DV) // A_HEADS
Q_BLOCK = 128
ROPE_THETA = 10000.0
D_FF = ((8 * D_MODEL // 3 + 127) // 128) * 128
N_EXPERTS = 8
TOP_K = 2
D_FF_EXPERT = 7 * D_MODEL // 2
EPS = 1e-6
SPLIT_SIZES = (M_HEADS * M_DK, M_HEADS * M_DK, M_HEADS * M_DV, M_HEADS * M_DV, 4 * M_HEADS,
               A_HEADS * A_DH, A_KV_HEADS * A_DH, A_KV_HEADS * A_DH)
D_IN = sum(SPLIT_SIZES)

kernel_name = 'hybrid_mlstm_gqa_moe_dit_prefix'


def _rms(x, w):
    xf = x.astype(jnp.float32)
    y = xf * lax.rsqrt(jnp.mean(xf * xf, axis=-1, keepdims=True) + EPS)
    return (y * w.astype(jnp.float32)).astype(x.dtype)


def _modulate(h, shift, scale):
    return h * (1.0 + scale) + shift


def _split(p):
    out, o = [], 0
    for s in SPLIT_SIZES:
        out.append(p[..., o:o + s])
        o += s
    return out


def _rope_tables(n_tok):
    rows = n_tok // GRID_W
    row = jnp.broadcast_to(jnp.arange(rows, dtype=jnp.float32)[:, None], (rows, GRID_W)).reshape(n_tok)
    col = jnp.broadcast_to(jnp.arange(GRID_W, dtype=jnp.float32)[None, :], (rows, GRID_W)).reshape(n_tok)
    n_freq = A_DH // 4
    inv_freq = ROPE_THETA ** (-jnp.arange(n_freq, dtype=jnp.float32) / n_freq)
    ang = jnp.concatenate([row[:, None] * inv_freq, col[:, None] * inv_freq], axis=-1)
    return jnp.cos(ang), jnp.sin(ang)


def _rope(x, cos, sin):
    xf = x.astype(jnp.float32).reshape(x.shape[:-1] + (A_DH // 2, 2))
    xe, xo = xf[..., 0], xf[..., 1]
    c, s = cos[None, :, None, :], sin[None, :, None, :]
    out = jnp.stack([xe * c - xo * s, xe * s + xo * c], axis=-1)
    return out.reshape(x.shape).astype(x.dtype)


def _mlstm_prep(mq, mk, mv, mg, gate_b):
    B, T, _ = mq.shape
    heads = lambda t: t.astype(jnp.float32).reshape(B, T, M_HEADS, -1).transpose(0, 2, 1, 3)
    q = heads(mq) * (M_DK ** -0.5)
    g = (mg.astype(jnp.float32) + gate_b.astype(jnp.float32)).reshape(B, T, 4, M_HEADS).transpose(2, 0, 3, 1)
    return (q, heads(mk), heads(mv), g[0], jax.nn.log_sigmoid(g[1]), g[2], jax.nn.log_sigmoid(g[3]))


def _mlstm_states(k, v, ig, lf, state0):
    B, H, T, dk = k.shape
    dv = v.shape[-1]
    nc = T // M_CHUNK
    kc = k.reshape(B, H, nc, M_CHUNK, dk)
    vc = v.reshape(B, H, nc, M_CHUNK, dv)
    igc = ig.reshape(B, H, nc, M_CHUNK)
    b = jnp.cumsum(lf.reshape(B, H, nc, M_CHUNK), axis=-1)
    b_end = b[..., -1]
    w_log = b_end[..., None] - b + igc
    m_loc = jnp.max(w_log, axis=-1)
    w = jnp.exp(w_log - m_loc[..., None])
    c_loc = jnp.einsum('bhcsd,bhcsk->bhcdk', vc * w[..., None], kc)
    n_loc = jnp.einsum('bhcs,bhcsk->bhck', w, kc)

    def step(carry, inp):
        c_prev, n_prev, m_prev = carry
        c_l, n_l, m_l, g = inp
        m_new = jnp.maximum(g + m_prev, m_l)
        a = jnp.exp(g + m_prev - m_new)
        s = jnp.exp(m_l - m_new)
        c_new = a[..., None, None] * c_prev + s[..., None, None] * c_l
        n_new = a[..., None] * n_prev + s[..., None] * n_l
        return (c_new, n_new, m_new), (c_prev, n_prev, m_prev)

    xs = (jnp.moveaxis(c_loc, 2, 0), jnp.moveaxis(n_loc, 2, 0), jnp.moveaxis(m_loc, 2, 0), jnp.moveaxis(b_end, 2, 0))
    final, enter = lax.scan(step, state0, xs)
    enter = (jnp.moveaxis(enter[0], 0, 2), jnp.moveaxis(enter[1], 0, 2), jnp.moveaxis(enter[2], 0, 2))
    return enter, final


def _mlstm_outputs(q, k, v, ig, lf, enter):
    c_e, n_e, m_e = enter
    B, H, T, dk = q.shape
    dv = v.shape[-1]
    nc = T // M_CHUNK
    qc = q.reshape(B, H, nc, M_CHUNK, dk)
    kc = k.reshape(B, H, nc, M_CHUNK, dk)
    vc = v.reshape(B, H, nc, M_CHUNK, dv)
    igc = ig.reshape(B, H, nc, M_CHUNK)
    b = jnp.cumsum(lf.reshape(B, H, nc, M_CHUNK), axis=-1)
    lower = jnp.tril(jnp.ones((M_CHUNK, M_CHUNK), dtype=bool))
    d_log = jnp.where(lower, b[..., :, None] - b[..., None, :] + igc[..., None, :], -jnp.inf)
    inter = b + m_e[..., None]
    m_t = jnp.maximum(inter, jnp.max(d_log, axis=-1))
    a = jnp.exp(inter - m_t)
    s = jnp.einsum('bhctk,bhcsk->bhcts', qc, kc) * jnp.exp(d_log - m_t[..., None])
    num = jnp.einsum('bhcts,bhcsd->bhctd', s, vc) + a[..., None] * jnp.einsum('bhcdk,bhctk->bhctd', c_e, qc)
    den = jnp.sum(s, axis=-1) + a * jnp.einsum('bhck,bhctk->bhct', n_e, qc)
    h = num / jnp.maximum(jnp.abs(den), jnp.exp(-m_t))[..., None]
    return h.reshape(B, H, T, dv)


def _mlstm_direction(ctx_feats, lat_feats, need_ctx):
    q_c, k_c, v_c, i_c, f_c = ctx_feats
    q_l, k_l, v_l, i_l, f_l = lat_feats
    B, H, _, dk = k_c.shape
    dv = v_c.shape[-1]
    zero = (jnp.zeros((B, H, dv, dk), jnp.float32), jnp.zeros((B, H, dk), jnp.float32), jnp.zeros((B, H), jnp.float32))
    ctx_enter, ctx_final = _mlstm_states(k_c, v_c, i_c, f_c, zero)
    lat_enter, _ = _mlstm_states(k_l, v_l, i_l, f_l, ctx_final)
    h_l = _mlstm_outputs(q_l, k_l, v_l, i_l, f_l, lat_enter)
    h_c = _mlstm_outputs(q_c, k_c, v_c, i_c, f_c, ctx_enter) if need_ctx else None
    return h_l, h_c


def _mlstm_merge(h, mo, m_norm_w):
    B, H, T, dv = h.shape
    hn = _rms(h, m_norm_w.reshape(H, 1, dv))
    return hn.transpose(0, 2, 1, 3).reshape(B, T, H * dv) * jax.nn.sigmoid(mo.astype(jnp.float32))


def _fwd(f):
    return (f[0], f[1], f[2], f[3], f[4])


def _bwd(f):
    return tuple(jnp.flip(t, axis=2) for t in (f[0], f[1], f[2], f[5], f[6]))


def _attn_q(aq, q_norm_w, rope):
    B, T, _ = aq.shape
    q = _rms(aq.reshape(B, T, A_HEADS, A_DH), q_norm_w)
    if rope is not None:
        q = _rope(q, *rope)
    q = q * (A_DH ** -0.5)
    return q.reshape(B, T, A_KV_HEADS, A_GROUP, A_DH).transpose(0, 2, 3, 1, 4)


def _attn_kv(ak, av, k_norm_w, rope):
    B, T, _ = ak.shape
    k = _rms(ak.reshape(B, T, A_KV_HEADS, A_DH), k_norm_w)
    if rope is not None:
        k = _rope(k, *rope)
    v = av.reshape(B, T, A_KV_HEADS, A_DH)
    return k.transpose(0, 2, 1, 3), v.transpose(0, 2, 1, 3)


def _attend(q, k, v):
    s = jnp.einsum('bkgqd,bksd->bkgqs', q, k, preferred_element_type=jnp.float32)
    p = jax.nn.softmax(s, axis=-1)
    return jnp.einsum('bkgqs,bksd->bkgqd', p.astype(v.dtype), v)


def _merge_heads(o):
    B, K, G, T, d = o.shape
    return o.transpose(0, 3, 1, 2, 4).reshape(B, T, K * G * d)


def _latent_attention(q, k_all, v_all):
    B, K, G, T, d = q.shape
    nb = T // Q_BLOCK
    qb = jnp.moveaxis(q.reshape(B, K, G, nb, Q_BLOCK, d), 3, 0)
    ob = lax.map(lambda blk: _attend(blk, k_all, v_all), qb)
    return _merge_heads(jnp.moveaxis(ob, 0, 3).reshape(B, K, G, T, d))


def _mixer(px, pc, rope, gate_b, m_norm_w, q_norm_w, k_norm_w, need_ctx):
    mq_x, mk_x, mv_x, mo_x, mg_x, aq_x, ak_x, av_x = _split(px)
    mq_c, mk_c, mv_c, mo_c, mg_c, aq_c, ak_c, av_c = _split(pc)
    lat = _mlstm_prep(mq_x, mk_x, mv_x, mg_x, gate_b)
    ctf = _mlstm_prep(mq_c, mk_c, mv_c, mg_c, gate_b)
    hf_x, hf_c = _mlstm_direction(_fwd(ctf), _fwd(lat), need_ctx)
    hb_x, hb_c = _mlstm_direction(_bwd(ctf), _bwd(lat), need_ctx)
    m_x = _mlstm_merge(hf_x + jnp.flip(hb_x, axis=2), mo_x, m_norm_w)
    k_x, v_x = _attn_kv(ak_x, av_x, k_norm_w, rope)
    k_c, v_c = _attn_kv(ak_c, av_c, k_norm_w, None)
    k_all = jnp.concatenate([k_c, k_x], axis=2)
    v_all = jnp.concatenate([v_c, v_x], axis=2)
    a_x = _latent_attention(_attn_q(aq_x, q_norm_w, rope), k_all, v_all)
    y_x = jnp.concatenate([m_x.astype(px.dtype), a_x.astype(px.dtype)], axis=-1)
    if not need_ctx:
        return y_x, None
    m_c = _mlstm_merge(hf_c + jnp.flip(hb_c, axis=2), mo_c, m_norm_w)
    a_c = _merge_heads(_attend(_attn_q(aq_c, q_norm_w, None), k_c, v_c))
    y_c = jnp.concatenate([m_c.astype(pc.dtype), a_c.astype(pc.dtype)], axis=-1)
    return y_x, y_c


def _swiglu(h, wg, wu, wd):
    return (jax.nn.silu(h @ wg) * (h @ wu)) @ wd


def _moe(h, router, wg, wu, wd):
    logits = (h @ router).astype(jnp.float32)
    top_v, top_i = lax.top_k(logits, TOP_K)
    gates = jax.nn.softmax(top_v, axis=-1)
    y = jnp.zeros_like(h)
    for e in range(N_EXPERTS):
        w_e = jnp.sum(jnp.where(top_i == e, gates, 0.0), axis=-1).astype(h.dtype)
        y = y + w_e[..., None] * _swiglu(h, wg[e], wu[e], wd[e])
    return y


def setup_inputs(seed: int = 0) -> dict:
    key = jax.random.key(seed)
    ks = jax.random.split(key, 26)
    D = D_MODEL
    n_dense = (DEPTH + 1) // 2
    n_moe = DEPTH // 2

    def nrm(k, shape, scale):
        return jax.random.normal(k, shape, jnp.float32) * scale

    f_bias = jnp.linspace(3.0, 6.0, M_HEADS, dtype=jnp.float32)
    mlstm_gate_b = jnp.concatenate([
        nrm(ks[8], (DEPTH, M_HEADS), 0.1),
        f_bias + nrm(ks[9], (DEPTH, M_HEADS), 0.1),
        nrm(ks[10], (DEPTH, M_HEADS), 0.1),
        f_bias + nrm(ks[11], (DEPTH, M_HEADS), 0.1)], axis=-1)
    return {
        'x': nrm(ks[0], (BATCH, SEQ, D), 1.0),
        'c': nrm(ks[1], (BATCH, D), 1.0),
        'ctx': nrm(ks[2], (BATCH, CTX_LEN, D), 1.0),
        'c_ctx': nrm(ks[3], (D,), 1.0),
        'ada_w': nrm(ks[4], (DEPTH, D, 6 * D), 0.5 * D ** -0.5),
        'ada_b': nrm(ks[5], (DEPTH, 6 * D), 0.02),
        'norm1_w': 1.0 + nrm(ks[6], (DEPTH, D), 0.02),
        'norm2_w': 1.0 + nrm(ks[7], (DEPTH, D), 0.02),
        'w_in': nrm(ks[12], (DEPTH, D, D_IN), D ** -0.5),
        'mlstm_gate_b': mlstm_gate_b,
        'mlstm_norm_w': 1.0 + nrm(ks[13], (DEPTH, M_HEADS * M_DV), 0.02),
        'q_norm_w': 1.0 + nrm(ks[14], (DEPTH, A_DH), 0.02),
        'k_norm_w': 1.0 + nrm(ks[15], (DEPTH, A_DH), 0.02),
        'w_out': nrm(ks[16], (DEPTH, MIX_WIDTH, D), MIX_WIDTH ** -0.5),
        'ffn_w_gate': nrm(ks[17], (n_dense, D, D_FF), D ** -0.5),
        'ffn_w_up': nrm(ks[18], (n_dense, D, D_FF), D ** -0.5),
        'ffn_w_down': nrm(ks[19], (n_dense, D_FF, D), D_FF ** -0.5),
        'moe_router': nrm(ks[20], (n_moe, D, N_EXPERTS), D ** -0.5),
        'moe_w_gate': nrm(ks[21], (n_moe, N_EXPERTS, D, D_FF_EXPERT), D ** -0.5),
        'moe_w_up': nrm(ks[22], (n_moe, N_EXPERTS, D, D_FF_EXPERT), D ** -0.5),
        'moe_w_down': nrm(ks[23], (n_moe, N_EXPERTS, D_FF_EXPERT, D), D_FF_EXPERT ** -0.5),
        'final_norm_w': 1.0 + nrm(ks[24], (D,), 0.02),
    }


def reference(x, c, ctx, c_ctx, ada_w, ada_b, norm1_w, norm2_w, w_in, mlstm_gate_b, mlstm_norm_w,
              q_norm_w, k_norm_w, w_out, ffn_w_gate, ffn_w_up, ffn_w_down, moe_router, moe_w_gate,
              moe_w_up, moe_w_down, final_norm_w):
    rope = _rope_tables(x.shape[1])
    silu_c = jax.nn.silu(c)
    silu_cc = jax.nn.silu(c_ctx)
    for i in range(DEPTH):
        need_ctx = i < DEPTH - 1
        mod_x = [m[:, None, :] for m in jnp.split(silu_c @ ada_w[i] + ada_b[i], 6, axis=-1)]
        mod_c = jnp.split(silu_cc @ ada_w[i] + ada_b[i], 6, axis=-1)
        hx = _modulate(_rms(x, norm1_w[i]), mod_x[0], mod_x[1])
        hc = _modulate(_rms(ctx, norm1_w[i]), mod_c[0], mod_c[1])
        y_x, y_c = _mixer(hx @ w_in[i], hc @ w_in[i], rope, mlstm_gate_b[i], mlstm_norm_w[i],
                          q_norm_w[i], k_norm_w[i], need_ctx)
        x = x + mod_x[2] * (y_x @ w_out[i])
        if i % 2 == 0:
            ffn = lambda h, j=i // 2: _swiglu(h, ffn_w_gate[j], ffn_w_up[j], ffn_w_down[j])
        else:
            ffn = lambda h, j=i // 2: _moe(h, moe_router[j], moe_w_gate[j], moe_w_up[j], moe_w_down[j])
        x = x + mod_x[5] * ffn(_modulate(_rms(x, norm2_w[i]), mod_x[3], mod_x[4]))
        if need_ctx:
            ctx = ctx + mod_c[2] * (y_c @ w_out[i])
            ctx = ctx + mod_c[5] * ffn(_modulate(_rms(ctx, norm2_w[i]), mod_c[3], mod_c[4]))
    return _rms(x, final_norm_w)
```

```python
import numpy as np
import ml_dtypes
from contextlib import ExitStack
import concourse.bass as bass
import concourse.mybir as mybir
from concourse.bass_utils import run_bass_kernel_spmd

F32 = mybir.dt.float32
BF16 = mybir.dt.bfloat16
I32 = mybir.dt.int32
AF = mybir.ActivationFunctionType
ALU = mybir.AluOpType
AX = mybir.AxisListType

ENGS = ['pe', 'act', 'dve', 'pool', 'sp']
NRING = 8
ENG_ATTR = {'pe': 'tensor', 'act': 'scalar', 'dve': 'vector', 'pool': 'gpsimd', 'sp': 'sync'}


class Prog:
    def __init__(self, nc, stack):
        self.nc = nc
        self.cnt = {e: 0 for e in ENGS}
        self.ring = {e: [0, [0] * NRING] for e in ENGS}
        self.sems = {}
        for e in ENGS:
            self.sems[('c', e)] = stack.enter_context(nc.semaphore('c_' + e))
            if e in ('sp', 'pool', 'act'):
                for s in range(NRING):
                    self.sems[('d', e, s)] = stack.enter_context(nc.semaphore('d_%s_%d' % (e, s)))
        self.waited = {e: {} for e in ENGS}
        self.nphase = 0
        self._reset()

    def _reset(self):
        self.q = {e: [] for e in ENGS}
        self.lastw = {}
        self.readers = {}

    def _events(self, reads, writes):
        ev = []
        for k in reads:
            if k in self.lastw:
                ev.append(self.lastw[k])
        for k in writes:
            if k in self.lastw:
                ev.append(self.lastw[k])
            ev.extend(self.readers.get(k, ()))
        return ev

    def _waits(self, eng, evs):
        waits = []
        for (sk, v) in evs:
            if sk == ('c', 'pe') and eng == 'pe':
                continue
            if self.waited[eng].get(sk, 0) >= v:
                continue
            self.waited[eng][sk] = v
            waits.append((sk, v))
        return waits

    def _commit(self, me, reads, writes):
        for k in writes:
            self.lastw[k] = me
            self.readers[k] = []
        for k in reads:
            self.readers.setdefault(k, []).append(me)

    def op(self, eng, fn, reads=(), writes=(), banks=()):
        evs = self._events(reads, writes)
        for b in banks:
            lw = self.lastw.get(('bank', b))
            if lw is not None and lw[0] != ('c', eng):
                evs.append(lw)
        waits = self._waits(eng, evs)
        self.cnt[eng] += 1
        me = (('c', eng), self.cnt[eng])
        self.q[eng].append((waits, fn, me, 1))
        self._commit(me, reads, writes)
        for b in banks:
            self.lastw[('bank', b)] = me
        return me

    def dma(self, eng, fn, reads=(), writes=()):
        ring = self.ring[eng]
        slot = ring[0] % NRING
        ring[0] += 1
        sk = ('d', eng, slot)
        prev = ring[1][slot]
        evs = self._events(reads, writes)
        if prev > 0:
            evs.append((sk, prev))
        waits = self._waits(eng, evs)
        ring[1][slot] = prev + 16
        me = (sk, prev + 16)
        self.q[eng].append((waits, fn, me, 16))
        self._commit(me, reads, writes)
        return me

    def end_phase(self):
        evs = []
        for e in ENGS:
            r = self.ring[e]
            for s in range(NRING):
                if r[1][s] > 0:
                    evs.append((('d', e, s), r[1][s]))
            if e != 'sp' and self.cnt[e] > 0:
                evs.append((('c', e), self.cnt[e]))
        waits = self._waits('sp', evs)
        self.q['sp'].append((waits, None, None, 0))
        nc = self.nc
        sems = self.sems
        self.nphase += 1
        with nc.allow_low_precision(reason='bf16 matmul operands by design'), nc.Block('ph%d' % self.nphase) as block:
            def mk(e):
                def body(engine):
                    for (waits, fn, me, inc) in self.q[e]:
                        for (sk, v) in waits:
                            engine.wait_ge(sems[sk], v)
                        if fn is not None:
                            fn(engine).then_inc(sems[me[0]], inc)
                return body
            for e in ENGS:
                if self.q[e]:
                    getattr(block, ENG_ATTR[e])(mk(e))
        for e in ENGS:
            for e2 in ENGS:
                self.waited[e][('c', e2)] = self.cnt[e2]
                for s in range(NRING):
                    self.waited[e][('d', e2, s)] = self.ring[e2][1][s]
        self._reset()


D = 1024
DIN = 2320
NCT = 2
DFF = 2816
DFE = 3584
NEXP = 8
_ORIG = dict(mq=(0, 256), mk=(256, 512), mv=(512, 1024), mo=(1024, 1536), mg=(1536, 1552),
             aq=(1552, 2064), ak=(2064, 2192), av=(2192, 2320))
_PERM = np.concatenate([np.arange(*_ORIG[k]) for k in ('mq', 'mk', 'mv', 'mo', 'aq', 'ak', 'av', 'mg')])


def build(cfg):
    T = cfg['T']
    NL = cfg.get('layers', 2)
    dbg = cfg.get('debug', False)
    NLT = T // 128
    NT = NCT + NLT
    NTOK = NT * 128
    nc = bass.Bass("TRN2", target_bir_lowering=False)
    SCR = "ExternalOutput" if dbg else "Internal"

    def din(name, shape, dt=F32):
        return nc.dram_tensor(name, list(shape), dt, kind="ExternalInput").ap()

    def dscr(name, shape, dt):
        return nc.dram_tensor(name, list(shape), dt, kind=SCR).ap()

    xin = din("xin", [NTOK, D])
    cvec = din("cvec", [128, 16])
    ada_w = din("ada_w", [2, D, 6 * D])
    ada_b = din("ada_b", [2, 6 * D])
    nwc = din("nwc", [128, 32])
    fnw = din("fnw", [1, D])
    w_in = din("w_in", [2, D, DIN])
    gate_b = din("gate_b", [2, 16])
    mnw = din("mnw", [2, 512])
    qnw = din("qnw", [2, 64])
    knw = din("knw", [2, 64])
    w_out = din("w_out", [2, D, D])
    ffn_g = din("ffn_g", [D, DFF])
    ffn_u = din("ffn_u", [D, DFF])
    ffn_d = din("ffn_d", [DFF, D])
    router = din("router", [D, NEXP])
    moe_g = din("moe_g", [NEXP, D, DFE])
    moe_u = din("moe_u", [NEXP, D, DFE])
    moe_d = din("moe_d", [NEXP, DFE, D])
    identf_d = din("identf", [128, 128])
    identb_d = din("identb", [128, 128], BF16)
    trif_d = din("trif", [128, 128])
    trib_d = din("trib", [128, 128])
    maskf_d = din("maskf", [128, 128], BF16)
    maskb_d = din("maskb", [128, 128], BF16)
    rope_d = din("rope", [NT, 128, 64])
    out = nc.dram_tensor("out", [T, D], F32, kind="ExternalOutput").ap()
    SPARSE = cfg.get('sparse', True) and NL > 1
    CAP = T
    NJ = CAP // 512
    NG = (2 * T + NEXP * 511) // 512
    NMC = 8 + NJ + NG + 7 + 14 + 4
    n2w = din("n2w", [2, D])
    mconst_d = din("mconst", [128, NMC])
    if SPARSE:
        MGp = nc.dram_tensor("MGp", [NEXP * 7 * 128, 8 * 512], BF16, kind="Internal").ap()
        MUp = nc.dram_tensor("MUp", [NEXP * 7 * 128, 8 * 512], BF16, kind="Internal").ap()
        MDp = nc.dram_tensor("MDp", [NEXP * 2 * 7 * 128, 4 * 512], BF16, kind="Internal").ap()
        HS = nc.dram_tensor("HS", [NEXP * CAP, D], BF16, kind="Internal").ap()
        YS = nc.dram_tensor("YS", [NG * 512, D], F32, kind="Internal").ap()

    X = dscr("X", [NTOK, D], F32)
    QT = dscr("QT", [8, 64, NTOK], BF16)
    KT = dscr("KT", [2, 64, NTOK], BF16)
    VA = dscr("VA", [NTOK, 2, 65], BF16)
    MQ = [dscr("MQ%d" % d, [4, 64, NTOK], BF16) for d in range(2)]
    MK = [dscr("MK%d" % d, [4, 64, NTOK], BF16) for d in range(2)]
    MK2 = [dscr("MK2%d" % d, [NTOK, 4, 64], BF16) for d in range(2)]
    MVA = dscr("MVA", [NTOK, 4, 129], BF16)
    MO = dscr("MO", [NTOK, 512], BF16)
    DEC = dscr("DEC", [NT, 64, 8], F32)
    HB = dscr("HB", [NTOK, 512], F32)
    Y = dscr("Y", [NTOK, D], BF16)
    WINb = nc.dram_tensor("WINb", [2, D, DIN], BF16, kind=SCR).ap()
    dGS = dscr("dGS", [2, 128, 32], F32)
    dGATE = dscr("dGATE", [4, 128, D], F32)
    dhT = dscr("dhT", [128, 8, 128], BF16)
    WOb = nc.dram_tensor("WOb", [2, D, D], BF16, kind="Internal").ap()
    FGb = nc.dram_tensor("FGb", [D, DFF], BF16, kind="Internal").ap()
    FUb = nc.dram_tensor("FUb", [D, DFF], BF16, kind="Internal").ap()
    FDb = nc.dram_tensor("FDb", [DFF, D], BF16, kind="Internal").ap()
    MGb = nc.dram_tensor("MGb", [NEXP, D, DFE], BF16, kind="Internal").ap()
    MUb = nc.dram_tensor("MUb", [NEXP, D, DFE], BF16, kind="Internal").ap()
    MDb = nc.dram_tensor("MDb", [NEXP, DFE, D], BF16, kind="Internal").ap()

    with ExitStack() as gs:
        P = Prog(nc, gs)

        uid = [0]

        def sbt(st, name, shape, dt=F32):
            uid[0] += 1
            return st.enter_context(nc.sbuf_tensor("s%d_%s" % (uid[0], name), list(shape), dt))

        banks = [gs.enter_context(nc.psum_tensor("bank%d" % i, [128, 512], F32)) for i in range(8)]

        def bk(i):
            return banks[i][:, :]

        def bkb(i):
            return banks[i][:, :].bitcast(BF16)

        identf = sbt(gs, "identf", [128, 128])
        identb = sbt(gs, "identb", [128, 128], BF16)
        trif = sbt(gs, "trif", [128, 128])
        trib = sbt(gs, "trib", [128, 128])
        onesf = sbt(gs, "onesf", [128, 128])
        maskf = sbt(gs, "maskf", [128, 128], BF16)
        maskb = sbt(gs, "maskb", [128, 128], BF16)
        cv = sbt(gs, "cv", [128, 16])
        scv = sbt(gs, "scv", [128, 48])
        nwt = sbt(gs, "nwt", [128, 32])
        GATE = [[sbt(gs, "GATE%d_%d" % (w, m), [128, D]) for m in range(2)] for w in range(2)]
        COLS = [sbt(gs, "COLS%d" % w, [128, 32]) for w in range(2)]
        GS = [sbt(gs, "GS%d" % w, [128, 32]) for w in range(2)]
        N2R = sbt(gs, "N2R", [128, D])
        G2R = sbt(gs, "G2R", [128, D])
        S2R = sbt(gs, "S2R", [128, D])
        mconst = sbt(gs, "mconst", [128, NMC])

        def cast_dram(dst, src, tag):
            n = 1
            for s in src.shape:
                n *= s
            letters = "abcd"[:len(src.shape)]
            pat = " ".join(letters) + " -> (" + " ".join(letters) + ")"
            s1 = src.rearrange(pat).rearrange("(n m) -> n m", m=1024)
            d1 = dst.rearrange(pat).rearrange("(n m) -> n m", m=1024)
            rows = n // 1024
            step = 2048
            for r0 in range(0, rows, step):
                r1 = min(rows, r0 + step)
                P.dma('pool', lambda e, r0=r0, r1=r1: e.dma_start(out=d1[r0:r1, :], in_=s1[r0:r1, :]),
                      writes=[(tag, r0)])

        def phase_w():
            for (t_, d_) in ((identf, identf_d), (identb, identb_d), (trif, trif_d), (trib, trib_d),
                             (maskf, maskf_d), (maskb, maskb_d), (cv, cvec), (nwt, nwc)):
                P.dma('sp', lambda e, t_=t_, d_=d_: e.dma_start(out=t_[:], in_=d_[:, :]), writes=[t_.name])
            P.op('pool', lambda e: e.memset(onesf[:], 1.0), writes=['onesf'])
            P.op('act', lambda e: e.activation(out=scv[:, 0:16], in_=cv[:], func=AF.Exp, scale=-1.0),
                 reads=[cv.name], writes=['scv0'])
            P.op('dve', lambda e: e.tensor_scalar(out=scv[:, 16:32], in0=scv[:, 0:16], scalar1=1.0, scalar2=None,
                                                  op0=ALU.add), reads=['scv0'], writes=['scv1'])
            P.op('dve', lambda e: e.reciprocal(out=scv[:, 0:16], in_=scv[:, 16:32]), reads=['scv1'], writes=['scv0'])
            P.op('dve', lambda e: e.tensor_tensor(out=scv[:, 32:48], in0=scv[:, 0:16], in1=cv[:], op=ALU.mult),
                 reads=['scv0', cv.name], writes=['scv2'])
            cast_dram(WINb, w_in, 'WINb')
            cast_dram(WOb, w_out, 'WOb')
            cast_dram(FGb, ffn_g, 'FGb')
            cast_dram(FUb, ffn_u, 'FUb')
            cast_dram(FDb, ffn_d, 'FDb')
            P.dma('sp', lambda e: e.dma_start(out=mconst[:], in_=mconst_d[:, :]), writes=['mconst'])
            if NL > 1 and not SPARSE:
                cast_dram(MGb, moe_g, 'MGb')
                cast_dram(MUb, moe_u, 'MUb')
                cast_dram(MDb, moe_d, 'MDb')
            P.end_phase()

        def prep_moe_weights():
            for e_ in range(NEXP):
                for c in range(7):
                    r0 = (e_ * 7 + c) * 128
                    for (dst, src, tag) in ((MGp, moe_g, 'MGp'), (MUp, moe_u, 'MUp')):
                        P.dma('pool', lambda e, dst=dst, src=src, r0=r0, e_=e_, c=c: e.dma_start(
                            out=dst[r0:r0 + 128, :].rearrange("p (k f) -> p k f", k=8),
                            in_=src[e_][:, c * 512:(c + 1) * 512].rearrange("(k p) f -> p k f", p=128)),
                            writes=[(tag, e_, c)])
                for half in range(2):
                    for q in range(7):
                        r0 = ((e_ * 2 + half) * 7 + q) * 128
                        P.dma('pool', lambda e, r0=r0, e_=e_, half=half, q=q: e.dma_start(
                            out=MDp[r0:r0 + 128, :].rearrange("p (c d) -> p c d", c=4),
                            in_=moe_d[e_][q * 512:(q + 1) * 512, half * 512:(half + 1) * 512].rearrange("(c p) d -> p c d", p=128)),
                            writes=[('MDp', e_, half, q)])

        def phase0(l):
            with ExitStack() as st:
                awb = [sbt(st, "awb%d" % i, [128, 8, 512]) for i in range(2)]
                abb = [sbt(st, "abb%d" % i, [128, 512]) for i in range(2)]
                row = [sbt(st, "row%d" % i, [128, 512]) for i in range(2)]
                SL = [sbt(st, "SL%d" % j, [128, 8, 128]) for j in range(2)]
                for j in range(2):
                    for k in range(8):
                        P.op('dve' if k % 2 else 'pool',
                             lambda e, j=j, k=k: e.tensor_copy(out=SL[j][:, k, :],
                                                               in_=scv[:, 32 + j * 8 + k:33 + j * 8 + k].to_broadcast([128, 128])),
                             writes=[('SL', j, k)])
                P.dma('sp', lambda e: e.dma_start(out=N2R[:], in_=n2w[l:l + 1, :].partition_broadcast(128)), writes=['N2R'])
                it = 0
                for grp in range(12):
                    m, half = grp // 2, grp % 2
                    b = grp % 2
                    P.dma('sp', lambda e, b=b, grp=grp: e.dma_start(
                        out=awb[b][:], in_=ada_w[l, :, grp * 512:(grp + 1) * 512].rearrange("(k p) n -> p k n", p=128)),
                        writes=[('awb', b)])
                    P.dma('sp', lambda e, b=b, grp=grp: e.dma_start(
                        out=abb[b][:], in_=ada_b[l:l + 1, grp * 512:(grp + 1) * 512].partition_broadcast(128)),
                        writes=[('abb', b)])
                    for w in range(2):
                        bm = (it % 2) * 2
                        rb = it % 2
                        it += 1
                        for k in range(8):
                            P.op('pe', lambda e, k=k, w=w, b=b, bm=bm: e.matmul(
                                bk(bm), lhsT=SL[w][:, k, :], rhs=awb[b][:, k, :], start=(k == 0), stop=(k == 7)),
                                reads=[('SL', w, k), ('awb', b)], writes=[('pm', bm)], banks=[bm])
                        if m in (2, 5):
                            gt_ = GATE[w][0 if m == 2 else 1]
                            P.op('dve', lambda e, gt_=gt_, half=half, bm=bm, b=b: e.tensor_tensor(
                                out=gt_[:, half * 512:(half + 1) * 512], in0=bk(bm), in1=abb[b][:], op=ALU.add),
                                reads=[('pm', bm), ('abb', b)], writes=[(gt_.name, half)], banks=[bm])
                        else:
                            P.op('dve', lambda e, rb=rb, bm=bm, b=b: e.tensor_tensor(
                                out=row[rb][:], in0=bk(bm), in1=abb[b][:], op=ALU.add),
                                reads=[('pm', bm), ('abb', b)], writes=[('row', rb)], banks=[bm])
                            if w == 0 and m == 3:
                                P.op('pool', lambda e, rb=rb, half=half: e.tensor_copy(
                                    out=S2R[:, half * 512:(half + 1) * 512], in_=row[rb][:]),
                                    reads=[('row', rb)], writes=[('S2R', half)])
                            if w == 0 and m == 4:
                                P.op('dve', lambda e, rb=rb, half=half: e.scalar_tensor_tensor(
                                    out=G2R[:, half * 512:(half + 1) * 512], in0=row[rb][:], scalar=1.0,
                                    in1=N2R[:, half * 512:(half + 1) * 512], op0=ALU.add, op1=ALU.mult),
                                    reads=[('row', rb), 'N2R'], writes=[('G2R', half)])
                            for blk in range(4):
                                P.op('pe', lambda e, rb=rb, bm=bm, blk=blk: e.transpose(
                                    out=bk(bm + 1)[:, blk * 128:(blk + 1) * 128], in_=row[rb][:, blk * 128:(blk + 1) * 128],
                                    identity=identf[:]),
                                    reads=[('row', rb), 'identf'], writes=[('pt', bm + 1)], banks=[bm + 1])
                            ci = {0: 0, 1: 1, 3: 2, 4: 3}[m]
                            P.op('dve', lambda e, w=w, ci=ci, half=half, bm=bm: e.tensor_copy(
                                out=COLS[w][:, ci * 8 + half * 4: ci * 8 + half * 4 + 4],
                                in_=bk(bm + 1).rearrange("p (a b) -> p a b", a=4)[:, :, 0]),
                                reads=[('pt', bm + 1)], writes=[('COLS', w, ci, half)], banks=[bm + 1])
                for w in range(2):
                    rd = [('COLS', w, ci, h) for ci in range(4) for h in range(2)]
                    for j in range(2):
                        nsl = nwt[:, l * 16 + j * 8: l * 16 + j * 8 + 8]
                        P.op('dve', lambda e, w=w, j=j, nsl=nsl: e.scalar_tensor_tensor(
                            out=GS[w][:, j * 16: j * 16 + 8], in0=COLS[w][:, j * 16 + 8: j * 16 + 16], scalar=1.0,
                            in1=nsl, op0=ALU.add, op1=ALU.mult),
                            reads=rd + ['nwt'], writes=[('GS', w, j, 0)])
                        P.op('dve', lambda e, w=w, j=j: e.tensor_copy(
                            out=GS[w][:, j * 16 + 8: j * 16 + 16], in_=COLS[w][:, j * 16: j * 16 + 8]),
                            reads=rd, writes=[('GS', w, j, 1)])
                if dbg:
                    for w in range(2):
                        P.dma('sp', lambda e, w=w: e.dma_start(out=dGS[w], in_=GS[w][:]), reads=[('GS', w, j, i) for j in range(2) for i in range(2)], writes=[('dGS', w)])
                        for m in range(2):
                            P.dma('sp', lambda e, w=w, m=m: e.dma_start(out=dGATE[w * 2 + m], in_=GATE[w][m][:]), reads=[(GATE[w][m].name, hh) for hh in range(2)], writes=[('dGATE', w, m)])
                P.end_phase()

        def rstd_ops(stt, c_in, c_tmp, c_out, n, inv_n, key):
            P.op('dve', lambda e: e.tensor_scalar(out=stt[:, c_tmp:c_tmp + n], in0=stt[:, c_in:c_in + n],
                                                  scalar1=inv_n, scalar2=1e-6, op0=ALU.mult, op1=ALU.add),
                 reads=[(key, 'ss')], writes=[(key, 'v')])
            P.op('act', lambda e: e.activation(out=stt[:, c_in:c_in + n], in_=stt[:, c_tmp:c_tmp + n], func=AF.Ln),
                 reads=[(key, 'v')], writes=[(key, 'ln')])
            P.op('act', lambda e: e.activation(out=stt[:, c_out:c_out + n], in_=stt[:, c_in:c_in + n], func=AF.Exp,
                                               scale=-0.5),
                 reads=[(key, 'ln')], writes=[(key, 'r')])

        def norm_to_hT(st_tiles, xsrc_ap, xkey, w, j, hT_dst_fn, hkey_fn, bA, bB, tb, hF_dst_fn=None):
            junk, stt, xn = st_tiles
            key = ('nst', tb)
            P.op('act', lambda e: e.activation(out=junk[:], in_=xsrc_ap, func=AF.Square, accum_out=stt[:, 0:1]),
                 reads=[xkey], writes=['junk', (key, 'ss')])
            rstd_ops(stt, 0, 1, 2, 1, 1.0 / D, key)
            P.op('dve', lambda e: e.tensor_scalar(out=xn[:], in0=xsrc_ap, scalar1=stt[:, 2:3], scalar2=None,
                                                  op0=ALU.mult), reads=[xkey, (key, 'r')], writes=['xn'])
            for k in range(8):
                bb = bA if k < 4 else bB
                P.op('pe', lambda e, k=k, bb=bb: e.transpose(out=bk(bb)[:, (k % 4) * 128:(k % 4 + 1) * 128],
                                                            in_=xn[:, k * 128:(k + 1) * 128], identity=identf[:]),
                     reads=['xn', 'identf'], writes=[('pT', bb, k % 4)], banks=[bb])
            for k in range(8):
                bb = bA if k < 4 else bB
                src = bk(bb)[:, (k % 4) * 128:(k % 4 + 1) * 128]
                gcol = GS[w][:, j * 16 + k: j * 16 + k + 1]
                scol = GS[w][:, j * 16 + 8 + k: j * 16 + 9 + k]
                if hF_dst_fn is not None:
                    P.op('dve', lambda e, k=k, src=src, gcol=gcol, scol=scol: e.tensor_scalar(
                        out=hF_dst_fn(k), in0=src, scalar1=gcol, scalar2=scol, op0=ALU.mult, op1=ALU.add),
                        reads=[('pT', bb, k % 4), ('GS', w, j, 0), ('GS', w, j, 1)], writes=[('hF', k)], banks=[bb])
                    P.op('act', lambda e, k=k: e.copy(out=hT_dst_fn(k), in_=hF_dst_fn(k)),
                         reads=[('hF', k)], writes=[hkey_fn(k)])
                else:
                    P.op('dve', lambda e, k=k, src=src, gcol=gcol, scol=scol: e.tensor_scalar(
                        out=hT_dst_fn(k), in0=src, scalar1=gcol, scalar2=scol, op0=ALU.mult, op1=ALU.add),
                        reads=[('pT', bb, k % 4), ('GS', w, j, 0), ('GS', w, j, 1)], writes=[hkey_fn(k)], banks=[bb])

        def phaseA(l):
            with ExitStack() as st:
                win = sbt(st, "win", [128, 8, DIN], BF16)
                gbr = sbt(st, "gbr", [128, 16])
                qnr = sbt(st, "qnr", [128, 64])
                knr = sbt(st, "knr", [128, 64])
                xt = [sbt(st, "xt%d" % i, [128, D]) for i in range(2)]
                rp = [sbt(st, "rp%d" % i, [128, 64]) for i in range(2)]
                junk = sbt(st, "junk", [128, D], BF16)
                stt = [sbt(st, "stt%d" % i, [128, 8]) for i in range(2)]
                xn = sbt(st, "xn", [128, D])
                hT = [sbt(st, "hT%d" % i, [128, 8, 128], BF16) for i in range(2)]
                gt = sbt(st, "gt", [128, 16])
                e8 = sbt(st, "e8", [128, 8])
                L8 = sbt(st, "L8", [128, 8])
                cs = sbt(st, "cs", [128, 24])
                sc = sbt(st, "sc", [128, 24])
                tmp8 = sbt(st, "tmp8", [128, 16])
                dec = sbt(st, "dec", [128, 8])
                pqk = sbt(st, "pqk", [128, 512])
                sq6 = [sbt(st, "sq6_%d" % i, [128, 4, 64], BF16) for i in range(6)]
                trs = sbt(st, "trs", [64, 16, 128], BF16)
                vaug = [sbt(st, "vaug%d" % i, [128, 4, 129], BF16) for i in range(2)]
                mot = sbt(st, "mot", [128, 512])
                mob = sbt(st, "mob", [128, 512], BF16)
                sqq = sbt(st, "sqq", [128, 640])
                aqs = sbt(st, "aqs", [128, 24])
                qn = sbt(st, "qn", [128, 640])
                qn2 = sbt(st, "qn2", [128, 640])
                rt = [sbt(st, "rt%d" % i, [128, 10, 32]) for i in range(4)]
                qr = sbt(st, "qr", [128, 640], BF16)
                qT = sbt(st, "qT", [64, 10, 128], BF16)
                vat = [sbt(st, "vat%d" % i, [128, 2, 65], BF16) for i in range(2)]

                P.dma('sp', lambda e: e.dma_start(out=win[:], in_=WINb[l].rearrange("(k p) n -> p k n", p=128)),
                      writes=['win'])
                P.dma('sp', lambda e: e.dma_start(out=gbr[:], in_=gate_b[l:l + 1, :].partition_broadcast(128)),
                      writes=['gbr'])
                P.dma('sp', lambda e: e.dma_start(out=qnr[:], in_=qnw[l:l + 1, :].partition_broadcast(128)),
                      writes=['qnr0'])
                P.dma('sp', lambda e: e.dma_start(out=knr[:], in_=knw[l:l + 1, :].partition_broadcast(128)),
                      writes=['knr'])
                P.op('dve', lambda e: e.tensor_scalar(out=qnr[:], in0=qnr[:], scalar1=0.125, scalar2=None, op0=ALU.mult),
                     reads=['qnr0'], writes=['qnr'])
                for i in range(2):
                    P.op('pool', lambda e, i=i: e.memset(vaug[i][:], 1.0), writes=[('vaug', i)])
                    P.op('pool', lambda e, i=i: e.memset(vat[i][:], 1.0), writes=[('vat', i)])
                xsrc = xin if l == 0 else X

                def frontA(t):
                    tb = t % 2
                    w = 1 if t < NCT else 0
                    r0 = t * 128
                    P.dma('sp', lambda e, tb=tb, r0=r0: e.dma_start(out=xt[tb][:], in_=xsrc[r0:r0 + 128, :]),
                          writes=[('xt', tb)])
                    P.dma('sp', lambda e, tb=tb, t=t: e.dma_start(out=rp[tb][:], in_=rope_d[t]), writes=[('rp', tb)])
                    norm_to_hT((junk, stt[tb], xn), xt[tb][:], ('xt', tb), w, 0,
                               lambda k, tb=tb: hT[tb][:, k, :], lambda k, tb=tb: ('hT', tb, k), 0, 1, tb)
                    if dbg and t == 2:
                        P.dma('sp', lambda e, tb=tb: e.dma_start(out=dhT, in_=hT[tb][:]), reads=[('hT', tb, k) for k in range(8)], writes=['dhT'])

                def mmA(t):
                    tb = t % 2
                    gcols = [(0, 512), (512, 1024), (1024, 1536), (1536, 2048), (2048, 2320)]
                    for g, (c0, c1) in enumerate(gcols):
                        for k in range(8):
                            P.op('pe', lambda e, g=g, c0=c0, c1=c1, k=k, tb=tb: e.matmul(
                                bk(2 + g)[:, 0:c1 - c0], lhsT=hT[tb][:, k, :], rhs=win[:, k, c0:c1],
                                start=(k == 0), stop=(k == 7)),
                                reads=[('hT', tb, k), 'win'], writes=[('pp', g)], banks=[2 + g])

                def postA(t):
                    tb = t % 2
                    r0 = t * 128
                    P.op('dve', lambda e: e.tensor_tensor(out=gt[:], in0=bk(6)[:, 256:272], in1=gbr[:], op=ALU.add),
                         reads=[('pp', 4), 'gbr'], writes=['gt'], banks=[6])
                    P.op('act', lambda e: e.activation(out=e8[:, 0:4], in_=gt[:, 4:8], func=AF.Exp, scale=-1.0),
                         reads=['gt'], writes=['e8a'])
                    P.op('act', lambda e: e.activation(out=e8[:, 4:8], in_=gt[:, 12:16], func=AF.Exp, scale=-1.0),
                         reads=['gt'], writes=['e8b'])
                    P.op('dve', lambda e: e.tensor_scalar(out=e8[:], in0=e8[:], scalar1=1.0, scalar2=None, op0=ALU.add),
                         reads=['e8a', 'e8b'], writes=['e8'])
                    P.op('act', lambda e: e.activation(out=L8[:], in_=e8[:], func=AF.Ln), reads=['e8'], writes=['L8'])
                    for i, tri in enumerate((trif, trib, onesf)):
                        P.op('pe', lambda e, i=i, tri=tri: e.matmul(bk(7)[:, i * 8:(i + 1) * 8], lhsT=tri[:], rhs=L8[:],
                                                                    start=True, stop=True),
                             reads=['L8', tri.name], writes=[('pg', i)], banks=[7])
                    P.op('dve', lambda e: e.tensor_copy(out=cs[:], in_=bk(7)[:, 0:24]),
                         reads=[('pg', 0), ('pg', 1), ('pg', 2)], writes=['cs'], banks=[7])
                    P.op('act', lambda e: e.activation(out=sc[:, 0:4], in_=cs[:, 0:4], func=AF.Exp, scale=-1.0),
                         reads=['cs'], writes=['sc0'])
                    P.op('act', lambda e: e.activation(out=sc[:, 4:8], in_=cs[:, 12:16], func=AF.Exp, scale=-1.0),
                         reads=['cs'], writes=['sc1'])
                    P.op('dve', lambda e: e.tensor_scalar(out=sc[:, 0:8], in0=sc[:, 0:8], scalar1=0.125, scalar2=None,
                                                          op0=ALU.mult), reads=['sc0', 'sc1'], writes=['scq'])
                    P.op('dve', lambda e: e.tensor_tensor(out=tmp8[:, 0:4], in0=gt[:, 0:4], in1=cs[:, 0:4], op=ALU.add),
                         reads=['gt', 'cs'], writes=['t8a'])
                    P.op('dve', lambda e: e.tensor_tensor(out=tmp8[:, 4:8], in0=gt[:, 8:12], in1=cs[:, 12:16], op=ALU.add),
                         reads=['gt', 'cs'], writes=['t8b'])
                    P.op('act', lambda e: e.activation(out=sc[:, 8:16], in_=tmp8[:, 0:8], func=AF.Exp),
                         reads=['t8a', 't8b'], writes=['sck'])
                    P.op('dve', lambda e: e.tensor_tensor(out=tmp8[:, 8:16], in0=tmp8[:, 0:8], in1=cs[:, 16:24],
                                                          op=ALU.subtract), reads=['t8a', 't8b', 'cs'], writes=['t8c'])
                    P.op('act', lambda e: e.activation(out=sc[:, 16:24], in_=tmp8[:, 8:16], func=AF.Exp),
                         reads=['t8c'], writes=['sckk'])
                    P.op('act', lambda e: e.activation(out=dec[:], in_=cs[:, 16:24], func=AF.Exp, scale=-1.0),
                         reads=['cs'], writes=['dec'])
                    P.dma('sp', lambda e, t=t: e.dma_start(out=DEC[t], in_=dec[0:64, :]), reads=['dec'], writes=[('DEC', t)])
                    P.op('act', lambda e: e.copy(out=pqk[:], in_=bk(2)), reads=[('pp', 0)], writes=['pqk'], banks=[2])
                    specs = [(0, 0, 'scq'), (0, 4, 'scq'), (256, 8, 'sck'), (256, 12, 'sck'), (256, 16, 'sckk'), (256, 20, 'sckk')]
                    for i, (c0, s0, skey) in enumerate(specs):
                        P.op('dve' if i % 2 == 0 else 'pool', lambda e, i=i, c0=c0, s0=s0: e.tensor_tensor(
                            out=sq6[i][:], in0=pqk[:, c0:c0 + 256].rearrange("p (h d) -> p h d", h=4),
                            in1=sc[:, s0:s0 + 4].unsqueeze(2).to_broadcast([128, 4, 64]), op=ALU.mult),
                            reads=['pqk', skey], writes=[('sq6', i)])
                    for i in range(4):
                        for h in range(4):
                            j = i * 4 + h
                            bb = 0 if j < 8 else 1
                            P.op('pe', lambda e, i=i, h=h, j=j, bb=bb: e.transpose(
                                out=bkb(bb)[0:64, (j % 8) * 128:(j % 8 + 1) * 128], in_=sq6[i][:, h, :], identity=identb[:]),
                                reads=[('sq6', i), 'identb'], writes=[('ptr', bb)], banks=[bb])
                    for bb in range(2):
                        P.op('act' if bb else 'dve', lambda e, bb=bb: e.tensor_copy(
                            out=trs[:, bb * 8:(bb + 1) * 8, :], in_=bkb(bb)[0:64, :].rearrange("p (a b) -> p a b", a=8))
                            if bb == 0 else e.copy(
                            out=trs[:, bb * 8:(bb + 1) * 8, :], in_=bkb(bb)[0:64, :].rearrange("p (a b) -> p a b", a=8)),
                            reads=[('ptr', bb)], writes=[('trs', bb)], banks=[bb])
                    for i, dst in enumerate((MQ[0], MQ[1], MK[0], MK[1])):
                        P.dma('sp', lambda e, i=i, dst=dst, r0=r0: e.dma_start(
                            out=dst[:, :, r0:r0 + 128].rearrange("h d n -> d h n"), in_=trs[:, i * 4:(i + 1) * 4, :]),
                            reads=[('trs', i // 2)], writes=[('MQK', i, t)])
                    for d_ in range(2):
                        P.dma('sp', lambda e, d_=d_, r0=r0: e.dma_start(out=MK2[d_][r0:r0 + 128], in_=sq6[4 + d_][:]),
                              reads=[('sq6', 4 + d_)], writes=[('MK2', d_, t)])
                    P.op('act', lambda e, tb=tb: e.copy(out=vaug[tb][:, :, 0:128],
                                                        in_=bk(3).rearrange("p (h d) -> p h d", h=4)),
                         reads=[('pp', 1)], writes=[('vaug', tb)], banks=[3])
                    P.dma('sp', lambda e, tb=tb, r0=r0: e.dma_start(out=MVA[r0:r0 + 128], in_=vaug[tb][:]),
                          reads=[('vaug', tb)], writes=[('MVA', t)])
                    P.op('act', lambda e: e.activation(out=mot[:], in_=bk(4), func=AF.Exp, scale=-1.0),
                         reads=[('pp', 2)], writes=['mot'], banks=[4])
                    P.op('act', lambda e: e.activation(out=mot[:], in_=mot[:], func=AF.Ln, bias=onesf[:, 0:1], scale=1.0),
                         reads=['mot', 'onesf'], writes=['mot'])
                    P.op('act', lambda e: e.activation(out=mob[:], in_=mot[:], func=AF.Exp, scale=-1.0), reads=['mot'], writes=['mob'])
                    P.dma('sp', lambda e, r0=r0: e.dma_start(out=MO[r0:r0 + 128, :], in_=mob[:]), reads=['mob'],
                          writes=[('MO', t)])
                    P.op('act', lambda e: e.activation(out=sqq[:, 0:512], in_=bk(5), func=AF.Square),
                         reads=[('pp', 3)], writes=['sqq_q'], banks=[5])
                    P.op('act', lambda e: e.activation(out=sqq[:, 512:640], in_=bk(6)[:, 0:128], func=AF.Square),
                         reads=[('pp', 4)], writes=['sqq_k'], banks=[6])
                    P.op('dve', lambda e: e.tensor_reduce(out=aqs[:, 0:10], in_=sqq[:].rearrange("p (h d) -> p h d", d=64),
                                                          axis=AX.X, op=ALU.add),
                         reads=['sqq_q', 'sqq_k'], writes=[('aq', 'ss')])
                    rstd_ops(aqs, 0, 10, 10, 10, 1.0 / 64, 'aq')
                    P.op('dve', lambda e: e.tensor_tensor(
                        out=qn[:, 0:512].rearrange("p (h d) -> p h d", d=64), in0=bk(5).rearrange("p (h d) -> p h d", d=64),
                        in1=aqs[:, 10:18].unsqueeze(2).to_broadcast([128, 8, 64]), op=ALU.mult),
                        reads=[('pp', 3), ('aq', 'r')], writes=['qn_q'], banks=[5])
                    P.op('dve', lambda e: e.tensor_tensor(
                        out=qn[:, 512:640].rearrange("p (h d) -> p h d", d=64),
                        in0=bk(6)[:, 0:128].rearrange("p (h d) -> p h d", d=64),
                        in1=aqs[:, 18:20].unsqueeze(2).to_broadcast([128, 2, 64]), op=ALU.mult),
                        reads=[('pp', 4), ('aq', 'r')], writes=['qn_k'], banks=[6])
                    P.op('pool', lambda e: e.tensor_tensor(
                        out=qn2[:, 0:512].rearrange("p (h d) -> p h d", d=64), in0=qn[:, 0:512].rearrange("p (h d) -> p h d", d=64),
                        in1=qnr[:].unsqueeze(1).to_broadcast([128, 8, 64]), op=ALU.mult),
                        reads=['qn_q', 'qnr'], writes=['qn2_q'])
                    P.op('pool', lambda e: e.tensor_tensor(
                        out=qn2[:, 512:640].rearrange("p (h d) -> p h d", d=64),
                        in0=qn[:, 512:640].rearrange("p (h d) -> p h d", d=64),
                        in1=knr[:].unsqueeze(1).to_broadcast([128, 2, 64]), op=ALU.mult),
                        reads=['qn_k', 'knr'], writes=['qn2_k'])
                    q4 = qn2[:].rearrange("p (h j two) -> p h j two", j=32, two=2)
                    qe, qo = q4[:, :, :, 0], q4[:, :, :, 1]
                    cosb = rp[tb][:, 0:32].unsqueeze(1).to_broadcast([128, 10, 32])
                    sinb = rp[tb][:, 32:64].unsqueeze(1).to_broadcast([128, 10, 32])
                    rd = ['qn2_q', 'qn2_k', ('rp', tb)]
                    P.op('dve', lambda e, cosb=cosb: e.tensor_tensor(out=rt[0][:], in0=qe, in1=cosb, op=ALU.mult), reads=rd, writes=['rt0'])
                    P.op('pool', lambda e, sinb=sinb: e.tensor_tensor(out=rt[1][:], in0=qo, in1=sinb, op=ALU.mult), reads=rd, writes=['rt1'])
                    P.op('dve', lambda e, sinb=sinb: e.tensor_tensor(out=rt[2][:], in0=qe, in1=sinb, op=ALU.mult), reads=rd, writes=['rt2'])
                    P.op('pool', lambda e, cosb=cosb: e.tensor_tensor(out=rt[3][:], in0=qo, in1=cosb, op=ALU.mult), reads=rd, writes=['rt3'])
                    r4 = qr[:].rearrange("p (h j two) -> p h j two", j=32, two=2)
                    P.op('dve', lambda e: e.tensor_tensor(out=r4[:, :, :, 0], in0=rt[0][:], in1=rt[1][:], op=ALU.subtract),
                         reads=['rt0', 'rt1'], writes=['qr_e'])
                    P.op('pool', lambda e: e.tensor_tensor(out=r4[:, :, :, 1], in0=rt[2][:], in1=rt[3][:], op=ALU.add),
                         reads=['rt2', 'rt3'], writes=['qr_o'])
                    for h in range(10):
                        bb = 0 if h < 8 else 1
                        P.op('pe', lambda e, h=h, bb=bb: e.transpose(
                            out=bkb(bb)[0:64, (h % 8) * 128:(h % 8 + 1) * 128], in_=qr[:, h * 64:(h + 1) * 64], identity=identb[:]),
                            reads=['qr_e', 'qr_o', 'identb'], writes=[('ptq', bb)], banks=[bb])
                    P.op('dve', lambda e: e.tensor_copy(out=qT[:, 0:8, :], in_=bkb(0)[0:64, :].rearrange("p (a b) -> p a b", a=8)),
                         reads=[('ptq', 0)], writes=['qT_q'], banks=[0])
                    P.op('act', lambda e: e.copy(out=qT[:, 8:10, :], in_=bkb(1)[0:64, 0:256].rearrange("p (a b) -> p a b", a=2)),
                         reads=[('ptq', 1)], writes=['qT_k'], banks=[1])
                    P.dma('sp', lambda e, r0=r0: e.dma_start(out=QT[:, :, r0:r0 + 128].rearrange("h d n -> d h n"),
                                                             in_=qT[:, 0:8, :]), reads=['qT_q'], writes=[('QT', t)])
                    P.dma('sp', lambda e, r0=r0: e.dma_start(out=KT[:, :, r0:r0 + 128].rearrange("h d n -> d h n"),
                                                             in_=qT[:, 8:10, :]), reads=['qT_k'], writes=[('KT', t)])
                    P.op('act', lambda e, tb=tb: e.copy(out=vat[tb][:, :, 0:64],
                                                        in_=bk(6)[:, 128:256].rearrange("p (h d) -> p h d", h=2)),
                         reads=[('pp', 4)], writes=[('vat', tb)], banks=[6])
                    P.dma('sp', lambda e, tb=tb, r0=r0: e.dma_start(out=VA[r0:r0 + 128], in_=vat[tb][:]),
                          reads=[('vat', tb)], writes=[('VA', t)])

                frontA(0)
                for t in range(NT):
                    mmA(t)
                    if t + 1 < NT:
                        frontA(t + 1)
                    postA(t)
                P.end_phase()

        def phaseB(l):
            need_ctx = (l < NL - 1)
            with ExitStack() as st:
                Cst = sbt(st, "Cst", [64, 4, 129])
                Cbf = sbt(st, "Cbf", [64, 4, 129], BF16)
                mnr = sbt(st, "mnr", [128, 512])
                qTt = [sbt(st, "bqT%d" % i, [64, 4, 128], BF16) for i in range(2)]
                kTt = [sbt(st, "bkT%d" % i, [64, 4, 128], BF16) for i in range(2)]
                k2t = [sbt(st, "bk2%d" % i, [128, 4, 64], BF16) for i in range(2)]
                vat = [sbt(st, "bva%d" % i, [128, 4, 129], BF16) for i in range(2)]
                dct = [sbt(st, "bdc%d" % i, [64, 8]) for i in range(2)]
                SM = sbt(st, "SM", [128, 4, 128], BF16)
                r4 = sbt(st, "r4", [128, 8])
                hd = [sbt(st, "hd%d" % i, [128, 512]) for i in range(2)]
                hbt = sbt(st, "hbt", [128, 512])
                hs = sbt(st, "hs", [128, 512])
                sqh = sbt(st, "sqh", [128, 512])
                hst = sbt(st, "hst", [128, 12])
                hn = sbt(st, "hn", [128, 512])
                hn2 = sbt(st, "hn2", [128, 512])
                mot = sbt(st, "bmo", [128, 512], BF16)
                ym = sbt(st, "ym", [128, 512], BF16)
                P.dma('sp', lambda e: e.dma_start(out=mnr[:], in_=mnw[l:l + 1, :].partition_broadcast(128)), writes=['mnr'])
                it = 0
                flatB = []
                for d_ in (1, 0):
                    order = ([1, 0] + list(range(NT - 1, NCT - 1, -1))) if d_ == 1 else list(range(NT))
                    flatB += [(d_, c, i == 0) for i, c in enumerate(order)]

                def loadsB(idx):
                    if idx >= len(flatB):
                        return
                    d_, c, _ = flatB[idx]
                    b = idx % 2
                    r0 = c * 128
                    P.dma('sp', lambda e, b=b, r0=r0, d_=d_: e.dma_start(
                        out=qTt[b][:], in_=MQ[d_][:, :, r0:r0 + 128].rearrange("h d n -> d h n")), writes=[('bq', b)])
                    P.dma('sp', lambda e, b=b, r0=r0, d_=d_: e.dma_start(
                        out=kTt[b][:], in_=MK[d_][:, :, r0:r0 + 128].rearrange("h d n -> d h n")), writes=[('bk', b)])
                    P.dma('sp', lambda e, b=b, r0=r0, d_=d_: e.dma_start(out=k2t[b][:], in_=MK2[d_][r0:r0 + 128]),
                          writes=[('bk2', b)])
                    P.dma('sp', lambda e, b=b, r0=r0: e.dma_start(out=vat[b][:], in_=MVA[r0:r0 + 128]), writes=[('bva', b)])
                    P.dma('sp', lambda e, b=b, c=c: e.dma_start(out=dct[b][:], in_=DEC[c]), writes=[('bdc', b)])

                loadsB(0)
                for idxB, (d_, c, first) in enumerate(flatB):
                    mask = maskb if d_ == 1 else maskf
                    if first:
                        P.op('dve', lambda e: e.memset(Cst[:], 0.0), writes=['Cst'])
                    if True:
                        b = it % 2
                        it += 1
                        r0 = c * 128
                        need_out = (c >= NCT) or need_ctx
                        loadsB(idxB + 1)
                        if need_out:
                            P.op('act', lambda e: e.copy(out=Cbf[:], in_=Cst[:]), reads=['Cst'], writes=['Cbf'])
                            for h in range(4):
                                P.op('pe', lambda e, h=h, b=b: e.matmul(bk(0)[:, h * 128:(h + 1) * 128], lhsT=kTt[b][:, h, :],
                                                                        rhs=qTt[b][:, h, :], start=True, stop=True),
                                     reads=[('bq', b), ('bk', b)], writes=['pS'], banks=[0])
                            P.op('dve', lambda e, mask=mask: e.tensor_tensor(
                                out=SM[:], in0=bk(0).rearrange("p (h n) -> p h n", h=4),
                                in1=mask[:].unsqueeze(1).to_broadcast([128, 4, 128]), op=ALU.mult),
                                reads=['pS', mask.name], writes=['SM'], banks=[0])
                            for h in range(4):
                                bb = 1 + h // 2
                                o0 = (h % 2) * 129
                                P.op('pe', lambda e, h=h, b=b, bb=bb, o0=o0: e.matmul(
                                    bk(bb)[:, o0:o0 + 129], lhsT=SM[:, h, :], rhs=vat[b][:, h, :], start=(h % 2 == 0),
                                    stop=False, skip_group_check=True),
                                    reads=['SM', ('bva', b)], writes=[('pO', bb)], banks=[bb])
                                P.op('pe', lambda e, h=h, b=b, bb=bb, o0=o0: e.matmul(
                                    bk(bb)[:, o0:o0 + 129], lhsT=qTt[b][:, h, :], rhs=Cbf[:, h, :], start=False,
                                    stop=True, skip_group_check=True),
                                    reads=[('bq', b), 'Cbf'], writes=[('pO', bb)], banks=[bb])
                            hb_ = it % 2
                            for bb in (1, 2):
                                ov = bk(bb)[:, 0:258].rearrange("p (h n) -> p h n", h=2)
                                P.op('dve', lambda e, bb=bb, ov=ov: e.tensor_scalar(
                                    out=r4[:, (bb - 1) * 2:(bb - 1) * 2 + 2], in0=ov[:, :, 128], scalar1=-1.0, scalar2=1.0,
                                    op0=ALU.mult, op1=ALU.max),
                                    reads=[('pO', bb)], writes=[('r4n', bb)], banks=[bb])
                                P.op('dve', lambda e, bb=bb, ov=ov: e.tensor_tensor(
                                    out=r4[:, (bb - 1) * 2:(bb - 1) * 2 + 2], in0=r4[:, (bb - 1) * 2:(bb - 1) * 2 + 2],
                                    in1=ov[:, :, 128], op=ALU.max),
                                    reads=[('pO', bb), ('r4n', bb)], writes=[('r4a', bb)], banks=[bb])
                                P.op('dve', lambda e, bb=bb: e.reciprocal(out=r4[:, 4 + (bb - 1) * 2:4 + (bb - 1) * 2 + 2],
                                                                          in_=r4[:, (bb - 1) * 2:(bb - 1) * 2 + 2]),
                                     reads=[('r4a', bb)], writes=[('r4b', bb)])
                                P.op('dve', lambda e, bb=bb, ov=ov, hb_=hb_: e.tensor_tensor(
                                    out=hd[hb_][:, (bb - 1) * 256:(bb - 1) * 256 + 256].rearrange("p (h n) -> p h n", h=2),
                                    in0=ov[:, :, 0:128],
                                    in1=r4[:, 4 + (bb - 1) * 2:4 + (bb - 1) * 2 + 2].unsqueeze(2).to_broadcast([128, 2, 128]),
                                    op=ALU.mult),
                                    reads=[('pO', bb), ('r4b', bb)], writes=[('hd', hb_, bb)], banks=[bb])
                            if d_ == 1:
                                P.dma('sp', lambda e, r0=r0, hb_=hb_: e.dma_start(out=HB[r0:r0 + 128, :], in_=hd[hb_][:]),
                                      reads=[('hd', hb_, 1), ('hd', hb_, 2)], writes=[('HB', c)])
                            else:
                                P.dma('sp', lambda e, r0=r0: e.dma_start(out=hbt[:], in_=HB[r0:r0 + 128, :]), writes=['hbt'])
                                P.dma('sp', lambda e, r0=r0: e.dma_start(out=mot[:], in_=MO[r0:r0 + 128, :]), writes=['bmo'])
                                P.op('pool', lambda e, hb_=hb_: e.tensor_tensor(out=hs[:], in0=hd[hb_][:], in1=hbt[:], op=ALU.add),
                                     reads=[('hd', hb_, 1), ('hd', hb_, 2), 'hbt'], writes=['hs'])
                                P.op('act', lambda e: e.activation(out=sqh[:], in_=hs[:], func=AF.Square),
                                     reads=['hs'], writes=['sqh'])
                                P.op('dve', lambda e: e.tensor_reduce(out=hst[:, 0:4], in_=sqh[:].rearrange("p (h n) -> p h n", h=4),
                                                                      axis=AX.X, op=ALU.add), reads=['sqh'], writes=[('hst', 'ss')])
                                rstd_ops(hst, 0, 4, 8, 4, 1.0 / 128, 'hst')
                                P.op('dve', lambda e: e.tensor_tensor(
                                    out=hn[:].rearrange("p (h n) -> p h n", h=4), in0=hs[:].rearrange("p (h n) -> p h n", h=4),
                                    in1=hst[:, 8:12].unsqueeze(2).to_broadcast([128, 4, 128]), op=ALU.mult),
                                    reads=['hs', ('hst', 'r')], writes=['hn'])
                                P.op('pool', lambda e: e.tensor_tensor(out=hn2[:], in0=hn[:], in1=mnr[:], op=ALU.mult),
                                     reads=['hn', 'mnr'], writes=['hn2'])
                                P.op('dve', lambda e: e.tensor_tensor(out=ym[:], in0=hn2[:], in1=mot[:], op=ALU.mult),
                                     reads=['hn2', 'bmo'], writes=['ym'])
                                P.dma('sp', lambda e, r0=r0: e.dma_start(out=Y[r0:r0 + 128, 0:512], in_=ym[:]),
                                      reads=['ym'], writes=[('Ym', c)])
                        for h in range(4):
                            bb = 3 + h // 2
                            o0 = (h % 2) * 129
                            P.op('pe', lambda e, h=h, b=b, bb=bb, o0=o0: e.matmul(
                                bk(bb)[0:64, o0:o0 + 129], lhsT=k2t[b][:, h, :], rhs=vat[b][:, h, :], start=(h % 2 == 0),
                                stop=True, skip_group_check=True),
                                reads=[('bk2', b), ('bva', b)], writes=[('pC', bb)], banks=[bb])
                        for h in range(4):
                            bb = 3 + h // 2
                            o0 = (h % 2) * 129
                            P.op('dve', lambda e, h=h, b=b, bb=bb, o0=o0, d_=d_: e.scalar_tensor_tensor(
                                out=Cst[:, h, :], in0=Cst[:, h, :], scalar=dct[b][:, d_ * 4 + h:d_ * 4 + h + 1],
                                in1=bk(bb)[0:64, o0:o0 + 129], op0=ALU.mult, op1=ALU.add),
                                reads=[('pC', bb), ('bdc', b), 'Cst'], writes=['Cst'], banks=[bb])
                P.end_phase()

        def phaseC(l):
            need_ctx = (l < NL - 1)
            with ExitStack() as st:
                KTs = sbt(st, "KTs", [64, 2, NTOK], BF16)
                VAs = sbt(st, "VAs", [128, NT, 2, 65], BF16)
                qt = [sbt(st, "cq%d" % i, [64, 512], BF16) for i in range(2)]
                pT = [sbt(st, "cp%d" % i, [128, 512], BF16) for i in range(3)]
                rr = [sbt(st, "crr%d" % i, [128, 4]) for i in range(2)]
                ot = [sbt(st, "cot%d" % i, [128, 4, 64], BF16) for i in range(2)]
                for kv in range(2):
                    P.dma('sp', lambda e, kv=kv: e.dma_start(out=KTs[:, kv, :], in_=KT[kv]), writes=[('KTs', kv)])
                VAv = VA.rearrange("(t p) k d -> p t k d", p=128)
                for t0 in range(0, NT, 8):
                    t1 = min(NT, t0 + 8)
                    P.dma('sp', lambda e, t0=t0, t1=t1: e.dma_start(out=VAs[:, t0:t1], in_=VAv[:, t0:t1]), writes=[('VAs', t0)])
                groups = []
                if need_ctx:
                    groups.append((0, NCT * 128, list(range(NCT))))
                for q0 in range(NCT * 128, NTOK, 512):
                    groups.append((q0, min(512, NTOK - q0), list(range(NT))))
                if SPARSE and l == 0:
                    prep_moe_weights()
                heads = [(q0, nq, kts, h) for (q0, nq, kts) in groups for h in range(8)]
                seq = [(hi, ji) for hi in range(len(heads)) for ji in range(len(heads[hi][2]))]
                NSB = 3

                def load_q(hi):
                    if hi >= len(heads):
                        return
                    q0, nq, kts, h = heads[hi]
                    b = hi % 2
                    P.dma('sp', lambda e, b=b, h=h, q0=q0, nq=nq: e.dma_start(out=qt[b][:, 0:nq], in_=QT[h, :, q0:q0 + nq]),
                          writes=[('cq', b)])

                def emit_S(n):
                    if n >= len(seq):
                        return
                    hi, ji = seq[n]
                    q0, nq, kts, h = heads[hi]
                    j = kts[ji]
                    kv = h // 4
                    b = hi % 2
                    bs = n % NSB
                    P.op('pe', lambda e, bs=bs, kv=kv, j=j, b=b, nq=nq: e.matmul(
                        bk(bs)[:, 0:nq], lhsT=KTs[:, kv, j * 128:(j + 1) * 128], rhs=qt[b][:, 0:nq], start=True, stop=True),
                        reads=[('KTs', kv), ('cq', b)], writes=[('cS', bs)], banks=[bs])

                load_q(0)
                load_q(1)
                emit_S(0)
                emit_S(1)
                for n, (hi, ji) in enumerate(seq):
                    q0, nq, kts, h = heads[hi]
                    nsub = nq // 128
                    j = kts[ji]
                    kv = h // 4
                    b = hi % 2
                    bo = 3 + b
                    bs = n % NSB
                    pb = n % 3
                    if ji == 0 and hi >= 1:
                        load_q(hi + 1)
                    emit_S(n + 2)
                    P.op('act', lambda e, bs=bs, pb=pb, nq=nq: e.activation(out=pT[pb][:, 0:nq], in_=bk(bs)[:, 0:nq], func=AF.Exp),
                         reads=[('cS', bs)], writes=[('cp', pb)], banks=[bs])
                    for s in range(nsub):
                        P.op('pe', lambda e, s=s, pb=pb, j=j, kv=kv, bo=bo, ji=ji, kts=kts: e.matmul(
                            bk(bo)[:, s * 65:(s + 1) * 65], lhsT=pT[pb][:, s * 128:(s + 1) * 128], rhs=VAs[:, j, kv, :],
                            start=(ji == 0 and s == 0), stop=(ji == len(kts) - 1), skip_group_check=True),
                            reads=[('cp', pb), ('VAs', (j // 8) * 8)], writes=[('cO', bo)], banks=[bo])
                    if ji == len(kts) - 1:
                        ov = bk(bo)[:, 0:nsub * 65].rearrange("p (s d) -> p s d", d=65)
                        P.op('dve', lambda e, b=b, ov=ov, nsub=nsub: e.reciprocal(out=rr[b][:, 0:nsub], in_=ov[:, :, 64]),
                             reads=[('cO', bo)], writes=[('crr', b)], banks=[bo])
                        P.op('dve', lambda e, b=b, ov=ov, nsub=nsub: e.tensor_tensor(
                            out=ot[b][:, 0:nsub, :], in0=ov[:, :, 0:64],
                            in1=rr[b][:, 0:nsub].unsqueeze(2).to_broadcast([128, nsub, 64]), op=ALU.mult),
                            reads=[('cO', bo), ('crr', b)], writes=[('cot', b)], banks=[bo])
                        P.dma('sp', lambda e, b=b, q0=q0, nq=nq, h=h, nsub=nsub: e.dma_start(
                            out=Y[q0:q0 + nq, 512 + h * 64:512 + (h + 1) * 64].rearrange("(s p) d -> p s d", p=128),
                            in_=ot[b][:, 0:nsub, :]), reads=[('cot', b)], writes=[('Ya', q0, h)])
                P.end_phase()

        def phaseD(l):
            last = (l == NL - 1)
            moe = (l % 2 == 1)
            F_ = DFE if moe else DFF
            NFC = F_ // 128
            experts = list(range(NEXP)) if moe else [0]
            with ExitStack() as st:
                wo = sbt(st, "wo", [128, 8, D], BF16)
                fnr = sbt(st, "fnr", [128, D])
                rtr = sbt(st, "rtr", [128, 8, 8])
                yt = [sbt(st, "dy%d" % i, [128, D], BF16) for i in range(2)]
                yT = sbt(st, "dyT", [128, 8, 128], BF16)
                xr = [sbt(st, "xr%d" % i, [128, D]) for i in range(4)]
                tmpx = sbt(st, "tmpx", [128, 512])
                junk = sbt(st, "djunk", [128, D], BF16)
                stt = [sbt(st, "dstt%d" % i, [128, 8]) for i in range(2)]
                xn = sbt(st, "dxn", [128, D])
                h2T = sbt(st, "h2T", [128, 8, 512], BF16)
                h2F = sbt(st, "h2F", [128, 8, 128])
                lg = sbt(st, "lg", [128, 8])
                m8 = sbt(st, "m8", [128, 8])
                gg = sbt(st, "gg", [128, 8])
                eq = [sbt(st, "eq%d" % i, [128, 8]) for i in range(2)]
                W8 = [sbt(st, "W8_%d" % i, [128, 8]) for i in range(4)]
                yacc = [sbt(st, "yacc%d" % i, [128, D]) for i in range(4)] if moe else None
                wgs = [sbt(st, "wgs%d" % i, [128, 8, 512], BF16) for i in range(2)]
                wus = [sbt(st, "wus%d" % i, [128, 8, 512], BF16) for i in range(2)]
                wds = [sbt(st, "wds%d" % i, [128, 4, 512], BF16) for i in range(2)]
                sg = [sbt(st, "sg%d" % i, [128, 512]) for i in range(2)]
                AT = sbt(st, "AT", [128, NFC, 512], BF16)
                ob = [sbt(st, "ob%d" % i, [128, D]) for i in range(2)]
                P.dma('sp', lambda e: e.dma_start(out=wo[:], in_=WOb[l].rearrange("(k p) n -> p k n", p=128)), writes=['wo'])
                if last:
                    P.dma('sp', lambda e: e.dma_start(out=fnr[:], in_=fnw[0:1, :].partition_broadcast(128)), writes=['fnr'])
                if moe:
                    P.dma('sp', lambda e: e.dma_start(out=rtr[:], in_=router.rearrange("(k p) n -> p k n", p=128)), writes=['rtr'])
                Gw = [MGb[e_] for e_ in range(NEXP)] if moe else [FGb]
                Uw = [MUb[e_] for e_ in range(NEXP)] if moe else [FUb]
                Dw = [MDb[e_] for e_ in range(NEXP)] if moe else [FDb]
                tiles = []
                if not last:
                    tiles.append(list(range(NCT)))
                for t0 in range(NCT, NT, 4):
                    tiles.append(list(range(t0, min(NT, t0 + 4))))
                wi = 0
                di = 0
                yi = 0
                for tl in tiles:
                    nsub = len(tl)
                    ntok = nsub * 128
                    w = 1 if tl[0] < NCT else 0
                    for s, t in enumerate(tl):
                        r0 = t * 128
                        yb = yi % 2
                        yi += 1
                        P.dma('sp', lambda e, yb=yb, r0=r0: e.dma_start(out=yt[yb][:], in_=Y[r0:r0 + 128, :]),
                              reads=[], writes=[('dy', yb)])
                        xs_ = xin if l == 0 else X
                        P.dma('sp', lambda e, s=s, r0=r0, xs_=xs_: e.dma_start(out=xr[s][:], in_=xs_[r0:r0 + 128, :]),
                              writes=[('xr', s)])
                        for k in range(8):
                            P.op('pe', lambda e, k=k, yb=yb: e.transpose(out=bkb(0)[:, k * 128:(k + 1) * 128],
                                                                        in_=yt[yb][:, k * 128:(k + 1) * 128], identity=identb[:]),
                                 reads=[('dy', yb), 'identb'], writes=['pyT'], banks=[0])
                        P.op('dve', lambda e: e.tensor_copy(out=yT[:, 0:4, :], in_=bkb(0)[:, 0:512].rearrange("p (a b) -> p a b", a=4)),
                             reads=['pyT'], writes=[('yT', 0)], banks=[0])
                        P.op('act', lambda e: e.copy(out=yT[:, 4:8, :], in_=bkb(0)[:, 512:1024].rearrange("p (a b) -> p a b", a=4)),
                             reads=['pyT'], writes=[('yT', 1)], banks=[0])
                        for half in range(2):
                            for k in range(8):
                                P.op('pe', lambda e, k=k, half=half: e.matmul(
                                    bk(1 + half), lhsT=yT[:, k, :], rhs=wo[:, k, half * 512:(half + 1) * 512],
                                    start=(k == 0), stop=(k == 7)),
                                    reads=[('yT', k // 4), 'wo'], writes=[('pwo', half)], banks=[1 + half])
                            P.op('dve', lambda e, half=half, w=w: e.tensor_tensor(
                                out=tmpx[:], in0=bk(1 + half), in1=GATE[w][0][:, half * 512:(half + 1) * 512], op=ALU.mult),
                                reads=[('pwo', half), (GATE[w][0].name, half)], writes=['tmpx'], banks=[1 + half])
                            P.op('pool', lambda e, half=half, s=s: e.tensor_tensor(
                                out=xr[s][:, half * 512:(half + 1) * 512], in0=tmpx[:], in1=xr[s][:, half * 512:(half + 1) * 512],
                                op=ALU.add), reads=['tmpx', ('xr', s)], writes=[('xr', s)])
                        tb = s % 2
                        norm_to_hT((junk, stt[tb], xn), xr[s][:], ('xr', s), w, 1,
                                   lambda k, s=s: h2T[:, k, s * 128:(s + 1) * 128], lambda k, s=s: ('h2T', s, k), 3, 4, tb,
                                   hF_dst_fn=(lambda k: h2F[:, k, :]) if moe else None)
                        if moe:
                            for k in range(8):
                                P.op('pe', lambda e, k=k: e.matmul(bk(5)[:, 0:8], lhsT=h2F[:, k, :], rhs=rtr[:, k, :],
                                                                   start=(k == 0), stop=(k == 7)),
                                     reads=[('hF', k), 'rtr'], writes=['plg'], banks=[5])
                            P.op('dve', lambda e: e.tensor_copy(out=lg[:], in_=bk(5)[:, 0:8]), reads=['plg'], writes=['lg'], banks=[5])
                            P.op('dve', lambda e: e.max(out=m8[:], in_=lg[:]), reads=['lg'], writes=['m8'])
                            P.op('dve', lambda e: e.tensor_tensor(out=gg[:, 0:1], in0=m8[:, 1:2], in1=m8[:, 0:1], op=ALU.subtract),
                                 reads=['m8'], writes=['gg0'])
                            P.op('act', lambda e: e.activation(out=gg[:, 1:2], in_=gg[:, 0:1], func=AF.Exp), reads=['gg0'], writes=['gg1'])
                            P.op('dve', lambda e: e.tensor_scalar(out=gg[:, 2:3], in0=gg[:, 1:2], scalar1=1.0, scalar2=None, op0=ALU.add),
                                 reads=['gg1'], writes=['gg2'])
                            P.op('dve', lambda e: e.reciprocal(out=gg[:, 3:4], in_=gg[:, 2:3]), reads=['gg2'], writes=['gg3'])
                            P.op('dve', lambda e: e.tensor_tensor(out=gg[:, 4:5], in0=gg[:, 1:2], in1=gg[:, 3:4], op=ALU.mult),
                                 reads=['gg1', 'gg3'], writes=['gg4'])
                            P.op('dve', lambda e: e.tensor_scalar(out=eq[0][:], in0=lg[:], scalar1=m8[:, 0:1], scalar2=gg[:, 3:4],
                                                                  op0=ALU.is_equal, op1=ALU.mult),
                                 reads=['lg', 'm8', 'gg3'], writes=['eq0'])
                            P.op('dve', lambda e: e.tensor_scalar(out=eq[1][:], in0=lg[:], scalar1=m8[:, 1:2], scalar2=gg[:, 4:5],
                                                                  op0=ALU.is_equal, op1=ALU.mult),
                                 reads=['lg', 'm8', 'gg4'], writes=['eq1'])
                            P.op('dve', lambda e, s=s: e.tensor_tensor(out=W8[s][:], in0=eq[0][:], in1=eq[1][:], op=ALU.add),
                                 reads=['eq0', 'eq1'], writes=[('W8', s)])
                    h2keys = [('h2T', s, k) for s in range(nsub) for k in range(8)]
                    for ei, e_ in enumerate(experts):
                        for fg0 in range(0, F_, 512):
                            fw = min(512, F_ - fg0)
                            wb = wi % 2
                            wi += 1
                            P.dma('sp', lambda e, wb=wb, e_=e_, fg0=fg0, fw=fw: e.dma_start(
                                out=wgs[wb][:, :, 0:fw], in_=Gw[e_][:, fg0:fg0 + fw].rearrange("(k p) f -> p k f", p=128)),
                                writes=[('wgs', wb)])
                            P.dma('sp', lambda e, wb=wb, e_=e_, fg0=fg0, fw=fw: e.dma_start(
                                out=wus[wb][:, :, 0:fw], in_=Uw[e_][:, fg0:fg0 + fw].rearrange("(k p) f -> p k f", p=128)),
                                writes=[('wus', wb)])
                            for fc in range(fw // 128):
                                fidx = fg0 // 128 + fc
                                gb = fidx % 2
                                for k in range(8):
                                    P.op('pe', lambda e, k=k, wb=wb, fc=fc, gb=gb, ntok=ntok: e.matmul(
                                        bk(6 + gb)[:, 0:ntok], lhsT=wgs[wb][:, k, fc * 128:(fc + 1) * 128], rhs=h2T[:, k, 0:ntok],
                                        start=(k == 0), stop=(k == 7)),
                                        reads=[('wgs', wb)] + ([('h2T', s, k) for s in range(nsub)]), writes=[('pG', gb)], banks=[6 + gb])
                                for k in range(8):
                                    P.op('pe', lambda e, k=k, wb=wb, fc=fc, gb=gb, ntok=ntok: e.matmul(
                                        bk(4 + gb)[:, 0:ntok], lhsT=wus[wb][:, k, fc * 128:(fc + 1) * 128], rhs=h2T[:, k, 0:ntok],
                                        start=(k == 0), stop=(k == 7)),
                                        reads=[('wus', wb)] + ([('h2T', s, k) for s in range(nsub)]), writes=[('pU', gb)], banks=[4 + gb])
                                P.op('act', lambda e, gb=gb, ntok=ntok: e.activation(out=sg[gb][:, 0:ntok], in_=bk(6 + gb)[:, 0:ntok],
                                                                                     func=AF.Silu),
                                     reads=[('pG', gb)], writes=[('sg', gb)], banks=[6 + gb])
                                P.op('dve', lambda e, gb=gb, ntok=ntok, fidx=fidx: e.tensor_tensor(
                                    out=AT[:, fidx, 0:ntok], in0=bk(4 + gb)[:, 0:ntok], in1=sg[gb][:, 0:ntok], op=ALU.mult),
                                    reads=[('pU', gb), ('sg', gb)], writes=[('AT', fidx)], banks=[4 + gb])
                        for half in range(2):
                            for fg0 in range(0, F_, 512):
                                fw = min(512, F_ - fg0)
                                db = di % 2
                                di += 1
                                P.dma('sp', lambda e, db=db, e_=e_, fg0=fg0, fw=fw, half=half: e.dma_start(
                                    out=wds[db][:, 0:fw // 128, :],
                                    in_=Dw[e_][fg0:fg0 + fw, half * 512:(half + 1) * 512].rearrange("(c p) d -> p c d", p=128)),
                                    writes=[('wds', db)])
                                for fc in range(fw // 128):
                                    fidx = fg0 // 128 + fc
                                    for s in range(nsub):
                                        P.op('pe', lambda e, s=s, fidx=fidx, fc=fc, db=db: e.matmul(
                                            bk(s), lhsT=AT[:, fidx, s * 128:(s + 1) * 128], rhs=wds[db][:, fc, :],
                                            start=(fidx == 0), stop=(fidx == NFC - 1)),
                                            reads=[('AT', fidx), ('wds', db)], writes=[('pD', s)], banks=[s])
                            for s in range(nsub):
                                hs_ = slice(half * 512, (half + 1) * 512)
                                if moe:
                                    if ei == 0:
                                        P.op('dve', lambda e, s=s, hs_=hs_, e_=e_: e.tensor_scalar(
                                            out=yacc[s][:, hs_], in0=bk(s), scalar1=W8[s][:, e_:e_ + 1], scalar2=None, op0=ALU.mult),
                                            reads=[('pD', s), ('W8', s)], writes=[('yacc', s, half)], banks=[s])
                                    else:
                                        P.op('dve', lambda e, s=s, hs_=hs_, e_=e_: e.scalar_tensor_tensor(
                                            out=yacc[s][:, hs_], in0=bk(s), scalar=W8[s][:, e_:e_ + 1], in1=yacc[s][:, hs_],
                                            op0=ALU.mult, op1=ALU.add),
                                            reads=[('pD', s), ('W8', s), ('yacc', s, half)], writes=[('yacc', s, half)], banks=[s])
                                else:
                                    P.op('dve', lambda e, s=s, hs_=hs_, w=w: e.tensor_tensor(
                                        out=tmpx[:], in0=bk(s), in1=GATE[w][1][:, hs_], op=ALU.mult),
                                        reads=[('pD', s), (GATE[w][1].name, half)], writes=['tmpx'], banks=[s])
                                    P.op('pool', lambda e, s=s, hs_=hs_: e.tensor_tensor(
                                        out=xr[s][:, hs_], in0=tmpx[:], in1=xr[s][:, hs_], op=ALU.add),
                                        reads=['tmpx', ('xr', s)], writes=[('xr', s)])
                    for s, t in enumerate(tl):
                        r0 = t * 128
                        if moe:
                            for half in range(2):
                                hs_ = slice(half * 512, (half + 1) * 512)
                                P.op('pool', lambda e, s=s, hs_=hs_, w=w: e.tensor_tensor(
                                    out=yacc[s][:, hs_], in0=yacc[s][:, hs_], in1=GATE[w][1][:, hs_], op=ALU.mult),
                                    reads=[('yacc', s, half), (GATE[w][1].name, half)], writes=[('yacc', s, half)])
                                P.op('dve', lambda e, s=s, hs_=hs_: e.tensor_tensor(
                                    out=xr[s][:, hs_], in0=yacc[s][:, hs_], in1=xr[s][:, hs_], op=ALU.add),
                                    reads=[('yacc', s, half), ('xr', s)], writes=[('xr', s)])
                        if not last:
                            P.dma('sp', lambda e, s=s, r0=r0: e.dma_start(out=X[r0:r0 + 128, :], in_=xr[s][:]),
                                  reads=[('xr', s)], writes=[('X', t)])
                        else:
                            tb = s % 2
                            key = ('fst', tb)
                            P.op('act', lambda e, s=s, tb=tb: e.activation(out=junk[:], in_=xr[s][:], func=AF.Square,
                                                                           accum_out=stt[tb][:, 4:5]),
                                 reads=[('xr', s)], writes=['junk', (key, 'ss')])
                            rstd_ops(stt[tb], 4, 5, 6, 1, 1.0 / D, key)
                            P.op('dve', lambda e, s=s, tb=tb: e.scalar_tensor_tensor(
                                out=ob[tb][:], in0=xr[s][:], scalar=stt[tb][:, 6:7], in1=fnr[:], op0=ALU.mult, op1=ALU.mult),
                                reads=[('xr', s), (key, 'r'), 'fnr'], writes=[('ob', tb)])
                            o0 = r0 - NCT * 128
                            P.dma('sp', lambda e, tb=tb, o0=o0: e.dma_start(out=out[o0:o0 + 128, :], in_=ob[tb][:]),
                                  reads=[('ob', tb)], writes=[('out', t)])
                P.end_phase()

        def phaseD_sparse(l):
            assert l == NL - 1
            w = 0
            oR, oJ, oG = 0, 8, 8 + NJ
            oCGU = oG + NG
            oCD = oCGU + 7
            oCH = oCD + 14
            REGB = mconst[:, oR:oR + 8]
            J512 = mconst[:, oJ:oJ + NJ]
            G40 = mconst[:, oG:oG + NG]
            CGU = mconst[:, oCGU:oCGU + 7]
            CD = mconst[:, oCD:oCD + 14]
            CH = mconst[:, oCH:oCH + 4]
            NFC = DFE // 128
            with ExitStack() as pst:
                E0 = sbt(pst, "E0", [128, NLT, 8])
                E1 = sbt(pst, "E1", [128, NLT, 8])
                POS = sbt(pst, "POS", [128, NLT, 8])
                G12 = sbt(pst, "G12", [128, NLT, 2])
                RUN = sbt(pst, "RUN", [128, 8])
                BASE = sbt(pst, "BASE", [128, 8])
                IDXGU = sbt(pst, "IDXGU", [128, NG, 7], I32)
                IDXU = sbt(pst, "IDXU", [128, NG, 7], I32)
                IDXD = sbt(pst, "IDXD", [128, NG, 14], I32)
                IDXH = sbt(pst, "IDXH", [128, NG, 4], I32)
                RAi = sbt(pst, "RAi", [128, NLT], I32)
                RBi = sbt(pst, "RBi", [128, NLT], I32)
                fnr = sbt(pst, "sfnr", [128, D])
                with ExitStack() as st:
                    wo = sbt(st, "wo", [128, 8, D], BF16)
                    rtr = sbt(st, "rtr", [128, 8, 8])
                    yt = [sbt(st, "dy%d" % i, [128, D], BF16) for i in range(2)]
                    yT = sbt(st, "dyT", [128, 8, 128], BF16)
                    xr = [sbt(st, "xr%d" % i, [128, D]) for i in range(3)]
                    tmpx = sbt(st, "tmpx", [128, 512])
                    junk = sbt(st, "djunk", [128, D], BF16)
                    stt = [sbt(st, "dstt%d" % i, [128, 8]) for i in range(2)]
                    xn = sbt(st, "dxn", [128, D])
                    h2 = sbt(st, "h2", [128, D])
                    H2b = [sbt(st, "H2b%d" % i, [128, D], BF16) for i in range(2)]
                    h2F = sbt(st, "h2F", [128, 8, 128])
                    lg = sbt(st, "lg", [128, 8])
                    m8 = sbt(st, "m8", [128, 8])
                    gg = sbt(st, "gg", [128, 8])
                    M8 = sbt(st, "M8", [128, 8])
                    t8 = sbt(st, "t8", [128, 8])
                    p8 = sbt(st, "p8", [128, 16])
                    slf = sbt(st, "slf", [128, 2])
                    sli = [sbt(st, "sli%d" % i, [128, 2], I32) for i in range(2)]
                    P.dma('sp', lambda e: e.dma_start(out=wo[:], in_=WOb[l].rearrange("(k p) n -> p k n", p=128)), writes=['wo'])
                    P.dma('sp', lambda e: e.dma_start(out=fnr[:], in_=fnw[0:1, :].partition_broadcast(128)), writes=['fnr'])
                    P.dma('sp', lambda e: e.dma_start(out=rtr[:], in_=router.rearrange("(k p) n -> p k n", p=128)), writes=['rtr'])
                    P.op('dve', lambda e: e.memset(RUN[:], 0.0), writes=['RUN'])

                    def loadsD1(ti_):
                        if ti_ >= NLT:
                            return
                        r0_ = (NCT + ti_) * 128
                        b_ = ti_ % 2
                        s_ = ti_ % 3
                        P.dma('sp', lambda e, b_=b_, r0_=r0_: e.dma_start(out=yt[b_][:], in_=Y[r0_:r0_ + 128, :]), writes=[('dy', b_)])
                        P.dma('sp', lambda e, s_=s_, r0_=r0_: e.dma_start(out=xr[s_][:], in_=X[r0_:r0_ + 128, :]), writes=[('xr', s_)])

                    def frontD1(ti):
                        t = NCT + ti
                        r0 = t * 128
                        yb = ti % 2
                        s = ti % 3
                        if ti == 0:
                            loadsD1(0)
                        loadsD1(ti + 1)
                        for k in range(8):
                            P.op('pe', lambda e, k=k, yb=yb: e.transpose(out=bkb(0)[:, k * 128:(k + 1) * 128],
                                                                        in_=yt[yb][:, k * 128:(k + 1) * 128], identity=identb[:]),
                                 reads=[('dy', yb), 'identb'], writes=['pyT'], banks=[0])
                        P.op('dve', lambda e: e.tensor_copy(out=yT[:, 0:4, :], in_=bkb(0)[:, 0:512].rearrange("p (a b) -> p a b", a=4)),
                             reads=['pyT'], writes=[('yT', 0)], banks=[0])
                        P.op('act', lambda e: e.copy(out=yT[:, 4:8, :], in_=bkb(0)[:, 512:1024].rearrange("p (a b) -> p a b", a=4)),
                             reads=['pyT'], writes=[('yT', 1)], banks=[0])
                        for half in range(2):
                            for k in range(8):
                                P.op('pe', lambda e, k=k, half=half: e.matmul(
                                    bk(1 + half), lhsT=yT[:, k, :], rhs=wo[:, k, half * 512:(half + 1) * 512],
                                    start=(k == 0), stop=(k == 7)),
                                    reads=[('yT', k // 4), 'wo'], writes=[('pwo', half)], banks=[1 + half])
                            P.op('dve', lambda e, half=half: e.tensor_tensor(
                                out=tmpx[:], in0=bk(1 + half), in1=GATE[w][0][:, half * 512:(half + 1) * 512], op=ALU.mult),
                                reads=[('pwo', half), (GATE[w][0].name, half)], writes=['tmpx'], banks=[1 + half])
                            P.op('pool', lambda e, half=half, s=s: e.tensor_tensor(
                                out=xr[s][:, half * 512:(half + 1) * 512], in0=tmpx[:], in1=xr[s][:, half * 512:(half + 1) * 512],
                                op=ALU.add), reads=['tmpx', ('xr', s)], writes=[('xr', s)])
                        P.dma('sp', lambda e, s=s, r0=r0: e.dma_start(out=X[r0:r0 + 128, :], in_=xr[s][:]),
                              reads=[('xr', s)], writes=[('X', t)])

                    def backD1(ti):
                        t = NCT + ti
                        s = ti % 3
                        tb = ti % 2
                        key = ('nst', tb)
                        P.op('act', lambda e, s=s, tb=tb: e.activation(out=junk[:], in_=xr[s][:], func=AF.Square,
                                                                       accum_out=stt[tb][:, 0:1]),
                             reads=[('xr', s)], writes=['junk', (key, 'ss')])
                        rstd_ops(stt[tb], 0, 1, 2, 1, 1.0 / D, key)
                        P.op('dve', lambda e, s=s, tb=tb: e.tensor_scalar(out=xn[:], in0=xr[s][:], scalar1=stt[tb][:, 2:3], scalar2=None,
                                                                          op0=ALU.mult), reads=[('xr', s), (key, 'r')], writes=['xn'])
                        P.op('pool', lambda e: e.tensor_tensor(out=h2[:], in0=xn[:], in1=G2R[:], op=ALU.mult),
                             reads=['xn', ('G2R', 0), ('G2R', 1)], writes=['h2a'])
                        P.op('dve', lambda e: e.tensor_tensor(out=h2[:], in0=h2[:], in1=S2R[:], op=ALU.add),
                             reads=['h2a', ('S2R', 0), ('S2R', 1)], writes=['h2'])
                        hb = ti % 2
                        P.op('act', lambda e, hb=hb: e.copy(out=H2b[hb][:], in_=h2[:]), reads=['h2'], writes=[('H2b', hb)])
                        for k in range(8):
                            bb = 3 if k < 4 else 4
                            P.op('pe', lambda e, k=k, bb=bb: e.transpose(out=bk(bb)[:, (k % 4) * 128:(k % 4 + 1) * 128],
                                                                        in_=h2[:, k * 128:(k + 1) * 128], identity=identf[:]),
                                 reads=['h2', 'identf'], writes=[('pT', bb)], banks=[bb])
                        P.op('dve', lambda e: e.tensor_copy(out=h2F[:, 0:4, :], in_=bk(3).rearrange("p (a b) -> p a b", a=4)),
                             reads=[('pT', 3)], writes=[('h2F', 0)], banks=[3])
                        P.op('act', lambda e: e.copy(out=h2F[:, 4:8, :], in_=bk(4).rearrange("p (a b) -> p a b", a=4)),
                             reads=[('pT', 4)], writes=[('h2F', 1)], banks=[4])
                        for k in range(8):
                            P.op('pe', lambda e, k=k: e.matmul(bk(5)[:, 0:8], lhsT=h2F[:, k, :], rhs=rtr[:, k, :],
                                                               start=(k == 0), stop=(k == 7)),
                                 reads=[('h2F', k // 4), 'rtr'], writes=['plg'], banks=[5])
                        P.op('dve', lambda e: e.tensor_copy(out=lg[:], in_=bk(5)[:, 0:8]), reads=['plg'], writes=['lg'], banks=[5])
                        P.op('dve', lambda e: e.max(out=m8[:], in_=lg[:]), reads=['lg'], writes=['m8'])
                        P.op('dve', lambda e: e.tensor_tensor(out=gg[:, 0:1], in0=m8[:, 1:2], in1=m8[:, 0:1], op=ALU.subtract),
                             reads=['m8'], writes=['gg0'])
                        P.op('act', lambda e: e.activation(out=gg[:, 1:2], in_=gg[:, 0:1], func=AF.Exp), reads=['gg0'], writes=['gg1'])
                        P.op('dve', lambda e: e.tensor_scalar(out=gg[:, 2:3], in0=gg[:, 1:2], scalar1=1.0, scalar2=None, op0=ALU.add),
                             reads=['gg1'], writes=['gg2'])
                        P.op('dve', lambda e, ti=ti: e.reciprocal(out=G12[:, ti, 0:1], in_=gg[:, 2:3]), reads=['gg2'], writes=[('G12a', ti)])
                        P.op('dve', lambda e, ti=ti: e.tensor_tensor(out=G12[:, ti, 1:2], in0=gg[:, 1:2], in1=G12[:, ti, 0:1], op=ALU.mult),
                             reads=['gg1', ('G12a', ti)], writes=[('G12b', ti)])
                        P.op('dve', lambda e, ti=ti: e.tensor_scalar(out=E0[:, ti, :], in0=lg[:], scalar1=m8[:, 0:1], scalar2=None,
                                                                     op0=ALU.is_equal), reads=['lg', 'm8'], writes=[('E0', ti)])
                        P.op('dve', lambda e, ti=ti: e.tensor_scalar(out=E1[:, ti, :], in0=lg[:], scalar1=m8[:, 1:2], scalar2=None,
                                                                     op0=ALU.is_equal), reads=['lg', 'm8'], writes=[('E1', ti)])
                        P.op('dve', lambda e, ti=ti: e.tensor_tensor(out=M8[:], in0=E0[:, ti, :], in1=E1[:, ti, :], op=ALU.add),
                             reads=[('E0', ti), ('E1', ti)], writes=['M8'])
                        P.op('pe', lambda e: e.matmul(bk(6)[:, 0:8], lhsT=trif[:], rhs=M8[:], start=True, stop=True),
                             reads=['M8', 'trif'], writes=['ppx'], banks=[6])
                        P.op('pe', lambda e: e.matmul(bk(6)[:, 8:16], lhsT=onesf[:], rhs=M8[:], start=True, stop=True),
                             reads=['M8', 'onesf'], writes=['ppt'], banks=[6])
                        P.op('dve', lambda e: e.tensor_copy(out=p8[:], in_=bk(6)[:, 0:16]), reads=['ppx', 'ppt'], writes=['p8'], banks=[6])
                        P.op('dve', lambda e: e.tensor_tensor(out=t8[:], in0=p8[:, 0:8], in1=M8[:], op=ALU.subtract),
                             reads=['p8', 'M8'], writes=['t8'])
                        P.op('dve', lambda e, ti=ti: e.tensor_tensor(out=POS[:, ti, :], in0=t8[:], in1=RUN[:], op=ALU.add),
                             reads=['t8', 'RUN'], writes=[('POS', ti)])
                        P.op('dve', lambda e: e.tensor_tensor(out=RUN[:], in0=RUN[:], in1=p8[:, 8:16], op=ALU.add),
                             reads=['RUN', 'p8', ('POS', ti)], writes=['RUN'])
                        P.op('dve', lambda e, ti=ti: e.tensor_tensor(out=t8[:], in0=POS[:, ti, :], in1=REGB, op=ALU.add),
                             reads=[('POS', ti), 'mconst', 't8'], writes=['t8r'])
                        P.op('dve', lambda e, ti=ti: e.tensor_tensor(out=M8[:], in0=t8[:], in1=E0[:, ti, :], op=ALU.mult),
                             reads=['t8r', ('E0', ti), 'M8', 'p8', 't8'], writes=['M8a'])
                        P.op('dve', lambda e: e.tensor_reduce(out=slf[:, 0:1], in_=M8[:], axis=AX.X, op=ALU.add),
                             reads=['M8a'], writes=['slfa'])
                        P.op('dve', lambda e, ti=ti: e.tensor_tensor(out=M8[:], in0=t8[:], in1=E1[:, ti, :], op=ALU.mult),
                             reads=['t8r', ('E1', ti), 'M8a', 'slfa'], writes=['M8b'])
                        P.op('dve', lambda e: e.tensor_reduce(out=slf[:, 1:2], in_=M8[:], axis=AX.X, op=ALU.add),
                             reads=['M8b'], writes=['slfb'])
                        P.op('dve', lambda e, hb=hb: e.tensor_copy(out=sli[hb][:], in_=slf[:]), reads=['slfa', 'slfb'], writes=[('sli', hb)])
                        for j in range(2):
                            P.dma('pool', lambda e, hb=hb, j=j: e.indirect_dma_start(
                                out=HS, out_offset=bass.IndirectOffsetOnAxis(ap=sli[hb][:, j:j + 1], axis=0),
                                in_=H2b[hb][:], in_offset=None),
                                reads=[('H2b', hb), ('sli', hb)], writes=[('HS', ti, j)])
                    frontD1(0)
                    for ti in range(NLT):
                        if ti + 1 < NLT:
                            frontD1(ti + 1)
                        backD1(ti)
                    ng = sbt(st, "ng", [128, 8])
                    cum = sbt(st, "cum", [128, 8])
                    cj = sbt(st, "cj", [128, NJ])
                    cg = sbt(st, "cg", [128, NG])
                    EG = sbt(st, "EG", [128, NG])
                    CE = sbt(st, "CE", [128, NG])
                    LG = sbt(st, "LG", [128, NG])
                    ROWB = sbt(st, "ROWB", [128, NG])
                    EGa = sbt(st, "EGa", [128, NG])
                    EGb = sbt(st, "EGb", [128, NG])
                    for e_ in range(NEXP):
                        P.op('dve', lambda e, e_=e_: e.tensor_scalar(out=cj[:], in0=J512, scalar1=RUN[:, e_:e_ + 1], scalar2=None,
                                                                     op0=ALU.is_lt), reads=['RUN', 'mconst', ('ngr', e_ - 1)], writes=[('cj', e_)])
                        P.op('dve', lambda e, e_=e_: e.tensor_reduce(out=ng[:, e_:e_ + 1], in_=cj[:], axis=AX.X, op=ALU.add),
                             reads=[('cj', e_)], writes=[('ngr', e_)])
                    ngk = [('ngr', e_) for e_ in range(NEXP)]
                    P.op('dve', lambda e: e.tensor_copy(out=cum[:, 0:1], in_=ng[:, 0:1]), reads=ngk, writes=[('cum', 0)])
                    for e_ in range(1, NEXP):
                        P.op('dve', lambda e, e_=e_: e.tensor_tensor(out=cum[:, e_:e_ + 1], in0=cum[:, e_ - 1:e_], in1=ng[:, e_:e_ + 1],
                                                                     op=ALU.add), reads=ngk + [('cum', e_ - 1)], writes=[('cum', e_)])
                    cumk = [('cum', e_) for e_ in range(NEXP)]
                    P.op('dve', lambda e: e.tensor_tensor(out=BASE[:], in0=cum[:], in1=ng[:], op=ALU.subtract), reads=cumk + ngk, writes=['BASE0'])
                    P.op('dve', lambda e: e.tensor_scalar(out=BASE[:], in0=BASE[:], scalar1=512.0, scalar2=None, op0=ALU.mult),
                         reads=['BASE0'], writes=['BASE'])
                    P.op('dve', lambda e: e.memset(EG[:], 0.0), writes=['EG'])
                    P.op('dve', lambda e: e.memset(CE[:], 0.0), writes=['CE'])
                    for e_ in range(NEXP):
                        P.op('dve', lambda e, e_=e_: e.tensor_scalar(out=cg[:], in0=G40, scalar1=cum[:, e_:e_ + 1], scalar2=None,
                                                                     op0=ALU.is_ge), reads=cumk + ['mconst', 'EG', 'CE'], writes=['cg'])
                        P.op('dve', lambda e: e.tensor_tensor(out=EG[:], in0=EG[:], in1=cg[:], op=ALU.add), reads=['cg', 'EG'], writes=['EG'])
                        P.op('dve', lambda e, e_=e_: e.scalar_tensor_tensor(out=CE[:], in0=cg[:], scalar=ng[:, e_:e_ + 1], in1=CE[:],
                                                                            op0=ALU.mult, op1=ALU.add), reads=['cg', 'CE'] + ngk, writes=['CE'])
                    P.op('dve', lambda e: e.tensor_scalar(out=EG[:], in0=EG[:], scalar1=float(NEXP - 1), scalar2=None, op0=ALU.min),
                         reads=['EG'], writes=['EGc'])
                    P.op('dve', lambda e: e.tensor_tensor(out=LG[:], in0=G40, in1=CE[:], op=ALU.subtract), reads=['CE', 'mconst'], writes=['LG0'])
                    P.op('dve', lambda e: e.tensor_scalar(out=LG[:], in0=LG[:], scalar1=float(NJ - 1), scalar2=512.0, op0=ALU.min, op1=ALU.mult),
                         reads=['LG0'], writes=['LG'])
                    P.op('dve', lambda e: e.scalar_tensor_tensor(out=ROWB[:], in0=EG[:], scalar=float(CAP), in1=LG[:], op0=ALU.mult, op1=ALU.add),
                         reads=['EGc', 'LG'], writes=['ROWB'])
                    P.op('dve', lambda e: e.tensor_scalar(out=EGa[:], in0=EG[:], scalar1=float(7 * 128), scalar2=None, op0=ALU.mult),
                         reads=['EGc'], writes=['EGa'])
                    P.op('dve', lambda e: e.tensor_scalar(out=EGb[:], in0=EG[:], scalar1=float(14 * 128), scalar2=None, op0=ALU.mult),
                         reads=['EGc'], writes=['EGb'])
                    P.op('dve', lambda e: e.tensor_tensor(out=IDXGU[:], in0=EGa[:].unsqueeze(2).to_broadcast([128, NG, 7]),
                                                          in1=CGU.unsqueeze(1).to_broadcast([128, NG, 7]), op=ALU.add),
                         reads=['EGa', 'mconst'], writes=['IDXGU'])
                    P.op('dve', lambda e: e.tensor_tensor(out=IDXU[:], in0=EGa[:].unsqueeze(2).to_broadcast([128, NG, 7]),
                                                          in1=CGU.unsqueeze(1).to_broadcast([128, NG, 7]), op=ALU.add),
                         reads=['EGa', 'mconst'], writes=['IDXU'])
                    P.op('dve', lambda e: e.tensor_tensor(out=IDXD[:], in0=EGb[:].unsqueeze(2).to_broadcast([128, NG, 14]),
                                                          in1=CD.unsqueeze(1).to_broadcast([128, NG, 14]), op=ALU.add),
                         reads=['EGb', 'mconst'], writes=['IDXD'])
                    P.op('dve', lambda e: e.tensor_tensor(out=IDXH[:], in0=ROWB[:].unsqueeze(2).to_broadcast([128, NG, 4]),
                                                          in1=CH.unsqueeze(1).to_broadcast([128, NG, 4]), op=ALU.add),
                         reads=['ROWB', 'mconst'], writes=['IDXH'])
                    TP = sbt(st, "TP", [128, NLT, 8])
                    TQ = sbt(st, "TQ", [128, NLT, 8])
                    RAf = sbt(st, "RAf", [128, NLT])
                    posk = [('POS', ti) for ti in range(NLT)]
                    P.op('dve', lambda e: e.tensor_tensor(out=TP[:], in0=POS[:], in1=BASE[:].unsqueeze(1).to_broadcast([128, NLT, 8]),
                                                          op=ALU.add), reads=posk + ['BASE'], writes=['TP'])
                    for (EE, RI, nm) in ((E0, RAi, 'E0'), (E1, RBi, 'E1')):
                        P.op('dve', lambda e, EE=EE: e.tensor_tensor(out=TQ[:], in0=TP[:], in1=EE[:], op=ALU.mult),
                             reads=['TP', 'RAf'] + [(nm, ti) for ti in range(NLT)], writes=['TQ'])
                        P.op('dve', lambda e: e.tensor_reduce(out=RAf[:], in_=TQ[:], axis=AX.X, op=ALU.add), reads=['TQ'], writes=['RAf'])
                        P.op('dve', lambda e, RI=RI: e.tensor_copy(out=RI[:], in_=RAf[:]), reads=['RAf'], writes=[RI.name])
                    P.end_phase()
                with ExitStack() as st:
                    hs = [sbt(st, "hs%d" % i, [128, D], BF16) for i in range(8)]
                    h2T = sbt(st, "h2T", [128, 8, 512], BF16)
                    wgs = [sbt(st, "wgs%d" % i, [128, 8 * 512], BF16) for i in range(2)]
                    wus = [sbt(st, "wus%d" % i, [128, 8 * 512], BF16) for i in range(2)]
                    wds = [sbt(st, "wds%d" % i, [128, 4 * 512], BF16) for i in range(2)]
                    sg = [sbt(st, "sg%d" % i, [128, 512]) for i in range(2)]
                    AT = sbt(st, "AT", [128, NFC, 512], BF16)
                    yst = [sbt(st, "yst%d" % i, [128, 512]) for i in range(2)]
                    wi = 0
                    di = 0
                    yi = 0
                    for g in range(NG):
                        for s in range(4):
                            hb = (g % 2) * 4 + s
                            P.dma('pool', lambda e, hb=hb, g=g, s=s: e.indirect_dma_start(
                                out=hs[hb][:], out_offset=None, in_=HS,
                                in_offset=bass.IndirectOffsetOnAxis(ap=IDXH[:, g, s:s + 1], axis=0)), writes=[('hs', hb)])
                        for s in range(4):
                            hb = (g % 2) * 4 + s
                            bb = 4 + s
                            for k in range(8):
                                P.op('pe', lambda e, k=k, hb=hb, bb=bb: e.transpose(
                                    out=bkb(bb)[:, k * 128:(k + 1) * 128], in_=hs[hb][:, k * 128:(k + 1) * 128], identity=identb[:]),
                                    reads=[('hs', hb), 'identb'], writes=[('phT', bb)], banks=[bb])
                            if s % 2 == 0:
                                P.op('dve', lambda e, s=s, bb=bb: e.tensor_copy(
                                    out=h2T[:, :, s * 128:(s + 1) * 128], in_=bkb(bb)[:, :].rearrange("p (k n) -> p k n", k=8)),
                                    reads=[('phT', bb)], writes=[('h2T', s)], banks=[bb])
                            else:
                                P.op('act', lambda e, s=s, bb=bb: e.copy(
                                    out=h2T[:, :, s * 128:(s + 1) * 128], in_=bkb(bb)[:, :].rearrange("p (k n) -> p k n", k=8)),
                                    reads=[('phT', bb)], writes=[('h2T', s)], banks=[bb])
                        h2k = [('h2T', s) for s in range(4)]
                        for c in range(7):
                            wb = wi % 2
                            wi += 1
                            P.dma('pool', lambda e, wb=wb, g=g, c=c: e.indirect_dma_start(
                                out=wgs[wb][:], out_offset=None, in_=MGp,
                                in_offset=bass.IndirectOffsetOnAxis(ap=IDXGU[:, g, c:c + 1], axis=0)), writes=[('wgs', wb)])
                            P.dma('pool', lambda e, wb=wb, g=g, c=c: e.indirect_dma_start(
                                out=wus[wb][:], out_offset=None, in_=MUp,
                                in_offset=bass.IndirectOffsetOnAxis(ap=IDXU[:, g, c:c + 1], axis=0)), writes=[('wus', wb)])
                            for fc in range(4):
                                fidx = c * 4 + fc
                                gb = fidx % 2
                                for k in range(8):
                                    P.op('pe', lambda e, k=k, wb=wb, fc=fc, gb=gb: e.matmul(
                                        bk(6 + gb), lhsT=wgs[wb][:, k * 512 + fc * 128:k * 512 + (fc + 1) * 128], rhs=h2T[:, k, :],
                                        start=(k == 0), stop=(k == 7)),
                                        reads=[('wgs', wb)] + h2k, writes=[('pG', gb)], banks=[6 + gb])
                                for k in range(8):
                                    P.op('pe', lambda e, k=k, wb=wb, fc=fc, gb=gb: e.matmul(
                                        bk(4 + gb), lhsT=wus[wb][:, k * 512 + fc * 128:k * 512 + (fc + 1) * 128], rhs=h2T[:, k, :],
                                        start=(k == 0), stop=(k == 7)),
                                        reads=[('wus', wb)] + h2k, writes=[('pU', gb)], banks=[4 + gb])
                                P.op('act', lambda e, gb=gb: e.activation(out=sg[gb][:], in_=bk(6 + gb), func=AF.Silu),
                                     reads=[('pG', gb)], writes=[('sg', gb)], banks=[6 + gb])
                                P.op('dve', lambda e, gb=gb, fidx=fidx: e.tensor_tensor(
                                    out=AT[:, fidx, :], in0=bk(4 + gb), in1=sg[gb][:], op=ALU.mult),
                                    reads=[('pU', gb), ('sg', gb)], writes=[('AT', fidx)], banks=[4 + gb])
                        for half in range(2):
                            for q in range(7):
                                db = di % 2
                                di += 1
                                P.dma('pool', lambda e, db=db, g=g, half=half, q=q: e.indirect_dma_start(
                                    out=wds[db][:], out_offset=None, in_=MDp,
                                    in_offset=bass.IndirectOffsetOnAxis(ap=IDXD[:, g, half * 7 + q:half * 7 + q + 1], axis=0)), writes=[('wds', db)])
                                for fc in range(4):
                                    fidx = q * 4 + fc
                                    for s in range(4):
                                        P.op('pe', lambda e, s=s, fidx=fidx, fc=fc, db=db: e.matmul(
                                            bk(s), lhsT=AT[:, fidx, s * 128:(s + 1) * 128], rhs=wds[db][:, fc * 512:(fc + 1) * 512],
                                            start=(fidx == 0), stop=(fidx == NFC - 1)),
                                            reads=[('AT', fidx), ('wds', db)], writes=[('pD', s)], banks=[s])
                            for s in range(4):
                                yb = yi % 2
                                yi += 1
                                if yb == 0:
                                    P.op('dve', lambda e, s=s, yb=yb: e.tensor_copy(out=yst[yb][:], in_=bk(s)),
                                         reads=[('pD', s)], writes=[('yst', yb)], banks=[s])
                                else:
                                    P.op('act', lambda e, s=s, yb=yb: e.copy(out=yst[yb][:], in_=bk(s)),
                                         reads=[('pD', s)], writes=[('yst', yb)], banks=[s])
                                rr0 = g * 512 + s * 128
                                P.dma('sp', lambda e, yb=yb, rr0=rr0, half=half: e.dma_start(
                                    out=YS[rr0:rr0 + 128, half * 512:(half + 1) * 512], in_=yst[yb][:]),
                                    reads=[('yst', yb)], writes=[('YS', g, s, half)])
                    P.end_phase()
                with ExitStack() as st:
                    ya = [sbt(st, "ya%d" % i, [128, D]) for i in range(2)]
                    yb_ = [sbt(st, "yb%d" % i, [128, D]) for i in range(2)]
                    x1 = [sbt(st, "x1_%d" % i, [128, D]) for i in range(2)]
                    accs = [sbt(st, "acc%d" % i, [128, D]) for i in range(2)]
                    acc2s = [sbt(st, "acc2_%d" % i, [128, D]) for i in range(2)]
                    junk = sbt(st, "djunk", [128, D], BF16)
                    stt = [sbt(st, "dstt%d" % i, [128, 8]) for i in range(2)]
                    ob = [sbt(st, "ob%d" % i, [128, D]) for i in range(2)]
                    def loadsD3(ti_):
                        if ti_ >= NLT:
                            return
                        r0_ = (NCT + ti_) * 128
                        b_ = ti_ % 2
                        P.dma('pool', lambda e, b_=b_, ti_=ti_: e.indirect_dma_start(
                            out=ya[b_][:], out_offset=None, in_=YS, in_offset=bass.IndirectOffsetOnAxis(ap=RAi[:, ti_:ti_ + 1], axis=0)),
                            writes=[('ya', b_)])
                        P.dma('pool', lambda e, b_=b_, ti_=ti_: e.indirect_dma_start(
                            out=yb_[b_][:], out_offset=None, in_=YS, in_offset=bass.IndirectOffsetOnAxis(ap=RBi[:, ti_:ti_ + 1], axis=0)),
                            writes=[('yb', b_)])
                        P.dma('sp', lambda e, b_=b_, r0_=r0_: e.dma_start(out=x1[b_][:], in_=X[r0_:r0_ + 128, :]), writes=[('x1', b_)])

                    for ti in range(NLT):
                        t = NCT + ti
                        r0 = t * 128
                        b = ti % 2
                        if ti == 0:
                            loadsD3(0)
                        loadsD3(ti + 1)
                        acc = accs[b]
                        acc2 = acc2s[b]
                        P.op('dve', lambda e, b=b, ti=ti, acc=acc: e.tensor_scalar(out=acc[:], in0=ya[b][:], scalar1=G12[:, ti, 0:1], scalar2=None,
                                                                                   op0=ALU.mult), reads=[('ya', b)], writes=[('acc0', b)])
                        P.op('dve', lambda e, b=b, ti=ti, acc=acc: e.scalar_tensor_tensor(out=acc[:], in0=yb_[b][:], scalar=G12[:, ti, 1:2], in1=acc[:],
                                                                                          op0=ALU.mult, op1=ALU.add),
                             reads=[('yb', b), ('acc0', b)], writes=[('acc1', b)])
                        P.op('pool', lambda e, acc=acc, acc2=acc2: e.tensor_tensor(out=acc2[:], in0=acc[:], in1=GATE[w][1][:], op=ALU.mult),
                             reads=[('acc1', b)], writes=[('acc2', b)])
                        P.op('dve', lambda e, b=b, acc2=acc2: e.tensor_tensor(out=x1[b][:], in0=acc2[:], in1=x1[b][:], op=ALU.add),
                             reads=[('acc2', b), ('x1', b)], writes=[('x1', b)])
                        key = ('fst', b)
                        P.op('act', lambda e, b=b: e.activation(out=junk[:], in_=x1[b][:], func=AF.Square, accum_out=stt[b][:, 4:5]),
                             reads=[('x1', b)], writes=['junk', (key, 'ss')])
                        rstd_ops(stt[b], 4, 5, 6, 1, 1.0 / D, key)
                        P.op('dve', lambda e, b=b: e.scalar_tensor_tensor(
                            out=ob[b][:], in0=x1[b][:], scalar=stt[b][:, 6:7], in1=fnr[:], op0=ALU.mult, op1=ALU.mult),
                            reads=[('x1', b), (key, 'r')], writes=[('ob', b)])
                        o0 = r0 - NCT * 128
                        P.dma('sp', lambda e, b=b, o0=o0: e.dma_start(out=out[o0:o0 + 128, :], in_=ob[b][:]),
                              reads=[('ob', b)], writes=[('out', t)])
                    P.end_phase()

        phase_w()
        upto = cfg.get('upto', 'D')
        for l in range(NL if upto != 'W' else 0):
            phase0(l)
            phaseA(l)
            if upto == 'A':
                break
            phaseB(l)
            if upto == 'B':
                break
            phaseC(l)
            if upto == 'C':
                break
            if SPARSE and l % 2 == 1:
                phaseD_sparse(l)
            else:
                phaseD(l)
    return nc


def _consts(T):
    NT = NCT + T // 128
    identf = np.eye(128, dtype=np.float32)
    s = np.arange(128)
    trif = (s[:, None] <= s[None, :]).astype(np.float32)
    trib = (s[:, None] >= s[None, :]).astype(np.float32)
    rows = T // 64
    row = np.repeat(np.arange(rows, dtype=np.float32), 64)
    col = np.tile(np.arange(64, dtype=np.float32), rows)
    inv = (10000.0 ** (-np.arange(16, dtype=np.float32) / 16)).astype(np.float32)
    ang = np.concatenate([row[:, None] * inv, col[:, None] * inv], axis=-1).astype(np.float32)
    rope = np.zeros((NT * 128, 64), np.float32)
    rope[:NCT * 128, 0:32] = 1.0
    rope[NCT * 128:, 0:32] = np.cos(ang)
    rope[NCT * 128:, 32:64] = np.sin(ang)
    return dict(identf=identf, identb=identf.astype(ml_dtypes.bfloat16), trif=trif, trib=trib,
                maskf=trif.astype(ml_dtypes.bfloat16), maskb=trib.astype(ml_dtypes.bfloat16),
                rope=rope.reshape(NT, 128, 64))


def make_in_maps(inputs, nb, T):
    c = _consts(T)
    f = lambda a: np.ascontiguousarray(np.asarray(a, dtype=np.float32))
    nwc = np.zeros((128, 32), np.float32)
    for l in range(2):
        for j, nm in enumerate(('norm1_w', 'norm2_w')):
            nwc[:, l * 16 + j * 8: l * 16 + j * 8 + 8] = f(inputs[nm])[l].reshape(8, 128).T
    CAP = T
    NJ = CAP // 512
    NG = (2 * T + NEXP * 511) // 512
    p = np.arange(128, dtype=np.float32)[:, None]
    mconst = np.concatenate([
        np.broadcast_to(np.arange(NEXP, dtype=np.float32)[None, :] * CAP, (128, NEXP)),
        np.broadcast_to(np.arange(NJ, dtype=np.float32)[None, :] * 512, (128, NJ)),
        np.broadcast_to(np.arange(NG, dtype=np.float32)[None, :], (128, NG)),
        np.arange(7, dtype=np.float32)[None, :] * 128 + p,
        np.arange(14, dtype=np.float32)[None, :] * 128 + p,
        np.arange(4, dtype=np.float32)[None, :] * 128 + p], axis=1).astype(np.float32)
    shared = dict(
        n2w=f(inputs['norm2_w']), mconst=np.ascontiguousarray(mconst),
        ada_w=f(inputs['ada_w']), ada_b=f(inputs['ada_b']), nwc=nwc, fnw=f(inputs['final_norm_w']).reshape(1, D),
        w_in=np.ascontiguousarray(f(inputs['w_in'])[:, :, _PERM]), gate_b=f(inputs['mlstm_gate_b']),
        mnw=f(inputs['mlstm_norm_w']), qnw=f(inputs['q_norm_w']), knw=f(inputs['k_norm_w']), w_out=f(inputs['w_out']),
        ffn_g=f(inputs['ffn_w_gate'])[0], ffn_u=f(inputs['ffn_w_up'])[0], ffn_d=f(inputs['ffn_w_down'])[0],
        router=f(inputs['moe_router'])[0], moe_g=f(inputs['moe_w_gate'])[0], moe_u=f(inputs['moe_w_up'])[0],
        moe_d=f(inputs['moe_w_down'])[0], **c)
    x = f(inputs['x'])
    ctx = f(inputs['ctx'])
    cc = f(inputs['c'])
    c_ctx = f(inputs['c_ctx'])
    maps = []
    for b in range(nb):
        cvec = np.concatenate([cc[b].reshape(8, 128).T, c_ctx.reshape(8, 128).T], axis=1)
        m = dict(shared)
        m['xin'] = np.ascontiguousarray(np.concatenate([ctx[b], x[b]], axis=0))
        m['cvec'] = np.ascontiguousarray(cvec)
        maps.append(m)
    return maps


def kernel(**inputs):
    x = np.asarray(inputs['x'])
    B, T, _ = x.shape
    nc = build(dict(T=T))
    maps = make_in_maps(inputs, B, T)
    res = run_bass_kernel_spmd(nc, maps, core_ids=list(range(B)))
    return np.stack([np.asarray(r['out'], dtype=np.float32) for r in res.results], axis=0)
```

```python
import numpy as np
import ml_dtypes
from contextlib import ExitStack
import concourse.bass as bass
import concourse.mybir as mybir
from concourse.bass_utils import run_bass_kernel_spmd

F32 = mybir.dt.float32
BF16 = mybir.dt.bfloat16
I32 = mybir.dt.int32
AF = mybir.ActivationFunctionType
ALU = mybir.AluOpType
AX = mybir.AxisListType

ENGS = ['pe', 'act', 'dve', 'pool', 'sp']
NRING = 8
ENG_ATTR = {'pe': 'tensor', 'act': 'scalar', 'dve': 'vector', 'pool': 'gpsimd', 'sp': 'sync'}


class Prog:
    def __init__(self, nc, stack):
        self.nc = nc
        self.cnt = {e: 0 for e in ENGS}
        self.ring = {e: [0, [0] * NRING] for e in ENGS}
        self.sems = {}
        for e in ENGS:
            self.sems[('c', e)] = stack.enter_context(nc.semaphore('c_' + e))
            if e in ('sp', 'pool', 'act'):
                for s in range(NRING):
                    self.sems[('d', e, s)] = stack.enter_context(nc.semaphore('d_%s_%d' % (e, s)))
        self.waited = {e: {} for e in ENGS}
        self.nphase = 0
        self._reset()

    def _reset(self):
        self.q = {e: [] for e in ENGS}
        self.lastw = {}
        self.readers = {}

    def _events(self, reads, writes):
        ev = []
        for k in reads:
            if k in self.lastw:
                ev.append(self.lastw[k])
        for k in writes:
            if k in self.lastw:
                ev.append(self.lastw[k])
            ev.extend(self.readers.get(k, ()))
        return ev

    def _waits(self, eng, evs):
        waits = []
        for (sk, v) in evs:
            if sk == ('c', 'pe') and eng == 'pe':
                continue
            if self.waited[eng].get(sk, 0) >= v:
                continue
            self.waited[eng][sk] = v
            waits.append((sk, v))
        return waits

    def _commit(self, me, reads, writes):
        for k in writes:
            self.lastw[k] = me
            self.readers[k] = []
        for k in reads:
            self.readers.setdefault(k, []).append(me)

    def op(self, eng, fn, reads=(), writes=(), banks=()):
        evs = self._events(reads, writes)
        for b in banks:
            lw = self.lastw.get(('bank', b))
            if lw is not None and lw[0] != ('c', eng):
                evs.append(lw)
        waits = self._waits(eng, evs)
        self.cnt[eng] += 1
        me = (('c', eng), self.cnt[eng])
        self.q[eng].append((waits, fn, me, 1))
        self._commit(me, reads, writes)
        for b in banks:
            self.lastw[('bank', b)] = me
        return me

    def dma(self, eng, fn, reads=(), writes=()):
        ring = self.ring[eng]
        slot = ring[0] % NRING
        ring[0] += 1
        sk = ('d', eng, slot)
        prev = ring[1][slot]
        evs = self._events(reads, writes)
        if prev > 0:
            evs.append((sk, prev))
        waits = self._waits(eng, evs)
        ring[1][slot] = prev + 16
        me = (sk, prev + 16)
        self.q[eng].append((waits, fn, me, 16))
        self._commit(me, reads, writes)
        return me

    def end_phase(self):
        evs = []
        for e in ENGS:
            r = self.ring[e]
            for s in range(NRING):
                if r[1][s] > 0:
                    evs.append((('d', e, s), r[1][s]))
            if e != 'sp' and self.cnt[e] > 0:
                evs.append((('c', e), self.cnt[e]))
        waits = self._waits('sp', evs)
        self.q['sp'].append((waits, None, None, 0))
        nc = self.nc
        sems = self.sems
        self.nphase += 1
        with nc.allow_low_precision(reason='bf16 matmul operands by design'), nc.Block('ph%d' % self.nphase) as block:
            def mk(e):
                def body(engine):
                    for (waits, fn, me, inc) in self.q[e]:
                        for (sk, v) in waits:
                            engine.wait_ge(sems[sk], v)
                        if fn is not None:
                            fn(engine).then_inc(sems[me[0]], inc)
                return body
            for e in ENGS:
                if self.q[e]:
                    getattr(block, ENG_ATTR[e])(mk(e))
        for e in ENGS:
            for e2 in ENGS:
                self.waited[e][('c', e2)] = self.cnt[e2]
                for s in range(NRING):
                    self.waited[e][('d', e2, s)] = self.ring[e2][1][s]
        self._reset()


D = 1024
DIN = 2320
NCT = 2
DFF = 2816
DFE = 3584
NEXP = 8
_ORIG = dict(mq=(0, 256), mk=(256, 512), mv=(512, 1024), mo=(1024, 1536), mg=(1536, 1552),
             aq=(1552, 2064), ak=(2064, 2192), av=(2192, 2320))
_PERM = np.concatenate([np.arange(*_ORIG[k]) for k in ('mq', 'mk', 'mv', 'mo', 'aq', 'ak', 'av', 'mg')])


def build(cfg):
    T = cfg['T']
    NL = cfg.get('layers', 2)
    dbg = cfg.get('debug', False)
    NLT = T // 128
    NT = NCT + NLT
    NTOK = NT * 128
    nc = bass.Bass("TRN2", target_bir_lowering=False)
    SCR = "ExternalOutput" if dbg else "Internal"

    def din(name, shape, dt=F32):
        return nc.dram_tensor(name, list(shape), dt, kind="ExternalInput").ap()

    def dscr(name, shape, dt):
        return nc.dram_tensor(name, list(shape), dt, kind=SCR).ap()

    xin = din("xin", [NTOK, D])
    cvec = din("cvec", [128, 16])
    ada_w = din("ada_w", [2, D, 6 * D])
    ada_b = din("ada_b", [2, 6 * D])
    nwc = din("nwc", [128, 32])
    fnw = din("fnw", [1, D])
    w_in = din("w_in", [2, D, DIN])
    gate_b = din("gate_b", [2, 16])
    mnw = din("mnw", [2, 512])
    qnw = din("qnw", [2, 64])
    knw = din("knw", [2, 64])
    w_out = din("w_out", [2, D, D])
    ffn_g = din("ffn_g", [D, DFF])
    ffn_u = din("ffn_u", [D, DFF])
    ffn_d = din("ffn_d", [DFF, D])
    router = din("router", [D, NEXP])
    moe_g = din("moe_g", [NEXP, D, DFE])
    moe_u = din("moe_u", [NEXP, D, DFE])
    moe_d = din("moe_d", [NEXP, DFE, D])
    identf_d = din("identf", [128, 128])
    identb_d = din("identb", [128, 128], BF16)
    trif_d = din("trif", [128, 128])
    trib_d = din("trib", [128, 128])
    maskf_d = din("maskf", [128, 128], BF16)
    maskb_d = din("maskb", [128, 128], BF16)
    rope_d = din("rope", [NT, 128, 64])
    out = nc.dram_tensor("out", [T, D], F32, kind="ExternalOutput").ap()
    SPARSE = cfg.get('sparse', True) and NL > 1
    CAP = T
    NJ = CAP // 512
    NG = (2 * T + NEXP * 511) // 512
    NMC = 8 + NJ + NG + 7 + 14 + 4
    n2w = din("n2w", [2, D])
    mconst_d = din("mconst", [128, NMC])
    if SPARSE:
        MGp = nc.dram_tensor("MGp", [NEXP * 7 * 128, 8 * 512], BF16, kind="Internal").ap()
        MUp = nc.dram_tensor("MUp", [NEXP * 7 * 128, 8 * 512], BF16, kind="Internal").ap()
        MDp = nc.dram_tensor("MDp", [NEXP * 2 * 7 * 128, 4 * 512], BF16, kind="Internal").ap()
        HS = nc.dram_tensor("HS", [NEXP * CAP, D], BF16, kind="Internal").ap()
        YS = nc.dram_tensor("YS", [NG * 512, D], F32, kind="Internal").ap()

    X = dscr("X", [NTOK, D], F32)
    QT = dscr("QT", [8, 64, NTOK], BF16)
    KT = dscr("KT", [2, 64, NTOK], BF16)
    VA = dscr("VA", [NTOK, 2, 65], BF16)
    MQ = [dscr("MQ%d" % d, [4, 64, NTOK], BF16) for d in range(2)]
    MK = [dscr("MK%d" % d, [4, 64, NTOK], BF16) for d in range(2)]
    MK2 = [dscr("MK2%d" % d, [NTOK, 4, 64], BF16) for d in range(2)]
    MVA = dscr("MVA", [NTOK, 4, 129], BF16)
    MO = dscr("MO", [NTOK, 512], BF16)
    DEC = dscr("DEC", [NT, 64, 8], F32)
    HB = dscr("HB", [NTOK, 512], F32)
    Y = dscr("Y", [NTOK, D], BF16)
    WINb = nc.dram_tensor("WINb", [2, D, DIN], BF16, kind=SCR).ap()
    dGS = dscr("dGS", [2, 128, 32], F32)
    dGATE = dscr("dGATE", [4, 128, D], F32)
    dhT = dscr("dhT", [128, 8, 128], BF16)
    WOb = nc.dram_tensor("WOb", [2, D, D], BF16, kind="Internal").ap()
    FGb = nc.dram_tensor("FGb", [D, DFF], BF16, kind="Internal").ap()
    FUb = nc.dram_tensor("FUb", [D, DFF], BF16, kind="Internal").ap()
    FDb = nc.dram_tensor("FDb", [DFF, D], BF16, kind="Internal").ap()
    MGb = nc.dram_tensor("MGb", [NEXP, D, DFE], BF16, kind="Internal").ap()
    MUb = nc.dram_tensor("MUb", [NEXP, D, DFE], BF16, kind="Internal").ap()
    MDb = nc.dram_tensor("MDb", [NEXP, DFE, D], BF16, kind="Internal").ap()

    with ExitStack() as gs:
        P = Prog(nc, gs)

        uid = [0]

        def sbt(st, name, shape, dt=F32):
            uid[0] += 1
            return st.enter_context(nc.sbuf_tensor("s%d_%s" % (uid[0], name), list(shape), dt))

        banks = [gs.enter_context(nc.psum_tensor("bank%d" % i, [128, 512], F32)) for i in range(8)]

        def bk(i):
            return banks[i][:, :]

        def bkb(i):
            return banks[i][:, :].bitcast(BF16)

        identf = sbt(gs, "identf", [128, 128])
        identb = sbt(gs, "identb", [128, 128], BF16)
        trif = sbt(gs, "trif", [128, 128])
        trib = sbt(gs, "trib", [128, 128])
        onesf = sbt(gs, "onesf", [128, 128])
        maskf = sbt(gs, "maskf", [128, 128], BF16)
        maskb = sbt(gs, "maskb", [128, 128], BF16)
        cv = sbt(gs, "cv", [128, 16])
        scv = sbt(gs, "scv", [128, 48])
        nwt = sbt(gs, "nwt", [128, 32])
        GATE = [[sbt(gs, "GATE%d_%d" % (w, m), [128, D]) for m in range(2)] for w in range(2)]
        COLS = [sbt(gs, "COLS%d" % w, [128, 32]) for w in range(2)]
        GS = [sbt(gs, "GS%d" % w, [128, 32]) for w in range(2)]
        N2R = sbt(gs, "N2R", [128, D])
        G2R = sbt(gs, "G2R", [128, D])
        S2R = sbt(gs, "S2R", [128, D])
        mconst = sbt(gs, "mconst", [128, NMC])

        def cast_dram(dst, src, tag):
            n = 1
            for s in src.shape:
                n *= s
            letters = "abcd"[:len(src.shape)]
            pat = " ".join(letters) + " -> (" + " ".join(letters) + ")"
            s1 = src.rearrange(pat).rearrange("(n m) -> n m", m=1024)
            d1 = dst.rearrange(pat).rearrange("(n m) -> n m", m=1024)
            rows = n // 1024
            step = 2048
            for r0 in range(0, rows, step):
                r1 = min(rows, r0 + step)
                P.dma('pool', lambda e, r0=r0, r1=r1: e.dma_start(out=d1[r0:r1, :], in_=s1[r0:r1, :]),
                      writes=[(tag, r0)])

        def phase_w():
            for (t_, d_) in ((identf, identf_d), (identb, identb_d), (trif, trif_d), (trib, trib_d),
                             (maskf, maskf_d), (maskb, maskb_d), (cv, cvec), (nwt, nwc)):
                P.dma('sp', lambda e, t_=t_, d_=d_: e.dma_start(out=t_[:], in_=d_[:, :]), writes=[t_.name])
            P.op('pool', lambda e: e.memset(onesf[:], 1.0), writes=['onesf'])
            P.op('act', lambda e: e.activation(out=scv[:, 0:16], in_=cv[:], func=AF.Exp, scale=-1.0),
                 reads=[cv.name], writes=['scv0'])
            P.op('dve', lambda e: e.tensor_scalar(out=scv[:, 16:32], in0=scv[:, 0:16], scalar1=1.0, scalar2=None,
                                                  op0=ALU.add), reads=['scv0'], writes=['scv1'])
            P.op('dve', lambda e: e.reciprocal(out=scv[:, 0:16], in_=scv[:, 16:32]), reads=['scv1'], writes=['scv0'])
            P.op('dve', lambda e: e.tensor_tensor(out=scv[:, 32:48], in0=scv[:, 0:16], in1=cv[:], op=ALU.mult),
                 reads=['scv0', cv.name], writes=['scv2'])
            cast_dram(WINb, w_in, 'WINb')
            cast_dram(WOb, w_out, 'WOb')
            cast_dram(FGb, ffn_g, 'FGb')
            cast_dram(FUb, ffn_u, 'FUb')
            cast_dram(FDb, ffn_d, 'FDb')
            P.dma('sp', lambda e: e.dma_start(out=mconst[:], in_=mconst_d[:, :]), writes=['mconst'])
            if NL > 1 and not SPARSE:
                cast_dram(MGb, moe_g, 'MGb')
                cast_dram(MUb, moe_u, 'MUb')
                cast_dram(MDb, moe_d, 'MDb')
            P.end_phase()

        def prep_moe_weights():
            for e_ in range(NEXP):
                for c in range(7):
                    r0 = (e_ * 7 + c) * 128
                    for (dst, src, tag) in ((MGp, moe_g, 'MGp'), (MUp, moe_u, 'MUp')):
                        P.dma('pool', lambda e, dst=dst, src=src, r0=r0, e_=e_, c=c: e.dma_start(
                            out=dst[r0:r0 + 128, :].rearrange("p (k f) -> p k f", k=8),
                            in_=src[e_][:, c * 512:(c + 1) * 512].rearrange("(k p) f -> p k f", p=128)),
                            writes=[(tag, e_, c)])
                for half in range(2):
                    for q in range(7):
                        r0 = ((e_ * 2 + half) * 7 + q) * 128
                        P.dma('pool', lambda e, r0=r0, e_=e_, half=half, q=q: e.dma_start(
                            out=MDp[r0:r0 + 128, :].rearrange("p (c d) -> p c d", c=4),
                            in_=moe_d[e_][q * 512:(q + 1) * 512, half * 512:(half + 1) * 512].rearrange("(c p) d -> p c d", p=128)),
                            writes=[('MDp', e_, half, q)])

        def phase0(l):
            with ExitStack() as st:
                awb = [sbt(st, "awb%d" % i, [128, 8, 512]) for i in range(2)]
                abb = [sbt(st, "abb%d" % i, [128, 512]) for i in range(2)]
                row = [sbt(st, "row%d" % i, [128, 512]) for i in range(2)]
                SL = [sbt(st, "SL%d" % j, [128, 8, 128]) for j in range(2)]
                for j in range(2):
                    for k in range(8):
                        P.op('dve' if k % 2 else 'pool',
                             lambda e, j=j, k=k: e.tensor_copy(out=SL[j][:, k, :],
                                                               in_=scv[:, 32 + j * 8 + k:33 + j * 8 + k].to_broadcast([128, 128])),
                             writes=[('SL', j, k)])
                P.dma('sp', lambda e: e.dma_start(out=N2R[:], in_=n2w[l:l + 1, :].partition_broadcast(128)), writes=['N2R'])
                it = 0
                for grp in range(12):
                    m, half = grp // 2, grp % 2
                    b = grp % 2
                    P.dma('sp', lambda e, b=b, grp=grp: e.dma_start(
                        out=awb[b][:], in_=ada_w[l, :, grp * 512:(grp + 1) * 512].rearrange("(k p) n -> p k n", p=128)),
                        writes=[('awb', b)])
                    P.dma('sp', lambda e, b=b, grp=grp: e.dma_start(
                        out=abb[b][:], in_=ada_b[l:l + 1, grp * 512:(grp + 1) * 512].partition_broadcast(128)),
                        writes=[('abb', b)])
                    for w in range(2):
                        bm = (it % 2) * 2
                        rb = it % 2
                        it += 1
                        for k in range(8):
                            P.op('pe', lambda e, k=k, w=w, b=b, bm=bm: e.matmul(
                                bk(bm), lhsT=SL[w][:, k, :], rhs=awb[b][:, k, :], start=(k == 0), stop=(k == 7)),
                                reads=[('SL', w, k), ('awb', b)], writes=[('pm', bm)], banks=[bm])
                        if m in (2, 5):
                            gt_ = GATE[w][0 if m == 2 else 1]
                            P.op('dve', lambda e, gt_=gt_, half=half, bm=bm, b=b: e.tensor_tensor(
                                out=gt_[:, half * 512:(half + 1) * 512], in0=bk(bm), in1=abb[b][:], op=ALU.add),
                                reads=[('pm', bm), ('abb', b)], writes=[(gt_.name, half)], banks=[bm])
                        else:
                            P.op('dve', lambda e, rb=rb, bm=bm, b=b: e.tensor_tensor(
                                out=row[rb][:], in0=bk(bm), in1=abb[b][:], op=ALU.add),
                                reads=[('pm', bm), ('abb', b)], writes=[('row', rb)], banks=[bm])
                            if w == 0 and m == 3:
                                P.op('pool', lambda e, rb=rb, half=half: e.tensor_copy(
                                    out=S2R[:, half * 512:(half + 1) * 512], in_=row[rb][:]),
                                    reads=[('row', rb)], writes=[('S2R', half)])
                            if w == 0 and m == 4:
                                P.op('dve', lambda e, rb=rb, half=half: e.scalar_tensor_tensor(
                                    out=G2R[:, half * 512:(half + 1) * 512], in0=row[rb][:], scalar=1.0,
                                    in1=N2R[:, half * 512:(half + 1) * 512], op0=ALU.add, op1=ALU.mult),
                                    reads=[('row', rb), 'N2R'], writes=[('G2R', half)])
                            for blk in range(4):
                                P.op('pe', lambda e, rb=rb, bm=bm, blk=blk: e.transpose(
                                    out=bk(bm + 1)[:, blk * 128:(blk + 1) * 128], in_=row[rb][:, blk * 128:(blk + 1) * 128],
                                    identity=identf[:]),
                                    reads=[('row', rb), 'identf'], writes=[('pt', bm + 1)], banks=[bm + 1])
                            ci = {0: 0, 1: 1, 3: 2, 4: 3}[m]
                            P.op('dve', lambda e, w=w, ci=ci, half=half, bm=bm: e.tensor_copy(
                                out=COLS[w][:, ci * 8 + half * 4: ci * 8 + half * 4 + 4],
                                in_=bk(bm + 1).rearrange("p (a b) -> p a b", a=4)[:, :, 0]),
                                reads=[('pt', bm + 1)], writes=[('COLS', w, ci, half)], banks=[bm + 1])
                for w in range(2):
                    rd = [('COLS', w, ci, h) for ci in range(4) for h in range(2)]
                    for j in range(2):
                        nsl = nwt[:, l * 16 + j * 8: l * 16 + j * 8 + 8]
                        P.op('dve', lambda e, w=w, j=j, nsl=nsl: e.scalar_tensor_tensor(
                            out=GS[w][:, j * 16: j * 16 + 8], in0=COLS[w][:, j * 16 + 8: j * 16 + 16], scalar=1.0,
                            in1=nsl, op0=ALU.add, op1=ALU.mult),
                            reads=rd + ['nwt'], writes=[('GS', w, j, 0)])
                        P.op('dve', lambda e, w=w, j=j: e.tensor_copy(
                            out=GS[w][:, j * 16 + 8: j * 16 + 16], in_=COLS[w][:, j * 16: j * 16 + 8]),
                            reads=rd, writes=[('GS', w, j, 1)])
                if dbg:
                    for w in range(2):
                        P.dma('sp', lambda e, w=w: e.dma_start(out=dGS[w], in_=GS[w][:]), reads=[('GS', w, j, i) for j in range(2) for i in range(2)], writes=[('dGS', w)])
                        for m in range(2):
                            P.dma('sp', lambda e, w=w, m=m: e.dma_start(out=dGATE[w * 2 + m], in_=GATE[w][m][:]), reads=[(GATE[w][m].name, hh) for hh in range(2)], writes=[('dGATE', w, m)])
                P.end_phase()

        def rstd_ops(stt, c_in, c_tmp, c_out, n, inv_n, key):
            P.op('dve', lambda e: e.tensor_scalar(out=stt[:, c_tmp:c_tmp + n], in0=stt[:, c_in:c_in + n],
                                                  scalar1=inv_n, scalar2=1e-6, op0=ALU.mult, op1=ALU.add),
                 reads=[(key, 'ss')], writes=[(key, 'v')])
            P.op('act', lambda e: e.activation(out=stt[:, c_in:c_in + n], in_=stt[:, c_tmp:c_tmp + n], func=AF.Ln),
                 reads=[(key, 'v')], writes=[(key, 'ln')])
            P.op('act', lambda e: e.activation(out=stt[:, c_out:c_out + n], in_=stt[:, c_in:c_in + n], func=AF.Exp,
                                               scale=-0.5),
                 reads=[(key, 'ln')], writes=[(key, 'r')])

        def norm_to_hT(st_tiles, xsrc_ap, xkey, w, j, hT_dst_fn, hkey_fn, bA, bB, tb, hF_dst_fn=None):
            junk, stt, xn = st_tiles
            key = ('nst', tb)
            P.op('act', lambda e: e.activation(out=junk[:], in_=xsrc_ap, func=AF.Square, accum_out=stt[:, 0:1]),
                 reads=[xkey], writes=['junk', (key, 'ss')])
            rstd_ops(stt, 0, 1, 2, 1, 1.0 / D, key)
            P.op('dve', lambda e: e.tensor_scalar(out=xn[:], in0=xsrc_ap, scalar1=stt[:, 2:3], scalar2=None,
                                                  op0=ALU.mult), reads=[xkey, (key, 'r')], writes=['xn'])
            for k in range(8):
                bb = bA if k < 4 else bB
                P.op('pe', lambda e, k=k, bb=bb: e.transpose(out=bk(bb)[:, (k % 4) * 128:(k % 4 + 1) * 128],
                                                            in_=xn[:, k * 128:(k + 1) * 128], identity=identf[:]),
                     reads=['xn', 'identf'], writes=[('pT', bb, k % 4)], banks=[bb])
            for k in range(8):
                bb = bA if k < 4 else bB
                src = bk(bb)[:, (k % 4) * 128:(k % 4 + 1) * 128]
                gcol = GS[w][:, j * 16 + k: j * 16 + k + 1]
                scol = GS[w][:, j * 16 + 8 + k: j * 16 + 9 + k]
                if hF_dst_fn is not None:
                    P.op('dve', lambda e, k=k, src=src, gcol=gcol, scol=scol: e.tensor_scalar(
                        out=hF_dst_fn(k), in0=src, scalar1=gcol, scalar2=scol, op0=ALU.mult, op1=ALU.add),
                        reads=[('pT', bb, k % 4), ('GS', w, j, 0), ('GS', w, j, 1)], writes=[('hF', k)], banks=[bb])
                    P.op('act', lambda e, k=k: e.copy(out=hT_dst_fn(k), in_=hF_dst_fn(k)),
                         reads=[('hF', k)], writes=[hkey_fn(k)])
                else:
                    P.op('dve', lambda e, k=k, src=src, gcol=gcol, scol=scol: e.tensor_scalar(
                        out=hT_dst_fn(k), in0=src, scalar1=gcol, scalar2=scol, op0=ALU.mult, op1=ALU.add),
                        reads=[('pT', bb, k % 4), ('GS', w, j, 0), ('GS', w, j, 1)], writes=[hkey_fn(k)], banks=[bb])

        def phaseA(l):
            with ExitStack() as st:
                win = sbt(st, "win", [128, 8, DIN], BF16)
                gbr = sbt(st, "gbr", [128, 16])
                qnr = sbt(st, "qnr", [128, 64])
                knr = sbt(st, "knr", [128, 64])
                xt = [sbt(st, "xt%d" % i, [128, D]) for i in range(2)]
                rp = [sbt(st, "rp%d" % i, [128, 64]) for i in range(2)]
                junk = sbt(st, "junk", [128, D], BF16)
                stt = [sbt(st, "stt%d" % i, [128, 8]) for i in range(2)]
                xn = sbt(st, "xn", [128, D])
                hT = [sbt(st, "hT%d" % i, [128, 8, 128], BF16) for i in range(2)]
                gt = sbt(st, "gt", [128, 16])
                e8 = sbt(st, "e8", [128, 8])
                L8 = sbt(st, "L8", [128, 8])
                cs = sbt(st, "cs", [128, 24])
                sc = sbt(st, "sc", [128, 24])
                tmp8 = sbt(st, "tmp8", [128, 16])
                dec = sbt(st, "dec", [128, 8])
                pqk = sbt(st, "pqk", [128, 512])
                sq6 = [sbt(st, "sq6_%d" % i, [128, 4, 64], BF16) for i in range(6)]
                trs = sbt(st, "trs", [64, 16, 128], BF16)
                vaug = [sbt(st, "vaug%d" % i, [128, 4, 129], BF16) for i in range(2)]
                mot = sbt(st, "mot", [128, 512])
                mob = sbt(st, "mob", [128, 512], BF16)
                sqq = sbt(st, "sqq", [128, 640])
                aqs = sbt(st, "aqs", [128, 24])
                qn = sbt(st, "qn", [128, 640])
                qn2 = sbt(st, "qn2", [128, 640])
                rt = [sbt(st, "rt%d" % i, [128, 10, 32]) for i in range(4)]
                qr = sbt(st, "qr", [128, 640], BF16)
                qT = sbt(st, "qT", [64, 10, 128], BF16)
                vat = [sbt(st, "vat%d" % i, [128, 2, 65], BF16) for i in range(2)]

                P.dma('sp', lambda e: e.dma_start(out=win[:], in_=WINb[l].rearrange("(k p) n -> p k n", p=128)),
                      writes=['win'])
                P.dma('sp', lambda e: e.dma_start(out=gbr[:], in_=gate_b[l:l + 1, :].partition_broadcast(128)),
                      writes=['gbr'])
                P.dma('sp', lambda e: e.dma_start(out=qnr[:], in_=qnw[l:l + 1, :].partition_broadcast(128)),
                      writes=['qnr0'])
                P.dma('sp', lambda e: e.dma_start(out=knr[:], in_=knw[l:l + 1, :].partition_broadcast(128)),
                      writes=['knr'])
                P.op('dve', lambda e: e.tensor_scalar(out=qnr[:], in0=qnr[:], scalar1=0.125, scalar2=None, op0=ALU.mult),
                     reads=['qnr0'], writes=['qnr'])
                for i in range(2):
                    P.op('pool', lambda e, i=i: e.memset(vaug[i][:], 1.0), writes=[('vaug', i)])
                    P.op('pool', lambda e, i=i: e.memset(vat[i][:], 1.0), writes=[('vat', i)])
                xsrc = xin if l == 0 else X

                def frontA(t):
                    tb = t % 2
                    w = 1 if t < NCT else 0
                    r0 = t * 128
                    P.dma('sp', lambda e, tb=tb, r0=r0: e.dma_start(out=xt[tb][:], in_=xsrc[r0:r0 + 128, :]),
                          writes=[('xt', tb)])
                    P.dma('sp', lambda e, tb=tb, t=t: e.dma_start(out=rp[tb][:], in_=rope_d[t]), writes=[('rp', tb)])
                    norm_to_hT((junk, stt[tb], xn), xt[tb][:], ('xt', tb), w, 0,
                               lambda k, tb=tb: hT[tb][:, k, :], lambda k, tb=tb: ('hT', tb, k), 0, 1, tb)
                    if dbg and t == 2:
                        P.dma('sp', lambda e, tb=tb: e.dma_start(out=dhT, in_=hT[tb][:]), reads=[('hT', tb, k) for k in range(8)], writes=['dhT'])

                def mmA(t):
                    tb = t % 2
                    gcols = [(0, 512), (512, 1024), (1024, 1536), (1536, 2048), (2048, 2320)]
                    for g, (c0, c1) in enumerate(gcols):
                        for k in range(8):
                            P.op('pe', lambda e, g=g, c0=c0, c1=c1, k=k, tb=tb: e.matmul(
                                bk(2 + g)[:, 0:c1 - c0], lhsT=hT[tb][:, k, :], rhs=win[:, k, c0:c1],
                                start=(k == 0), stop=(k == 7)),
                                reads=[('hT', tb, k), 'win'], writes=[('pp', g)], banks=[2 + g])

                def postA(t):
                    tb = t % 2
                    r0 = t * 128
                    P.op('dve', lambda e: e.tensor_tensor(out=gt[:], in0=bk(6)[:, 256:272], in1=gbr[:], op=ALU.add),
                         reads=[('pp', 4), 'gbr'], writes=['gt'], banks=[6])
                    P.op('act', lambda e: e.activation(out=e8[:, 0:4], in_=gt[:, 4:8], func=AF.Exp, scale=-1.0),
                         reads=['gt'], writes=['e8a'])
                    P.op('act', lambda e: e.activation(out=e8[:, 4:8], in_=gt[:, 12:16], func=AF.Exp, scale=-1.0),
                         reads=['gt'], writes=['e8b'])
                    P.op('dve', lambda e: e.tensor_scalar(out=e8[:], in0=e8[:], scalar1=1.0, scalar2=None, op0=ALU.add),
                         reads=['e8a', 'e8b'], writes=['e8'])
                    P.op('act', lambda e: e.activation(out=L8[:], in_=e8[:], func=AF.Ln), reads=['e8'], writes=['L8'])
                    for i, tri in enumerate((trif, trib, onesf)):
                        P.op('pe', lambda e, i=i, tri=tri: e.matmul(bk(7)[:, i * 8:(i + 1) * 8], lhsT=tri[:], rhs=L8[:],
                                                                    start=True, stop=True),
                             reads=['L8', tri.name], writes=[('pg', i)], banks=[7])
                    P.op('dve', lambda e: e.tensor_copy(out=cs[:], in_=bk(7)[:, 0:24]),
                         reads=[('pg', 0), ('pg', 1), ('pg', 2)], writes=['cs'], banks=[7])
                    P.op('act', lambda e: e.activation(out=sc[:, 0:4], in_=cs[:, 0:4], func=AF.Exp, scale=-1.0),
                         reads=['cs'], writes=['sc0'])
                    P.op('act', lambda e: e.activation(out=sc[:, 4:8], in_=cs[:, 12:16], func=AF.Exp, scale=-1.0),
                         reads=['cs'], writes=['sc1'])
                    P.op('dve', lambda e: e.tensor_scalar(out=sc[:, 0:8], in0=sc[:, 0:8], scalar1=0.125, scalar2=None,
                                                          op0=ALU.mult), reads=['sc0', 'sc1'], writes=['scq'])
                    P.op('dve', lambda e: e.tensor_tensor(out=tmp8[:, 0:4], in0=gt[:, 0:4], in1=cs[:, 0:4], op=ALU.add),
                         reads=['gt', 'cs'], writes=['t8a'])
                    P.op('dve', lambda e: e.tensor_tensor(out=tmp8[:, 4:8], in0=gt[:, 8:12], in1=cs[:, 12:16], op=ALU.add),
                         reads=['gt', 'cs'], writes=['t8b'])
                    P.op('act', lambda e: e.activation(out=sc[:, 8:16], in_=tmp8[:, 0:8], func=AF.Exp),
                         reads=['t8a', 't8b'], writes=['sck'])
                    P.op('dve', lambda e: e.tensor_tensor(out=tmp8[:, 8:16], in0=tmp8[:, 0:8], in1=cs[:, 16:24],
                                                          op=ALU.subtract), reads=['t8a', 't8b', 'cs'], writes=['t8c'])
                    P.op('act', lambda e: e.activation(out=sc[:, 16:24], in_=tmp8[:, 8:16], func=AF.Exp),
                         reads=['t8c'], writes=['sckk'])
                    P.op('act', lambda e: e.activation(out=dec[:], in_=cs[:, 16:24], func=AF.Exp, scale=-1.0),
                         reads=['cs'], writes=['dec'])
                    P.dma('sp', lambda e, t=t: e.dma_start(out=DEC[t], in_=dec[0:64, :]), reads=['dec'], writes=[('DEC', t)])
                    P.op('act', lambda e: e.copy(out=pqk[:], in_=bk(2)), reads=[('pp', 0)], writes=['pqk'], banks=[2])
                    specs = [(0, 0, 'scq'), (0, 4, 'scq'), (256, 8, 'sck'), (256, 12, 'sck'), (256, 16, 'sckk'), (256, 20, 'sckk')]
                    for i, (c0, s0, skey) in enumerate(specs):
                        P.op('dve' if i % 2 == 0 else 'pool', lambda e, i=i, c0=c0, s0=s0: e.tensor_tensor(
                            out=sq6[i][:], in0=pqk[:, c0:c0 + 256].rearrange("p (h d) -> p h d", h=4),
                            in1=sc[:, s0:s0 + 4].unsqueeze(2).to_broadcast([128, 4, 64]), op=ALU.mult),
                            reads=['pqk', skey], writes=[('sq6', i)])
                    for i in range(4):
                        for h in range(4):
                            j = i * 4 + h
                            bb = 0 if j < 8 else 1
                            P.op('pe', lambda e, i=i, h=h, j=j, bb=bb: e.transpose(
                                out=bkb(bb)[0:64, (j % 8) * 128:(j % 8 + 1) * 128], in_=sq6[i][:, h, :], identity=identb[:]),
                                reads=[('sq6', i), 'identb'], writes=[('ptr', bb)], banks=[bb])
                    for bb in range(2):
                        P.op('act' if bb else 'dve', lambda e, bb=bb: e.tensor_copy(
                            out=trs[:, bb * 8:(bb + 1) * 8, :], in_=bkb(bb)[0:64, :].rearrange("p (a b) -> p a b", a=8))
                            if bb == 0 else e.copy(
                            out=trs[:, bb * 8:(bb + 1) * 8, :], in_=bkb(bb)[0:64, :].rearrange("p (a b) -> p a b", a=8)),
                            reads=[('ptr', bb)], writes=[('trs', bb)], banks=[bb])
                    for i, dst in enumerate((MQ[0], MQ[1], MK[0], MK[1])):
                        P.dma('sp', lambda e, i=i, dst=dst, r0=r0: e.dma_start(
                            out=dst[:, :, r0:r0 + 128].rearrange("h d n -> d h n"), in_=trs[:, i * 4:(i + 1) * 4, :]),
                            reads=[('trs', i // 2)], writes=[('MQK', i, t)])
                    for d_ in range(2):
                        P.dma('sp', lambda e, d_=d_, r0=r0: e.dma_start(out=MK2[d_][r0:r0 + 128], in_=sq6[4 + d_][:]),
                              reads=[('sq6', 4 + d_)], writes=[('MK2', d_, t)])
                    P.op('act', lambda e, tb=tb: e.copy(out=vaug[tb][:, :, 0:128],
                                                        in_=bk(3).rearrange("p (h d) -> p h d", h=4)),
                         reads=[('pp', 1)], writes=[('vaug', tb)], banks=[3])
                    P.dma('sp', lambda e, tb=tb, r0=r0: e.dma_start(out=MVA[r0:r0 + 128], in_=vaug[tb][:]),
                          reads=[('vaug', tb)], writes=[('MVA', t)])
                    P.op('act', lambda e: e.activation(out=mot[:], in_=bk(4), func=AF.Exp, scale=-1.0),
                         reads=[('pp', 2)], writes=['mot'], banks=[4])
                    P.op('act', lambda e: e.activation(out=mot[:], in_=mot[:], func=AF.Ln, bias=onesf[:, 0:1], scale=1.0),
                         reads=['mot', 'onesf'], writes=['mot'])
                    P.op('act', lambda e: e.activation(out=mob[:], in_=mot[:], func=AF.Exp, scale=-1.0), reads=['mot'], writes=['mob'])
                    P.dma('sp', lambda e, r0=r0: e.dma_start(out=MO[r0:r0 + 128, :], in_=mob[:]), reads=['mob'],
                          writes=[('MO', t)])
                    P.op('act', lambda e: e.activation(out=sqq[:, 0:512], in_=bk(5), func=AF.Square),
                         reads=[('pp', 3)], writes=['sqq_q'], banks=[5])
                    P.op('act', lambda e: e.activation(out=sqq[:, 512:640], in_=bk(6)[:, 0:128], func=AF.Square),
                         reads=[('pp', 4)], writes=['sqq_k'], banks=[6])
                    P.op('dve', lambda e: e.tensor_reduce(out=aqs[:, 0:10], in_=sqq[:].rearrange("p (h d) -> p h d", d=64),
                                                          axis=AX.X, op=ALU.add),
                         reads=['sqq_q', 'sqq_k'], writes=[('aq', 'ss')])
                    rstd_ops(aqs, 0, 10, 10, 10, 1.0 / 64, 'aq')
                    P.op('dve', lambda e: e.tensor_tensor(
                        out=qn[:, 0:512].rearrange("p (h d) -> p h d", d=64), in0=bk(5).rearrange("p (h d) -> p h d", d=64),
                        in1=aqs[:, 10:18].unsqueeze(2).to_broadcast([128, 8, 64]), op=ALU.mult),
                        reads=[('pp', 3), ('aq', 'r')], writes=['qn_q'], banks=[5])
                    P.op('dve', lambda e: e.tensor_tensor(
                        out=qn[:, 512:640].rearrange("p (h d) -> p h d", d=64),
                        in0=bk(6)[:, 0:128].rearrange("p (h d) -> p h d", d=64),
                        in1=aqs[:, 18:20].unsqueeze(2).to_broadcast([128, 2, 64]), op=ALU.mult),
                        reads=[('pp', 4), ('aq', 'r')], writes=['qn_k'], banks=[6])
                    P.op('pool', lambda e: e.tensor_tensor(
                        out=qn2[:, 0:512].rearrange("p (h d) -> p h d", d=64), in0=qn[:, 0:512].rearrange("p (h d) -> p h d", d=64),
                        in1=qnr[:].unsqueeze(1).to_broadcast([128, 8, 64]), op=ALU.mult),
                        reads=['qn_q', 'qnr'], writes=['qn2_q'])
                    P.op('pool', lambda e: e.tensor_tensor(
                        out=qn2[:, 512:640].rearrange("p (h d) -> p h d", d=64),
                        in0=qn[:, 512:640].rearrange("p (h d) -> p h d", d=64),
                        in1=knr[:].unsqueeze(1).to_broadcast([128, 2, 64]), op=ALU.mult),
                        reads=['qn_k', 'knr'], writes=['qn2_k'])
                    q4 = qn2[:].rearrange("p (h j two) -> p h j two", j=32, two=2)
                    qe, qo = q4[:, :, :, 0], q4[:, :, :, 1]
                    cosb = rp[tb][:, 0:32].unsqueeze(1).to_broadcast([128, 10, 32])
                    sinb = rp[tb][:, 32:64].unsqueeze(1).to_broadcast([128, 10, 32])
                    rd = ['qn2_q', 'qn2_k', ('rp', tb)]
                    P.op('dve', lambda e, cosb=cosb: e.tensor_tensor(out=rt[0][:], in0=qe, in1=cosb, op=ALU.mult), reads=rd, writes=['rt0'])
                    P.op('pool', lambda e, sinb=sinb: e.tensor_tensor(out=rt[1][:], in0=qo, in1=sinb, op=ALU.mult), reads=rd, writes=['rt1'])
                    P.op('dve', lambda e, sinb=sinb: e.tensor_tensor(out=rt[2][:], in0=qe, in1=sinb, op=ALU.mult), reads=rd, writes=['rt2'])
                    P.op('pool', lambda e, cosb=cosb: e.tensor_tensor(out=rt[3][:], in0=qo, in1=cosb, op=ALU.mult), reads=rd, writes=['rt3'])
                    r4 = qr[:].rearrange("p (h j two) -> p h j two", j=32, two=2)
                    P.op('dve', lambda e: e.tensor_tensor(out=r4[:, :, :, 0], in0=rt[0][:], in1=rt[1][:], op=ALU.subtract),
                         reads=['rt0', 'rt1'], writes=['qr_e'])
                    P.op('pool', lambda e: e.tensor_tensor(out=r4[:, :, :, 1], in0=rt[2][:], in1=rt[3][:], op=ALU.add),
                         reads=['rt2', 'rt3'], writes=['qr_o'])
                    for h in range(10):
                        bb = 0 if h < 8 else 1
                        P.op('pe', lambda e, h=h, bb=bb: e.transpose(
                            out=bkb(bb)[0:64, (h % 8) * 128:(h % 8 + 1) * 128], in_=qr[:, h * 64:(h + 1) * 64], identity=identb[:]),
                            reads=['qr_e', 'qr_o', 'identb'], writes=[('ptq', bb)], banks=[bb])
                    P.op('dve', lambda e: e.tensor_copy(out=qT[:, 0:8, :], in_=bkb(0)[0:64, :].rearrange("p (a b) -> p a b", a=8)),
                         reads=[('ptq', 0)], writes=['qT_q'], banks=[0])
                    P.op('act', lambda e: e.copy(out=qT[:, 8:10, :], in_=bkb(1)[0:64, 0:256].rearrange("p (a b) -> p a b", a=2)),
                         reads=[('ptq', 1)], writes=['qT_k'], banks=[1])
                    P.dma('sp', lambda e, r0=r0: e.dma_start(out=QT[:, :, r0:r0 + 128].rearrange("h d n -> d h n"),
                                                             in_=qT[:, 0:8, :]), reads=['qT_q'], writes=[('QT', t)])
                    P.dma('sp', lambda e, r0=r0: e.dma_start(out=KT[:, :, r0:r0 + 128].rearrange("h d n -> d h n"),
                                                             in_=qT[:, 8:10, :]), reads=['qT_k'], writes=[('KT', t)])
                    P.op('act', lambda e, tb=tb: e.copy(out=vat[tb][:, :, 0:64],
                                                        in_=bk(6)[:, 128:256].rearrange("p (h d) -> p h d", h=2)),
                         reads=[('pp', 4)], writes=[('vat', tb)], banks=[6])
                    P.dma('sp', lambda e, tb=tb, r0=r0: e.dma_start(out=VA[r0:r0 + 128], in_=vat[tb][:]),
                          reads=[('vat', tb)], writes=[('VA', t)])

                frontA(0)
                for t in range(NT):
                    mmA(t)
                    if t + 1 < NT:
                        frontA(t + 1)
                    postA(t)
                P.end_phase()

        def phaseB(l):
            need_ctx = (l < NL - 1)
            with ExitStack() as st:
                Cst = sbt(st, "Cst", [64, 4, 129])
                Cbf = sbt(st, "Cbf", [64, 4, 129], BF16)
                mnr = sbt(st, "mnr", [128, 512])
                qTt = [sbt(st, "bqT%d" % i, [64, 4, 128], BF16) for i in range(2)]
                kTt = [sbt(st, "bkT%d" % i, [64, 4, 128], BF16) for i in range(2)]
                k2t = [sbt(st, "bk2%d" % i, [128, 4, 64], BF16) for i in range(2)]
                vat = [sbt(st, "bva%d" % i, [128, 4, 129], BF16) for i in range(2)]
                dct = [sbt(st, "bdc%d" % i, [64, 8]) for i in range(2)]
                SM = sbt(st, "SM", [128, 4, 128], BF16)
                r4 = sbt(st, "r4", [128, 8])
                hd = [sbt(st, "hd%d" % i, [128, 512]) for i in range(2)]
                hbt = sbt(st, "hbt", [128, 512])
                hs = sbt(st, "hs", [128, 512])
                sqh = sbt(st, "sqh", [128, 512])
                hst = sbt(st, "hst", [128, 12])
                hn = sbt(st, "hn", [128, 512])
                hn2 = sbt(st, "hn2", [128, 512])
                mot = sbt(st, "bmo", [128, 512], BF16)
                ym = sbt(st, "ym", [128, 512], BF16)
                P.dma('sp', lambda e: e.dma_start(out=mnr[:], in_=mnw[l:l + 1, :].partition_broadcast(128)), writes=['mnr'])
                it = 0
                flatB = []
                for d_ in (1, 0):
                    order = ([1, 0] + list(range(NT - 1, NCT - 1, -1))) if d_ == 1 else list(range(NT))
                    flatB += [(d_, c, i == 0) for i, c in enumerate(order)]

                def loadsB(idx):
                    if idx >= len(flatB):
                        return
                    d_, c, _ = flatB[idx]
                    b = idx % 2
                    r0 = c * 128
                    P.dma('sp', lambda e, b=b, r0=r0, d_=d_: e.dma_start(
                        out=qTt[b][:], in_=MQ[d_][:, :, r0:r0 + 128].rearrange("h d n -> d h n")), writes=[('bq', b)])
                    P.dma('sp', lambda e, b=b, r0=r0, d_=d_: e.dma_start(
                        out=kTt[b][:], in_=MK[d_][:, :, r0:r0 + 128].rearrange("h d n -> d h n")), writes=[('bk', b)])
                    P.dma('sp', lambda e, b=b, r0=r0, d_=d_: e.dma_start(out=k2t[b][:], in_=MK2[d_][r0:r0 + 128]),
                          writes=[('bk2', b)])
                    P.dma('sp', lambda e, b=b, r0=r0: e.dma_start(out=vat[b][:], in_=MVA[r0:r0 + 128]), writes=[('bva', b)])
                    P.dma('sp', lambda e, b=b, c=c: e.dma_start(out=dct[b][:], in_=DEC[c]), writes=[('bdc', b)])

                loadsB(0)
                pendB = []
                for idxB, (d_, c, first) in enumerate(flatB):
                    mask = maskb if d_ == 1 else maskf
                    if first:
                        P.op('dve', lambda e: e.memset(Cst[:], 0.0), writes=['Cst'])
                    if True:
                        b = it % 2
                        it += 1
                        r0 = c * 128
                        need_out = (c >= NCT) or need_ctx
                        loadsB(idxB + 1)
                        if need_out:
                            P.op('act', lambda e: e.copy(out=Cbf[:], in_=Cst[:]), reads=['Cst'], writes=['Cbf'])
                            for h in range(4):
                                P.op('pe', lambda e, h=h, b=b: e.matmul(bk(0)[:, h * 128:(h + 1) * 128], lhsT=kTt[b][:, h, :],
                                                                        rhs=qTt[b][:, h, :], start=True, stop=True),
                                     reads=[('bq', b), ('bk', b)], writes=['pS'], banks=[0])
                            P.op('dve', lambda e, mask=mask: e.tensor_tensor(
                                out=SM[:], in0=bk(0).rearrange("p (h n) -> p h n", h=4),
                                in1=mask[:].unsqueeze(1).to_broadcast([128, 4, 128]), op=ALU.mult),
                                reads=['pS', mask.name], writes=['SM'], banks=[0])
                            for h in range(4):
                                bb = 1 + h // 2
                                o0 = (h % 2) * 129
                                P.op('pe', lambda e, h=h, b=b, bb=bb, o0=o0: e.matmul(
                                    bk(bb)[:, o0:o0 + 129], lhsT=SM[:, h, :], rhs=vat[b][:, h, :], start=(h % 2 == 0),
                                    stop=False, skip_group_check=True),
                                    reads=['SM', ('bva', b)], writes=[('pO', bb)], banks=[bb])
                                P.op('pe', lambda e, h=h, b=b, bb=bb, o0=o0: e.matmul(
                                    bk(bb)[:, o0:o0 + 129], lhsT=qTt[b][:, h, :], rhs=Cbf[:, h, :], start=False,
                                    stop=True, skip_group_check=True),
                                    reads=[('bq', b), 'Cbf'], writes=[('pO', bb)], banks=[bb])
                            hb_ = it % 2
                            for bb in (1, 2):
                                ov = bk(bb)[:, 0:258].rearrange("p (h n) -> p h n", h=2)
                                P.op('dve', lambda e, bb=bb, ov=ov: e.tensor_scalar(
                                    out=r4[:, (bb - 1) * 2:(bb - 1) * 2 + 2], in0=ov[:, :, 128], scalar1=-1.0, scalar2=1.0,
                                    op0=ALU.mult, op1=ALU.max),
                                    reads=[('pO', bb)], writes=[('r4n', bb)], banks=[bb])
                                P.op('dve', lambda e, bb=bb, ov=ov: e.tensor_tensor(
                                    out=r4[:, (bb - 1) * 2:(bb - 1) * 2 + 2], in0=r4[:, (bb - 1) * 2:(bb - 1) * 2 + 2],
                                    in1=ov[:, :, 128], op=ALU.max),
                                    reads=[('pO', bb), ('r4n', bb)], writes=[('r4a', bb)], banks=[bb])
                                P.op('dve', lambda e, bb=bb: e.reciprocal(out=r4[:, 4 + (bb - 1) * 2:4 + (bb - 1) * 2 + 2],
                                                                          in_=r4[:, (bb - 1) * 2:(bb - 1) * 2 + 2]),
                                     reads=[('r4a', bb)], writes=[('r4b', bb)])
                                P.op('dve', lambda e, bb=bb, ov=ov, hb_=hb_: e.tensor_tensor(
                                    out=hd[hb_][:, (bb - 1) * 256:(bb - 1) * 256 + 256].rearrange("p (h n) -> p h n", h=2),
                                    in0=ov[:, :, 0:128],
                                    in1=r4[:, 4 + (bb - 1) * 2:4 + (bb - 1) * 2 + 2].unsqueeze(2).to_broadcast([128, 2, 128]),
                                    op=ALU.mult),
                                    reads=[('pO', bb), ('r4b', bb)], writes=[('hd', hb_, bb)], banks=[bb])
                            while pendB:
                                pendB.pop(0)()
                            if d_ == 1:
                                P.dma('sp', lambda e, r0=r0, hb_=hb_: e.dma_start(out=HB[r0:r0 + 128, :], in_=hd[hb_][:]),
                                      reads=[('hd', hb_, 1), ('hd', hb_, 2)], writes=[('HB', c)])
                            else:
                              def mergeB(r0=r0, hb_=hb_, c=c):
                                P.dma('sp', lambda e, r0=r0: e.dma_start(out=hbt[:], in_=HB[r0:r0 + 128, :]), writes=['hbt'])
                                P.dma('sp', lambda e, r0=r0: e.dma_start(out=mot[:], in_=MO[r0:r0 + 128, :]), writes=['bmo'])
                                P.op('pool', lambda e, hb_=hb_: e.tensor_tensor(out=hs[:], in0=hd[hb_][:], in1=hbt[:], op=ALU.add),
                                     reads=[('hd', hb_, 1), ('hd', hb_, 2), 'hbt'], writes=['hs'])
                                P.op('act', lambda e: e.activation(out=sqh[:], in_=hs[:], func=AF.Square),
                                     reads=['hs'], writes=['sqh'])
                                P.op('dve', lambda e: e.tensor_reduce(out=hst[:, 0:4], in_=sqh[:].rearrange("p (h n) -> p h n", h=4),
                                                                      axis=AX.X, op=ALU.add), reads=['sqh'], writes=[('hst', 'ss')])
                                rstd_ops(hst, 0, 4, 8, 4, 1.0 / 128, 'hst')
                                P.op('dve', lambda e: e.tensor_tensor(
                                    out=hn[:].rearrange("p (h n) -> p h n", h=4), in0=hs[:].rearrange("p (h n) -> p h n", h=4),
                                    in1=hst[:, 8:12].unsqueeze(2).to_broadcast([128, 4, 128]), op=ALU.mult),
                                    reads=['hs', ('hst', 'r')], writes=['hn'])
                                P.op('pool', lambda e: e.tensor_tensor(out=hn2[:], in0=hn[:], in1=mnr[:], op=ALU.mult),
                                     reads=['hn', 'mnr'], writes=['hn2'])
                                P.op('dve', lambda e: e.tensor_tensor(out=ym[:], in0=hn2[:], in1=mot[:], op=ALU.mult),
                                     reads=['hn2', 'bmo'], writes=['ym'])
                                P.dma('sp', lambda e, r0=r0: e.dma_start(out=Y[r0:r0 + 128, 0:512], in_=ym[:]),
                                      reads=['ym'], writes=[('Ym', c)])
                              pendB.append(mergeB)
                        for h in range(4):
                            bb = 3 + h // 2
                            o0 = (h % 2) * 129
                            P.op('pe', lambda e, h=h, b=b, bb=bb, o0=o0: e.matmul(
                                bk(bb)[0:64, o0:o0 + 129], lhsT=k2t[b][:, h, :], rhs=vat[b][:, h, :], start=(h % 2 == 0),
                                stop=True, skip_group_check=True),
                                reads=[('bk2', b), ('bva', b)], writes=[('pC', bb)], banks=[bb])
                        for h in range(4):
                            bb = 3 + h // 2
                            o0 = (h % 2) * 129
                            P.op('dve', lambda e, h=h, b=b, bb=bb, o0=o0, d_=d_: e.scalar_tensor_tensor(
                                out=Cst[:, h, :], in0=Cst[:, h, :], scalar=dct[b][:, d_ * 4 + h:d_ * 4 + h + 1],
                                in1=bk(bb)[0:64, o0:o0 + 129], op0=ALU.mult, op1=ALU.add),
                                reads=[('pC', bb), ('bdc', b), 'Cst'], writes=['Cst'], banks=[bb])
                while pendB:
                    pendB.pop(0)()
                P.end_phase()

        def phaseC(l):
            need_ctx = (l < NL - 1)
            with ExitStack() as st:
                KTs = sbt(st, "KTs", [64, 2, NTOK], BF16)
                VAs = sbt(st, "VAs", [128, NT, 2, 65], BF16)
                qt = [sbt(st, "cq%d" % i, [64, 512], BF16) for i in range(2)]
                pT = [sbt(st, "cp%d" % i, [128, 512], BF16) for i in range(3)]
                rr = [sbt(st, "crr%d" % i, [128, 4]) for i in range(2)]
                ot = [sbt(st, "cot%d" % i, [128, 4, 64], BF16) for i in range(2)]
                for kv in range(2):
                    P.dma('sp', lambda e, kv=kv: e.dma_start(out=KTs[:, kv, :], in_=KT[kv]), writes=[('KTs', kv)])
                VAv = VA.rearrange("(t p) k d -> p t k d", p=128)
                for t0 in range(0, NT, 8):
                    t1 = min(NT, t0 + 8)
                    P.dma('sp', lambda e, t0=t0, t1=t1: e.dma_start(out=VAs[:, t0:t1], in_=VAv[:, t0:t1]), writes=[('VAs', t0)])
                groups = []
                if need_ctx:
                    groups.append((0, NCT * 128, list(range(NCT))))
                for q0 in range(NCT * 128, NTOK, 512):
                    groups.append((q0, min(512, NTOK - q0), list(range(NT))))
                if SPARSE and l == 0:
                    prep_moe_weights()
                heads = [(q0, nq, kts, h) for (q0, nq, kts) in groups for h in range(8)]
                seq = [(hi, ji) for hi in range(len(heads)) for ji in range(len(heads[hi][2]))]
                NSB = 3

                def load_q(hi):
                    if hi >= len(heads):
                        return
                    q0, nq, kts, h = heads[hi]
                    b = hi % 2
                    P.dma('sp', lambda e, b=b, h=h, q0=q0, nq=nq: e.dma_start(out=qt[b][:, 0:nq], in_=QT[h, :, q0:q0 + nq]),
                          writes=[('cq', b)])

                def emit_S(n):
                    if n >= len(seq):
                        return
                    hi, ji = seq[n]
                    q0, nq, kts, h = heads[hi]
                    j = kts[ji]
                    kv = h // 4
                    b = hi % 2
                    bs = n % NSB
                    P.op('pe', lambda e, bs=bs, kv=kv, j=j, b=b, nq=nq: e.matmul(
                        bk(bs)[:, 0:nq], lhsT=KTs[:, kv, j * 128:(j + 1) * 128], rhs=qt[b][:, 0:nq], start=True, stop=True),
                        reads=[('KTs', kv), ('cq', b)], writes=[('cS', bs)], banks=[bs])

                load_q(0)
                load_q(1)
                emit_S(0)
                emit_S(1)
                for n, (hi, ji) in enumerate(seq):
                    q0, nq, kts, h = heads[hi]
                    nsub = nq // 128
                    j = kts[ji]
                    kv = h // 4
                    b = hi % 2
                    bo = 3 + b
                    bs = n % NSB
                    pb = n % 3
                    if ji == 0 and hi >= 1:
                        load_q(hi + 1)
                    emit_S(n + 2)
                    P.op('act', lambda e, bs=bs, pb=pb, nq=nq: e.activation(out=pT[pb][:, 0:nq], in_=bk(bs)[:, 0:nq], func=AF.Exp),
                         reads=[('cS', bs)], writes=[('cp', pb)], banks=[bs])
                    for s in range(nsub):
                        P.op('pe', lambda e, s=s, pb=pb, j=j, kv=kv, bo=bo, ji=ji, kts=kts: e.matmul(
                            bk(bo)[:, s * 65:(s + 1) * 65], lhsT=pT[pb][:, s * 128:(s + 1) * 128], rhs=VAs[:, j, kv, :],
                            start=(ji == 0 and s == 0), stop=(ji == len(kts) - 1), skip_group_check=True),
                            reads=[('cp', pb), ('VAs', (j // 8) * 8)], writes=[('cO', bo)], banks=[bo])
                    if ji == len(kts) - 1:
                        ov = bk(bo)[:, 0:nsub * 65].rearrange("p (s d) -> p s d", d=65)
                        P.op('dve', lambda e, b=b, ov=ov, nsub=nsub: e.reciprocal(out=rr[b][:, 0:nsub], in_=ov[:, :, 64]),
                             reads=[('cO', bo)], writes=[('crr', b)], banks=[bo])
                        P.op('dve', lambda e, b=b, ov=ov, nsub=nsub: e.tensor_tensor(
                            out=ot[b][:, 0:nsub, :], in0=ov[:, :, 0:64],
                            in1=rr[b][:, 0:nsub].unsqueeze(2).to_broadcast([128, nsub, 64]), op=ALU.mult),
                            reads=[('cO', bo), ('crr', b)], writes=[('cot', b)], banks=[bo])
                        P.dma('sp', lambda e, b=b, q0=q0, nq=nq, h=h, nsub=nsub: e.dma_start(
                            out=Y[q0:q0 + nq, 512 + h * 64:512 + (h + 1) * 64].rearrange("(s p) d -> p s d", p=128),
                            in_=ot[b][:, 0:nsub, :]), reads=[('cot', b)], writes=[('Ya', q0, h)])
                P.end_phase()

        def phaseD(l):
            last = (l == NL - 1)
            moe = (l % 2 == 1)
            F_ = DFE if moe else DFF
            NFC = F_ // 128
            experts = list(range(NEXP)) if moe else [0]
            with ExitStack() as st:
                wo = sbt(st, "wo", [128, 8, D], BF16)
                fnr = sbt(st, "fnr", [128, D])
                rtr = sbt(st, "rtr", [128, 8, 8])
                yt = [sbt(st, "dy%d" % i, [128, D], BF16) for i in range(2)]
                yT = sbt(st, "dyT", [128, 8, 128], BF16)
                xr = [sbt(st, "xr%d" % i, [128, D]) for i in range(4)]
                tmpx = sbt(st, "tmpx", [128, 512])
                junk = sbt(st, "djunk", [128, D], BF16)
                stt = [sbt(st, "dstt%d" % i, [128, 8]) for i in range(2)]
                xn = sbt(st, "dxn", [128, D])
                h2T = sbt(st, "h2T", [128, 8, 512], BF16)
                h2F = sbt(st, "h2F", [128, 8, 128])
                lg = sbt(st, "lg", [128, 8])
                m8 = sbt(st, "m8", [128, 8])
                gg = sbt(st, "gg", [128, 8])
                eq = [sbt(st, "eq%d" % i, [128, 8]) for i in range(2)]
                W8 = [sbt(st, "W8_%d" % i, [128, 8]) for i in range(4)]
                yacc = [sbt(st, "yacc%d" % i, [128, D]) for i in range(4)] if moe else None
                wgs = [sbt(st, "wgs%d" % i, [128, 8, 512], BF16) for i in range(2)]
                wus = [sbt(st, "wus%d" % i, [128, 8, 512], BF16) for i in range(2)]
                wds = [sbt(st, "wds%d" % i, [128, 4, 512], BF16) for i in range(2)]
                sg = [sbt(st, "sg%d" % i, [128, 512]) for i in range(2)]
                AT = sbt(st, "AT", [128, NFC, 512], BF16)
                ob = [sbt(st, "ob%d" % i, [128, D]) for i in range(2)]
                P.dma('sp', lambda e: e.dma_start(out=wo[:], in_=WOb[l].rearrange("(k p) n -> p k n", p=128)), writes=['wo'])
                if last:
                    P.dma('sp', lambda e: e.dma_start(out=fnr[:], in_=fnw[0:1, :].partition_broadcast(128)), writes=['fnr'])
                if moe:
                    P.dma('sp', lambda e: e.dma_start(out=rtr[:], in_=router.rearrange("(k p) n -> p k n", p=128)), writes=['rtr'])
                Gw = [MGb[e_] for e_ in range(NEXP)] if moe else [FGb]
                Uw = [MUb[e_] for e_ in range(NEXP)] if moe else [FUb]
                Dw = [MDb[e_] for e_ in range(NEXP)] if moe else [FDb]
                tiles = []
                if not last:
                    tiles.append(list(range(NCT)))
                for t0 in range(NCT, NT, 4):
                    tiles.append(list(range(t0, min(NT, t0 + 4))))
                wi = 0
                di = 0
                yi = 0
                for tl in tiles:
                    nsub = len(tl)
                    ntok = nsub * 128
                    w = 1 if tl[0] < NCT else 0
                    for s, t in enumerate(tl):
                        r0 = t * 128
                        yb = yi % 2
                        yi += 1
                        P.dma('sp', lambda e, yb=yb, r0=r0: e.dma_start(out=yt[yb][:], in_=Y[r0:r0 + 128, :]),
                              reads=[], writes=[('dy', yb)])
                        xs_ = xin if l == 0 else X
                        P.dma('sp', lambda e, s=s, r0=r0, xs_=xs_: e.dma_start(out=xr[s][:], in_=xs_[r0:r0 + 128, :]),
                              writes=[('xr', s)])
                        for k in range(8):
                            P.op('pe', lambda e, k=k, yb=yb: e.transpose(out=bkb(0)[:, k * 128:(k + 1) * 128],
                                                                        in_=yt[yb][:, k * 128:(k + 1) * 128], identity=identb[:]),
                                 reads=[('dy', yb), 'identb'], writes=['pyT'], banks=[0])
                        P.op('dve', lambda e: e.tensor_copy(out=yT[:, 0:4, :], in_=bkb(0)[:, 0:512].rearrange("p (a b) -> p a b", a=4)),
                             reads=['pyT'], writes=[('yT', 0)], banks=[0])
                        P.op('act', lambda e: e.copy(out=yT[:, 4:8, :], in_=bkb(0)[:, 512:1024].rearrange("p (a b) -> p a b", a=4)),
                             reads=['pyT'], writes=[('yT', 1)], banks=[0])
                        for half in range(2):
                            for k in range(8):
                                P.op('pe', lambda e, k=k, half=half: e.matmul(
                                    bk(1 + half), lhsT=yT[:, k, :], rhs=wo[:, k, half * 512:(half + 1) * 512],
                                    start=(k == 0), stop=(k == 7)),
                                    reads=[('yT', k // 4), 'wo'], writes=[('pwo', half)], banks=[1 + half])
                            P.op('dve', lambda e, half=half, w=w: e.tensor_tensor(
                                out=tmpx[:], in0=bk(1 + half), in1=GATE[w][0][:, half * 512:(half + 1) * 512], op=ALU.mult),
                                reads=[('pwo', half), (GATE[w][0].name, half)], writes=['tmpx'], banks=[1 + half])
                            P.op('pool', lambda e, half=half, s=s: e.tensor_tensor(
                                out=xr[s][:, half * 512:(half + 1) * 512], in0=tmpx[:], in1=xr[s][:, half * 512:(half + 1) * 512],
                                op=ALU.add), reads=['tmpx', ('xr', s)], writes=[('xr', s)])
                        tb = s % 2
                        norm_to_hT((junk, stt[tb], xn), xr[s][:], ('xr', s), w, 1,
                                   lambda k, s=s: h2T[:, k, s * 128:(s + 1) * 128], lambda k, s=s: ('h2T', s, k), 3, 4, tb,
                                   hF_dst_fn=(lambda k: h2F[:, k, :]) if moe else None)
                        if moe:
                            for k in range(8):
                                P.op('pe', lambda e, k=k: e.matmul(bk(5)[:, 0:8], lhsT=h2F[:, k, :], rhs=rtr[:, k, :],
                                                                   start=(k == 0), stop=(k == 7)),
                                     reads=[('hF', k), 'rtr'], writes=['plg'], banks=[5])
                            P.op('dve', lambda e: e.tensor_copy(out=lg[:], in_=bk(5)[:, 0:8]), reads=['plg'], writes=['lg'], banks=[5])
                            P.op('dve', lambda e: e.max(out=m8[:], in_=lg[:]), reads=['lg'], writes=['m8'])
                            P.op('dve', lambda e: e.tensor_tensor(out=gg[:, 0:1], in0=m8[:, 1:2], in1=m8[:, 0:1], op=ALU.subtract),
                                 reads=['m8'], writes=['gg0'])
                            P.op('act', lambda e: e.activation(out=gg[:, 1:2], in_=gg[:, 0:1], func=AF.Exp), reads=['gg0'], writes=['gg1'])
                            P.op('dve', lambda e: e.tensor_scalar(out=gg[:, 2:3], in0=gg[:, 1:2], scalar1=1.0, scalar2=None, op0=ALU.add),
                                 reads=['gg1'], writes=['gg2'])
                            P.op('dve', lambda e: e.reciprocal(out=gg[:, 3:4], in_=gg[:, 2:3]), reads=['gg2'], writes=['gg3'])
                            P.op('dve', lambda e: e.tensor_tensor(out=gg[:, 4:5], in0=gg[:, 1:2], in1=gg[:, 3:4], op=ALU.mult),
                                 reads=['gg1', 'gg3'], writes=['gg4'])
                            P.op('dve', lambda e: e.tensor_scalar(out=eq[0][:], in0=lg[:], scalar1=m8[:, 0:1], scalar2=gg[:, 3:4],
                                                                  op0=ALU.is_equal, op1=ALU.mult),
                                 reads=['lg', 'm8', 'gg3'], writes=['eq0'])
                            P.op('dve', lambda e: e.tensor_scalar(out=eq[1][:], in0=lg[:], scalar1=m8[:, 1:2], scalar2=gg[:, 4:5],
                                                                  op0=ALU.is_equal, op1=ALU.mult),
                                 reads=['lg', 'm8', 'gg4'], writes=['eq1'])
                            P.op('dve', lambda e, s=s: e.tensor_tensor(out=W8[s][:], in0=eq[0][:], in1=eq[1][:], op=ALU.add),
                                 reads=['eq0', 'eq1'], writes=[('W8', s)])
                    h2keys = [('h2T', s, k) for s in range(nsub) for k in range(8)]
                    for ei, e_ in enumerate(experts):
                        for fg0 in range(0, F_, 512):
                            fw = min(512, F_ - fg0)
                            wb = wi % 2
                            wi += 1
                            P.dma('sp', lambda e, wb=wb, e_=e_, fg0=fg0, fw=fw: e.dma_start(
                                out=wgs[wb][:, :, 0:fw], in_=Gw[e_][:, fg0:fg0 + fw].rearrange("(k p) f -> p k f", p=128)),
                                writes=[('wgs', wb)])
                            P.dma('sp', lambda e, wb=wb, e_=e_, fg0=fg0, fw=fw: e.dma_start(
                                out=wus[wb][:, :, 0:fw], in_=Uw[e_][:, fg0:fg0 + fw].rearrange("(k p) f -> p k f", p=128)),
                                writes=[('wus', wb)])
                            for fc in range(fw // 128):
                                fidx = fg0 // 128 + fc
                                gb = fidx % 2
                                for k in range(8):
                                    P.op('pe', lambda e, k=k, wb=wb, fc=fc, gb=gb, ntok=ntok: e.matmul(
                                        bk(6 + gb)[:, 0:ntok], lhsT=wgs[wb][:, k, fc * 128:(fc + 1) * 128], rhs=h2T[:, k, 0:ntok],
                                        start=(k == 0), stop=(k == 7)),
                                        reads=[('wgs', wb)] + ([('h2T', s, k) for s in range(nsub)]), writes=[('pG', gb)], banks=[6 + gb])
                                for k in range(8):
                                    P.op('pe', lambda e, k=k, wb=wb, fc=fc, gb=gb, ntok=ntok: e.matmul(
                                        bk(4 + gb)[:, 0:ntok], lhsT=wus[wb][:, k, fc * 128:(fc + 1) * 128], rhs=h2T[:, k, 0:ntok],
                                        start=(k == 0), stop=(k == 7)),
                                        reads=[('wus', wb)] + ([('h2T', s, k) for s in range(nsub)]), writes=[('pU', gb)], banks=[4 + gb])
                                P.op('act', lambda e, gb=gb, ntok=ntok: e.activation(out=sg[gb][:, 0:ntok], in_=bk(6 + gb)[:, 0:ntok],
                                                                                     func=AF.Silu),
                                     reads=[('pG', gb)], writes=[('sg', gb)], banks=[6 + gb])
                                P.op('dve', lambda e, gb=gb, ntok=ntok, fidx=fidx: e.tensor_tensor(
                                    out=AT[:, fidx, 0:ntok], in0=bk(4 + gb)[:, 0:ntok], in1=sg[gb][:, 0:ntok], op=ALU.mult),
                                    reads=[('pU', gb), ('sg', gb)], writes=[('AT', fidx)], banks=[4 + gb])
                        for half in range(2):
                            for fg0 in range(0, F_, 512):
                                fw = min(512, F_ - fg0)
                                db = di % 2
                                di += 1
                                P.dma('sp', lambda e, db=db, e_=e_, fg0=fg0, fw=fw, half=half: e.dma_start(
                                    out=wds[db][:, 0:fw // 128, :],
                                    in_=Dw[e_][fg0:fg0 + fw, half * 512:(half + 1) * 512].rearrange("(c p) d -> p c d", p=128)),
                                    writes=[('wds', db)])
                                for fc in range(fw // 128):
                                    fidx = fg0 // 128 + fc
                                    for s in range(nsub):
                                        P.op('pe', lambda e, s=s, fidx=fidx, fc=fc, db=db: e.matmul(
                                            bk(s), lhsT=AT[:, fidx, s * 128:(s + 1) * 128], rhs=wds[db][:, fc, :],
                                            start=(fidx == 0), stop=(fidx == NFC - 1)),
                                            reads=[('AT', fidx), ('wds', db)], writes=[('pD', s)], banks=[s])
                            for s in range(nsub):
                                hs_ = slice(half * 512, (half + 1) * 512)
                                if moe:
                                    if ei == 0:
                                        P.op('dve', lambda e, s=s, hs_=hs_, e_=e_: e.tensor_scalar(
                                            out=yacc[s][:, hs_], in0=bk(s), scalar1=W8[s][:, e_:e_ + 1], scalar2=None, op0=ALU.mult),
                                            reads=[('pD', s), ('W8', s)], writes=[('yacc', s, half)], banks=[s])
                                    else:
                                        P.op('dve', lambda e, s=s, hs_=hs_, e_=e_: e.scalar_tensor_tensor(
                                            out=yacc[s][:, hs_], in0=bk(s), scalar=W8[s][:, e_:e_ + 1], in1=yacc[s][:, hs_],
                                            op0=ALU.mult, op1=ALU.add),
                                            reads=[('pD', s), ('W8', s), ('yacc', s, half)], writes=[('yacc', s, half)], banks=[s])
                                else:
                                    P.op('dve', lambda e, s=s, hs_=hs_, w=w: e.tensor_tensor(
                                        out=tmpx[:], in0=bk(s), in1=GATE[w][1][:, hs_], op=ALU.mult),
                                        reads=[('pD', s), (GATE[w][1].name, half)], writes=['tmpx'], banks=[s])
                                    P.op('pool', lambda e, s=s, hs_=hs_: e.tensor_tensor(
                                        out=xr[s][:, hs_], in0=tmpx[:], in1=xr[s][:, hs_], op=ALU.add),
                                        reads=['tmpx', ('xr', s)], writes=[('xr', s)])
                    for s, t in enumerate(tl):
                        r0 = t * 128
                        if moe:
                            for half in range(2):
                                hs_ = slice(half * 512, (half + 1) * 512)
                                P.op('pool', lambda e, s=s, hs_=hs_, w=w: e.tensor_tensor(
                                    out=yacc[s][:, hs_], in0=yacc[s][:, hs_], in1=GATE[w][1][:, hs_], op=ALU.mult),
                                    reads=[('yacc', s, half), (GATE[w][1].name, half)], writes=[('yacc', s, half)])
                                P.op('dve', lambda e, s=s, hs_=hs_: e.tensor_tensor(
                                    out=xr[s][:, hs_], in0=yacc[s][:, hs_], in1=xr[s][:, hs_], op=ALU.add),
                                    reads=[('yacc', s, half), ('xr', s)], writes=[('xr', s)])
                        if not last:
                            P.dma('sp', lambda e, s=s, r0=r0: e.dma_start(out=X[r0:r0 + 128, :], in_=xr[s][:]),
                                  reads=[('xr', s)], writes=[('X', t)])
                        else:
                            tb = s % 2
                            key = ('fst', tb)
                            P.op('act', lambda e, s=s, tb=tb: e.activation(out=junk[:], in_=xr[s][:], func=AF.Square,
                                                                           accum_out=stt[tb][:, 4:5]),
                                 reads=[('xr', s)], writes=['junk', (key, 'ss')])
                            rstd_ops(stt[tb], 4, 5, 6, 1, 1.0 / D, key)
                            P.op('dve', lambda e, s=s, tb=tb: e.scalar_tensor_tensor(
                                out=ob[tb][:], in0=xr[s][:], scalar=stt[tb][:, 6:7], in1=fnr[:], op0=ALU.mult, op1=ALU.mult),
                                reads=[('xr', s), (key, 'r'), 'fnr'], writes=[('ob', tb)])
                            o0 = r0 - NCT * 128
                            P.dma('sp', lambda e, tb=tb, o0=o0: e.dma_start(out=out[o0:o0 + 128, :], in_=ob[tb][:]),
                                  reads=[('ob', tb)], writes=[('out', t)])
                P.end_phase()

        def phaseD_sparse(l):
            assert l == NL - 1
            w = 0
            oR, oJ, oG = 0, 8, 8 + NJ
            oCGU = oG + NG
            oCD = oCGU + 7
            oCH = oCD + 14
            REGB = mconst[:, oR:oR + 8]
            J512 = mconst[:, oJ:oJ + NJ]
            G40 = mconst[:, oG:oG + NG]
            CGU = mconst[:, oCGU:oCGU + 7]
            CD = mconst[:, oCD:oCD + 14]
            CH = mconst[:, oCH:oCH + 4]
            NFC = DFE // 128
            with ExitStack() as pst:
                E0 = sbt(pst, "E0", [128, NLT, 8])
                E1 = sbt(pst, "E1", [128, NLT, 8])
                POS = sbt(pst, "POS", [128, NLT, 8])
                G12 = sbt(pst, "G12", [128, NLT, 2])
                RUN = sbt(pst, "RUN", [128, 8])
                BASE = sbt(pst, "BASE", [128, 8])
                IDXGU = sbt(pst, "IDXGU", [128, NG, 7], I32)
                IDXU = sbt(pst, "IDXU", [128, NG, 7], I32)
                IDXD = sbt(pst, "IDXD", [128, NG, 14], I32)
                IDXH = sbt(pst, "IDXH", [128, NG, 4], I32)
                RAi = sbt(pst, "RAi", [128, NLT], I32)
                RBi = sbt(pst, "RBi", [128, NLT], I32)
                fnr = sbt(pst, "sfnr", [128, D])
                with ExitStack() as st:
                    wo = sbt(st, "wo", [128, 8, D], BF16)
                    rtr = sbt(st, "rtr", [128, 8, 8])
                    yt = [sbt(st, "dy%d" % i, [128, D], BF16) for i in range(2)]
                    yT = sbt(st, "dyT", [128, 8, 128], BF16)
                    xr = [sbt(st, "xr%d" % i, [128, D]) for i in range(3)]
                    tmpx = sbt(st, "tmpx", [128, 512])
                    junk = sbt(st, "djunk", [128, D], BF16)
                    stt = [sbt(st, "dstt%d" % i, [128, 8]) for i in range(2)]
                    xn = sbt(st, "dxn", [128, D])
                    h2 = sbt(st, "h2", [128, D])
                    H2b = [sbt(st, "H2b%d" % i, [128, D], BF16) for i in range(2)]
                    h2F = sbt(st, "h2F", [128, 8, 128])
                    lg = sbt(st, "lg", [128, 8])
                    m8 = sbt(st, "m8", [128, 8])
                    gg = sbt(st, "gg", [128, 8])
                    M8 = sbt(st, "M8", [128, 8])
                    t8 = sbt(st, "t8", [128, 8])
                    p8 = sbt(st, "p8", [128, 16])
                    slf = sbt(st, "slf", [128, 2])
                    sli = [sbt(st, "sli%d" % i, [128, 2], I32) for i in range(2)]
                    P.dma('sp', lambda e: e.dma_start(out=wo[:], in_=WOb[l].rearrange("(k p) n -> p k n", p=128)), writes=['wo'])
                    P.dma('sp', lambda e: e.dma_start(out=fnr[:], in_=fnw[0:1, :].partition_broadcast(128)), writes=['fnr'])
                    P.dma('sp', lambda e: e.dma_start(out=rtr[:], in_=router.rearrange("(k p) n -> p k n", p=128)), writes=['rtr'])
                    P.op('dve', lambda e: e.memset(RUN[:], 0.0), writes=['RUN'])

                    def loadsD1(ti_):
                        if ti_ >= NLT:
                            return
                        r0_ = (NCT + ti_) * 128
                        b_ = ti_ % 2
                        s_ = ti_ % 3
                        P.dma('sp', lambda e, b_=b_, r0_=r0_: e.dma_start(out=yt[b_][:], in_=Y[r0_:r0_ + 128, :]), writes=[('dy', b_)])
                        P.dma('sp', lambda e, s_=s_, r0_=r0_: e.dma_start(out=xr[s_][:], in_=X[r0_:r0_ + 128, :]), writes=[('xr', s_)])

                    def frontD1(ti):
                        t = NCT + ti
                        r0 = t * 128
                        yb = ti % 2
                        s = ti % 3
                        if ti == 0:
                            loadsD1(0)
                        loadsD1(ti + 1)
                        for k in range(8):
                            P.op('pe', lambda e, k=k, yb=yb: e.transpose(out=bkb(0)[:, k * 128:(k + 1) * 128],
                                                                        in_=yt[yb][:, k * 128:(k + 1) * 128], identity=identb[:]),
                                 reads=[('dy', yb), 'identb'], writes=['pyT'], banks=[0])
                        P.op('dve', lambda e: e.tensor_copy(out=yT[:, 0:4, :], in_=bkb(0)[:, 0:512].rearrange("p (a b) -> p a b", a=4)),
                             reads=['pyT'], writes=[('yT', 0)], banks=[0])
                        P.op('act', lambda e: e.copy(out=yT[:, 4:8, :], in_=bkb(0)[:, 512:1024].rearrange("p (a b) -> p a b", a=4)),
                             reads=['pyT'], writes=[('yT', 1)], banks=[0])
                        for half in range(2):
                            for k in range(8):
                                P.op('pe', lambda e, k=k, half=half: e.matmul(
                                    bk(1 + half), lhsT=yT[:, k, :], rhs=wo[:, k, half * 512:(half + 1) * 512],
                                    start=(k == 0), stop=(k == 7)),
                                    reads=[('yT', k // 4), 'wo'], writes=[('pwo', half)], banks=[1 + half])
                            P.op('dve', lambda e, half=half: e.tensor_tensor(
                                out=tmpx[:], in0=bk(1 + half), in1=GATE[w][0][:, half * 512:(half + 1) * 512], op=ALU.mult),
                                reads=[('pwo', half), (GATE[w][0].name, half)], writes=['tmpx'], banks=[1 + half])
                            P.op('pool', lambda e, half=half, s=s: e.tensor_tensor(
                                out=xr[s][:, half * 512:(half + 1) * 512], in0=tmpx[:], in1=xr[s][:, half * 512:(half + 1) * 512],
                                op=ALU.add), reads=['tmpx', ('xr', s)], writes=[('xr', s)])
                        P.dma('sp', lambda e, s=s, r0=r0: e.dma_start(out=X[r0:r0 + 128, :], in_=xr[s][:]),
                              reads=[('xr', s)], writes=[('X', t)])

                    def backD1(ti):
                        t = NCT + ti
                        s = ti % 3
                        tb = ti % 2
                        key = ('nst', tb)
                        P.op('act', lambda e, s=s, tb=tb: e.activation(out=junk[:], in_=xr[s][:], func=AF.Square,
                                                                       accum_out=stt[tb][:, 0:1]),
                             reads=[('xr', s)], writes=['junk', (key, 'ss')])
                        rstd_ops(stt[tb], 0, 1, 2, 1, 1.0 / D, key)
                        P.op('dve', lambda e, s=s, tb=tb: e.tensor_scalar(out=xn[:], in0=xr[s][:], scalar1=stt[tb][:, 2:3], scalar2=None,
                                                                          op0=ALU.mult), reads=[('xr', s), (key, 'r')], writes=['xn'])
                        P.op('pool', lambda e: e.tensor_tensor(out=h2[:], in0=xn[:], in1=G2R[:], op=ALU.mult),
                             reads=['xn', ('G2R', 0), ('G2R', 1)], writes=['h2a'])
                        P.op('dve', lambda e: e.tensor_tensor(out=h2[:], in0=h2[:], in1=S2R[:], op=ALU.add),
                             reads=['h2a', ('S2R', 0), ('S2R', 1)], writes=['h2'])
                        hb = ti % 2
                        P.op('act', lambda e, hb=hb: e.copy(out=H2b[hb][:], in_=h2[:]), reads=['h2'], writes=[('H2b', hb)])
                        for k in range(8):
                            bb = 3 if k < 4 else 4
                            P.op('pe', lambda e, k=k, bb=bb: e.transpose(out=bk(bb)[:, (k % 4) * 128:(k % 4 + 1) * 128],
                                                                        in_=h2[:, k * 128:(k + 1) * 128], identity=identf[:]),
                                 reads=['h2', 'identf'], writes=[('pT', bb)], banks=[bb])
                        P.op('dve', lambda e: e.tensor_copy(out=h2F[:, 0:4, :], in_=bk(3).rearrange("p (a b) -> p a b", a=4)),
                             reads=[('pT', 3)], writes=[('h2F', 0)], banks=[3])
                        P.op('act', lambda e: e.copy(out=h2F[:, 4:8, :], in_=bk(4).rearrange("p (a b) -> p a b", a=4)),
                             reads=[('pT', 4)], writes=[('h2F', 1)], banks=[4])
                        for k in range(8):
                            P.op('pe', lambda e, k=k: e.matmul(bk(5)[:, 0:8], lhsT=h2F[:, k, :], rhs=rtr[:, k, :],
                                                               start=(k == 0), stop=(k == 7)),
                                 reads=[('h2F', k // 4), 'rtr'], writes=['plg'], banks=[5])
                        P.op('dve', lambda e: e.tensor_copy(out=lg[:], in_=bk(5)[:, 0:8]), reads=['plg'], writes=['lg'], banks=[5])
                        P.op('dve', lambda e: e.max(out=m8[:], in_=lg[:]), reads=['lg'], writes=['m8'])
                        P.op('dve', lambda e: e.tensor_tensor(out=gg[:, 0:1], in0=m8[:, 1:2], in1=m8[:, 0:1], op=ALU.subtract),
                             reads=['m8'], writes=['gg0'])
                        P.op('act', lambda e: e.activation(out=gg[:, 1:2], in_=gg[:, 0:1], func=AF.Exp), reads=['gg0'], writes=['gg1'])
                        P.op('dve', lambda e: e.tensor_scalar(out=gg[:, 2:3], in0=gg[:, 1:2], scalar1=1.0, scalar2=None, op0=ALU.add),
                             reads=['gg1'], writes=['gg2'])
                        P.op('dve', lambda e, ti=ti: e.reciprocal(out=G12[:, ti, 0:1], in_=gg[:, 2:3]), reads=['gg2'], writes=[('G12a', ti)])
                        P.op('dve', lambda e, ti=ti: e.tensor_tensor(out=G12[:, ti, 1:2], in0=gg[:, 1:2], in1=G12[:, ti, 0:1], op=ALU.mult),
                             reads=['gg1', ('G12a', ti)], writes=[('G12b', ti)])
                        P.op('dve', lambda e, ti=ti: e.tensor_scalar(out=E0[:, ti, :], in0=lg[:], scalar1=m8[:, 0:1], scalar2=None,
                                                                     op0=ALU.is_equal), reads=['lg', 'm8'], writes=[('E0', ti)])
                        P.op('dve', lambda e, ti=ti: e.tensor_scalar(out=E1[:, ti, :], in0=lg[:], scalar1=m8[:, 1:2], scalar2=None,
                                                                     op0=ALU.is_equal), reads=['lg', 'm8'], writes=[('E1', ti)])
                        P.op('dve', lambda e, ti=ti: e.tensor_tensor(out=M8[:], in0=E0[:, ti, :], in1=E1[:, ti, :], op=ALU.add),
                             reads=[('E0', ti), ('E1', ti)], writes=['M8'])
                        P.op('pe', lambda e: e.matmul(bk(6)[:, 0:8], lhsT=trif[:], rhs=M8[:], start=True, stop=True),
                             reads=['M8', 'trif'], writes=['ppx'], banks=[6])
                        P.op('pe', lambda e: e.matmul(bk(6)[:, 8:16], lhsT=onesf[:], rhs=M8[:], start=True, stop=True),
                             reads=['M8', 'onesf'], writes=['ppt'], banks=[6])
                        P.op('dve', lambda e: e.tensor_copy(out=p8[:], in_=bk(6)[:, 0:16]), reads=['ppx', 'ppt'], writes=['p8'], banks=[6])
                        P.op('dve', lambda e: e.tensor_tensor(out=t8[:], in0=p8[:, 0:8], in1=M8[:], op=ALU.subtract),
                             reads=['p8', 'M8'], writes=['t8'])
                        P.op('dve', lambda e, ti=ti: e.tensor_tensor(out=POS[:, ti, :], in0=t8[:], in1=RUN[:], op=ALU.add),
                             reads=['t8', 'RUN'], writes=[('POS', ti)])
                        P.op('dve', lambda e: e.tensor_tensor(out=RUN[:], in0=RUN[:], in1=p8[:, 8:16], op=ALU.add),
                             reads=['RUN', 'p8', ('POS', ti)], writes=['RUN'])
                        P.op('dve', lambda e, ti=ti: e.tensor_tensor(out=t8[:], in0=POS[:, ti, :], in1=REGB, op=ALU.add),
                             reads=[('POS', ti), 'mconst', 't8'], writes=['t8r'])
                        P.op('dve', lambda e, ti=ti: e.tensor_tensor(out=M8[:], in0=t8[:], in1=E0[:, ti, :], op=ALU.mult),
                             reads=['t8r', ('E0', ti), 'M8', 'p8', 't8'], writes=['M8a'])
                        P.op('dve', lambda e: e.tensor_reduce(out=slf[:, 0:1], in_=M8[:], axis=AX.X, op=ALU.add),
                             reads=['M8a'], writes=['slfa'])
                        P.op('dve', lambda e, ti=ti: e.tensor_tensor(out=M8[:], in0=t8[:], in1=E1[:, ti, :], op=ALU.mult),
                             reads=['t8r', ('E1', ti), 'M8a', 'slfa'], writes=['M8b'])
                        P.op('dve', lambda e: e.tensor_reduce(out=slf[:, 1:2], in_=M8[:], axis=AX.X, op=ALU.add),
                             reads=['M8b'], writes=['slfb'])
                        P.op('dve', lambda e, hb=hb: e.tensor_copy(out=sli[hb][:], in_=slf[:]), reads=['slfa', 'slfb'], writes=[('sli', hb)])
                        for j in range(2):
                            P.dma('pool', lambda e, hb=hb, j=j: e.indirect_dma_start(
                                out=HS, out_offset=bass.IndirectOffsetOnAxis(ap=sli[hb][:, j:j + 1], axis=0),
                                in_=H2b[hb][:], in_offset=None),
                                reads=[('H2b', hb), ('sli', hb)], writes=[('HS', ti, j)])
                    frontD1(0)
                    for ti in range(NLT):
                        if ti + 1 < NLT:
                            frontD1(ti + 1)
                        backD1(ti)
                    ng = sbt(st, "ng", [128, 8])
                    cum = sbt(st, "cum", [128, 8])
                    cj = sbt(st, "cj", [128, NJ])
                    cg = sbt(st, "cg", [128, NG])
                    EG = sbt(st, "EG", [128, NG])
                    CE = sbt(st, "CE", [128, NG])
                    LG = sbt(st, "LG", [128, NG])
                    ROWB = sbt(st, "ROWB", [128, NG])
                    EGa = sbt(st, "EGa", [128, NG])
                    EGb = sbt(st, "EGb", [128, NG])
                    for e_ in range(NEXP):
                        P.op('dve', lambda e, e_=e_: e.tensor_scalar(out=cj[:], in0=J512, scalar1=RUN[:, e_:e_ + 1], scalar2=None,
                                                                     op0=ALU.is_lt), reads=['RUN', 'mconst', ('ngr', e_ - 1)], writes=[('cj', e_)])
                        P.op('dve', lambda e, e_=e_: e.tensor_reduce(out=ng[:, e_:e_ + 1], in_=cj[:], axis=AX.X, op=ALU.add),
                             reads=[('cj', e_)], writes=[('ngr', e_)])
                    ngk = [('ngr', e_) for e_ in range(NEXP)]
                    P.op('dve', lambda e: e.tensor_copy(out=cum[:, 0:1], in_=ng[:, 0:1]), reads=ngk, writes=[('cum', 0)])
                    for e_ in range(1, NEXP):
                        P.op('dve', lambda e, e_=e_: e.tensor_tensor(out=cum[:, e_:e_ + 1], in0=cum[:, e_ - 1:e_], in1=ng[:, e_:e_ + 1],
                                                                     op=ALU.add), reads=ngk + [('cum', e_ - 1)], writes=[('cum', e_)])
                    cumk = [('cum', e_) for e_ in range(NEXP)]
                    P.op('dve', lambda e: e.tensor_tensor(out=BASE[:], in0=cum[:], in1=ng[:], op=ALU.subtract), reads=cumk + ngk, writes=['BASE0'])
                    P.op('dve', lambda e: e.tensor_scalar(out=BASE[:], in0=BASE[:], scalar1=512.0, scalar2=None, op0=ALU.mult),
                         reads=['BASE0'], writes=['BASE'])
                    P.op('dve', lambda e: e.memset(EG[:], 0.0), writes=['EG'])
                    P.op('dve', lambda e: e.memset(CE[:], 0.0), writes=['CE'])
                    for e_ in range(NEXP):
                        P.op('dve', lambda e, e_=e_: e.tensor_scalar(out=cg[:], in0=G40, scalar1=cum[:, e_:e_ + 1], scalar2=None,
                                                                     op0=ALU.is_ge), reads=cumk + ['mconst', 'EG', 'CE'], writes=['cg'])
                        P.op('dve', lambda e: e.tensor_tensor(out=EG[:], in0=EG[:], in1=cg[:], op=ALU.add), reads=['cg', 'EG'], writes=['EG'])
                        P.op('dve', lambda e, e_=e_: e.scalar_tensor_tensor(out=CE[:], in0=cg[:], scalar=ng[:, e_:e_ + 1], in1=CE[:],
                                                                            op0=ALU.mult, op1=ALU.add), reads=['cg', 'CE'] + ngk, writes=['CE'])
                    P.op('dve', lambda e: e.tensor_scalar(out=EG[:], in0=EG[:], scalar1=float(NEXP - 1), scalar2=None, op0=ALU.min),
                         reads=['EG'], writes=['EGc'])
                    P.op('dve', lambda e: e.tensor_tensor(out=LG[:], in0=G40, in1=CE[:], op=ALU.subtract), reads=['CE', 'mconst'], writes=['LG0'])
                    P.op('dve', lambda e: e.tensor_scalar(out=LG[:], in0=LG[:], scalar1=float(NJ - 1), scalar2=512.0, op0=ALU.min, op1=ALU.mult),
                         reads=['LG0'], writes=['LG'])
                    P.op('dve', lambda e: e.scalar_tensor_tensor(out=ROWB[:], in0=EG[:], scalar=float(CAP), in1=LG[:], op0=ALU.mult, op1=ALU.add),
                         reads=['EGc', 'LG'], writes=['ROWB'])
                    P.op('dve', lambda e: e.tensor_scalar(out=EGa[:], in0=EG[:], scalar1=float(7 * 128), scalar2=None, op0=ALU.mult),
                         reads=['EGc'], writes=['EGa'])
                    P.op('dve', lambda e: e.tensor_scalar(out=EGb[:], in0=EG[:], scalar1=float(14 * 128), scalar2=None, op0=ALU.mult),
                         reads=['EGc'], writes=['EGb'])
                    P.op('dve', lambda e: e.tensor_tensor(out=IDXGU[:], in0=EGa[:].unsqueeze(2).to_broadcast([128, NG, 7]),
                                                          in1=CGU.unsqueeze(1).to_broadcast([128, NG, 7]), op=ALU.add),
                         reads=['EGa', 'mconst'], writes=['IDXGU'])
                    P.op('dve', lambda e: e.tensor_tensor(out=IDXU[:], in0=EGa[:].unsqueeze(2).to_broadcast([128, NG, 7]),
                                                          in1=CGU.unsqueeze(1).to_broadcast([128, NG, 7]), op=ALU.add),
                         reads=['EGa', 'mconst'], writes=['IDXU'])
                    P.op('dve', lambda e: e.tensor_tensor(out=IDXD[:], in0=EGb[:].unsqueeze(2).to_broadcast([128, NG, 14]),
                                                          in1=CD.unsqueeze(1).to_broadcast([128, NG, 14]), op=ALU.add),
                         reads=['EGb', 'mconst'], writes=['IDXD'])
                    P.op('dve', lambda e: e.tensor_tensor(out=IDXH[:], in0=ROWB[:].unsqueeze(2).to_broadcast([128, NG, 4]),
                                                          in1=CH.unsqueeze(1).to_broadcast([128, NG, 4]), op=ALU.add),
                         reads=['ROWB', 'mconst'], writes=['IDXH'])
                    TP = sbt(st, "TP", [128, NLT, 8])
                    TQ = sbt(st, "TQ", [128, NLT, 8])
                    RAf = sbt(st, "RAf", [128, NLT])
                    posk = [('POS', ti) for ti in range(NLT)]
                    P.op('dve', lambda e: e.tensor_tensor(out=TP[:], in0=POS[:], in1=BASE[:].unsqueeze(1).to_broadcast([128, NLT, 8]),
                                                          op=ALU.add), reads=posk + ['BASE'], writes=['TP'])
                    for (EE, RI, nm) in ((E0, RAi, 'E0'), (E1, RBi, 'E1')):
                        P.op('dve', lambda e, EE=EE: e.tensor_tensor(out=TQ[:], in0=TP[:], in1=EE[:], op=ALU.mult),
                             reads=['TP', 'RAf'] + [(nm, ti) for ti in range(NLT)], writes=['TQ'])
                        P.op('dve', lambda e: e.tensor_reduce(out=RAf[:], in_=TQ[:], axis=AX.X, op=ALU.add), reads=['TQ'], writes=['RAf'])
                        P.op('dve', lambda e, RI=RI: e.tensor_copy(out=RI[:], in_=RAf[:]), reads=['RAf'], writes=[RI.name])
                    P.end_phase()
                with ExitStack() as st:
                    hs = [sbt(st, "hs%d" % i, [128, D], BF16) for i in range(8)]
                    h2T = sbt(st, "h2T", [128, 8, 512], BF16)
                    wgs = [sbt(st, "wgs%d" % i, [128, 8 * 512], BF16) for i in range(2)]
                    wus = [sbt(st, "wus%d" % i, [128, 8 * 512], BF16) for i in range(2)]
                    wds = [sbt(st, "wds%d" % i, [128, 4 * 512], BF16) for i in range(2)]
                    sg = [sbt(st, "sg%d" % i, [128, 512]) for i in range(2)]
                    AT = sbt(st, "AT", [128, NFC, 512], BF16)
                    yst = [sbt(st, "yst%d" % i, [128, 512]) for i in range(2)]
                    wi = 0
                    di = 0
                    yi = 0
                    for g in range(NG):
                        for s in range(4):
                            hb = (g % 2) * 4 + s
                            P.dma('pool', lambda e, hb=hb, g=g, s=s: e.indirect_dma_start(
                                out=hs[hb][:], out_offset=None, in_=HS,
                                in_offset=bass.IndirectOffsetOnAxis(ap=IDXH[:, g, s:s + 1], axis=0)), writes=[('hs', hb)])
                        for s in range(4):
                            hb = (g % 2) * 4 + s
                            bb = 4 + s
                            for k in range(8):
                                P.op('pe', lambda e, k=k, hb=hb, bb=bb: e.transpose(
                                    out=bkb(bb)[:, k * 128:(k + 1) * 128], in_=hs[hb][:, k * 128:(k + 1) * 128], identity=identb[:]),
                                    reads=[('hs', hb), 'identb'], writes=[('phT', bb)], banks=[bb])
                            if s % 2 == 0:
                                P.op('dve', lambda e, s=s, bb=bb: e.tensor_copy(
                                    out=h2T[:, :, s * 128:(s + 1) * 128], in_=bkb(bb)[:, :].rearrange("p (k n) -> p k n", k=8)),
                                    reads=[('phT', bb)], writes=[('h2T', s)], banks=[bb])
                            else:
                                P.op('act', lambda e, s=s, bb=bb: e.copy(
                                    out=h2T[:, :, s * 128:(s + 1) * 128], in_=bkb(bb)[:, :].rearrange("p (k n) -> p k n", k=8)),
                                    reads=[('phT', bb)], writes=[('h2T', s)], banks=[bb])
                        h2k = [('h2T', s) for s in range(4)]
                        for c in range(7):
                            wb = wi % 2
                            wi += 1
                            P.dma('pool', lambda e, wb=wb, g=g, c=c: e.indirect_dma_start(
                                out=wgs[wb][:], out_offset=None, in_=MGp,
                                in_offset=bass.IndirectOffsetOnAxis(ap=IDXGU[:, g, c:c + 1], axis=0)), writes=[('wgs', wb)])
                            P.dma('pool', lambda e, wb=wb, g=g, c=c: e.indirect_dma_start(
                                out=wus[wb][:], out_offset=None, in_=MUp,
                                in_offset=bass.IndirectOffsetOnAxis(ap=IDXU[:, g, c:c + 1], axis=0)), writes=[('wus', wb)])
                            for fc in range(4):
                                fidx = c * 4 + fc
                                gb = fidx % 2
                                for k in range(8):
                                    P.op('pe', lambda e, k=k, wb=wb, fc=fc, gb=gb: e.matmul(
                                        bk(6 + gb), lhsT=wgs[wb][:, k * 512 + fc * 128:k * 512 + (fc + 1) * 128], rhs=h2T[:, k, :],
                                        start=(k == 0), stop=(k == 7)),
                                        reads=[('wgs', wb)] + h2k, writes=[('pG', gb)], banks=[6 + gb])
                                for k in range(8):
                                    P.op('pe', lambda e, k=k, wb=wb, fc=fc, gb=gb: e.matmul(
                                        bk(4 + gb), lhsT=wus[wb][:, k * 512 + fc * 128:k * 512 + (fc + 1) * 128], rhs=h2T[:, k, :],
                                        start=(k == 0), stop=(k == 7)),
                                        reads=[('wus', wb)] + h2k, writes=[('pU', gb)], banks=[4 + gb])
                                P.op('act', lambda e, gb=gb: e.activation(out=sg[gb][:], in_=bk(6 + gb), func=AF.Silu),
                                     reads=[('pG', gb)], writes=[('sg', gb)], banks=[6 + gb])
                                P.op('dve', lambda e, gb=gb, fidx=fidx: e.tensor_tensor(
                                    out=AT[:, fidx, :], in0=bk(4 + gb), in1=sg[gb][:], op=ALU.mult),
                                    reads=[('pU', gb), ('sg', gb)], writes=[('AT', fidx)], banks=[4 + gb])
                        for half in range(2):
                            for q in range(7):
                                db = di % 2
                                di += 1
                                P.dma('pool', lambda e, db=db, g=g, half=half, q=q: e.indirect_dma_start(
                                    out=wds[db][:], out_offset=None, in_=MDp,
                                    in_offset=bass.IndirectOffsetOnAxis(ap=IDXD[:, g, half * 7 + q:half * 7 + q + 1], axis=0)), writes=[('wds', db)])
                                for fc in range(4):
                                    fidx = q * 4 + fc
                                    for s in range(4):
                                        P.op('pe', lambda e, s=s, fidx=fidx, fc=fc, db=db: e.matmul(
                                            bk(s), lhsT=AT[:, fidx, s * 128:(s + 1) * 128], rhs=wds[db][:, fc * 512:(fc + 1) * 512],
                                            start=(fidx == 0), stop=(fidx == NFC - 1)),
                                            reads=[('AT', fidx), ('wds', db)], writes=[('pD', s)], banks=[s])
                            for s in range(4):
                                yb = yi % 2
                                yi += 1
                                if yb == 0:
                                    P.op('dve', lambda e, s=s, yb=yb: e.tensor_copy(out=yst[yb][:], in_=bk(s)),
                                         reads=[('pD', s)], writes=[('yst', yb)], banks=[s])
                                else:
                                    P.op('act', lambda e, s=s, yb=yb: e.copy(out=yst[yb][:], in_=bk(s)),
                                         reads=[('pD', s)], writes=[('yst', yb)], banks=[s])
                                rr0 = g * 512 + s * 128
                                P.dma('sp', lambda e, yb=yb, rr0=rr0, half=half: e.dma_start(
                                    out=YS[rr0:rr0 + 128, half * 512:(half + 1) * 512], in_=yst[yb][:]),
                                    reads=[('yst', yb)], writes=[('YS', g, s, half)])
                    P.end_phase()
                with ExitStack() as st:
                    ya = [sbt(st, "ya%d" % i, [128, D]) for i in range(2)]
                    yb_ = [sbt(st, "yb%d" % i, [128, D]) for i in range(2)]
                    x1 = [sbt(st, "x1_%d" % i, [128, D]) for i in range(2)]
                    accs = [sbt(st, "acc%d" % i, [128, D]) for i in range(2)]
                    acc2s = [sbt(st, "acc2_%d" % i, [128, D]) for i in range(2)]
                    junk = sbt(st, "djunk", [128, D], BF16)
                    stt = [sbt(st, "dstt%d" % i, [128, 8]) for i in range(2)]
                    ob = [sbt(st, "ob%d" % i, [128, D]) for i in range(2)]
                    def loadsD3(ti_):
                        if ti_ >= NLT:
                            return
                        r0_ = (NCT + ti_) * 128
                        b_ = ti_ % 2
                        P.dma('pool', lambda e, b_=b_, ti_=ti_: e.indirect_dma_start(
                            out=ya[b_][:], out_offset=None, in_=YS, in_offset=bass.IndirectOffsetOnAxis(ap=RAi[:, ti_:ti_ + 1], axis=0)),
                            writes=[('ya', b_)])
                        P.dma('pool', lambda e, b_=b_, ti_=ti_: e.indirect_dma_start(
                            out=yb_[b_][:], out_offset=None, in_=YS, in_offset=bass.IndirectOffsetOnAxis(ap=RBi[:, ti_:ti_ + 1], axis=0)),
                            writes=[('yb', b_)])
                        P.dma('sp', lambda e, b_=b_, r0_=r0_: e.dma_start(out=x1[b_][:], in_=X[r0_:r0_ + 128, :]), writes=[('x1', b_)])

                    for ti in range(NLT):
                        t = NCT + ti
                        r0 = t * 128
                        b = ti % 2
                        if ti == 0:
                            loadsD3(0)
                        loadsD3(ti + 1)
                        acc = accs[b]
                        acc2 = acc2s[b]
                        P.op('dve', lambda e, b=b, ti=ti, acc=acc: e.tensor_scalar(out=acc[:], in0=ya[b][:], scalar1=G12[:, ti, 0:1], scalar2=None,
                                                                                   op0=ALU.mult), reads=[('ya', b)], writes=[('acc0', b)])
                        P.op('dve', lambda e, b=b, ti=ti, acc=acc: e.scalar_tensor_tensor(out=acc[:], in0=yb_[b][:], scalar=G12[:, ti, 1:2], in1=acc[:],
                                                                                          op0=ALU.mult, op1=ALU.add),
                             reads=[('yb', b), ('acc0', b)], writes=[('acc1', b)])
                        P.op('pool', lambda e, acc=acc, acc2=acc2: e.tensor_tensor(out=acc2[:], in0=acc[:], in1=GATE[w][1][:], op=ALU.mult),
                             reads=[('acc1', b)], writes=[('acc2', b)])
                        P.op('dve', lambda e, b=b, acc2=acc2: e.tensor_tensor(out=x1[b][:], in0=acc2[:], in1=x1[b][:], op=ALU.add),
                             reads=[('acc2', b), ('x1', b)], writes=[('x1', b)])
                        key = ('fst', b)
                        P.op('act', lambda e, b=b: e.activation(out=junk[:], in_=x1[b][:], func=AF.Square, accum_out=stt[b][:, 4:5]),
                             reads=[('x1', b)], writes=['junk', (key, 'ss')])
                        rstd_ops(stt[b], 4, 5, 6, 1, 1.0 / D, key)
                        P.op('dve', lambda e, b=b: e.scalar_tensor_tensor(
                            out=ob[b][:], in0=x1[b][:], scalar=stt[b][:, 6:7], in1=fnr[:], op0=ALU.mult, op1=ALU.mult),
                            reads=[('x1', b), (key, 'r')], writes=[('ob', b)])
                        o0 = r0 - NCT * 128
                        P.dma('sp', lambda e, b=b, o0=o0: e.dma_start(out=out[o0:o0 + 128, :], in_=ob[b][:]),
                              reads=[('ob', b)], writes=[('out', t)])
                    P.end_phase()

        phase_w()
        upto = cfg.get('upto', 'D')
        for l in range(NL if upto != 'W' else 0):
            phase0(l)
            phaseA(l)
            if upto == 'A':
                break
            phaseB(l)
            if upto == 'B':
                break
            phaseC(l)
            if upto == 'C':
                break
            if SPARSE and l % 2 == 1:
                phaseD_sparse(l)
            else:
                phaseD(l)
    return nc


def _consts(T):
    NT = NCT + T // 128
    identf = np.eye(128, dtype=np.float32)
    s = np.arange(128)
    trif = (s[:, None] <= s[None, :]).astype(np.float32)
    trib = (s[:, None] >= s[None, :]).astype(np.float32)
    rows = T // 64
    row = np.repeat(np.arange(rows, dtype=np.float32), 64)
    col = np.tile(np.arange(64, dtype=np.float32), rows)
    inv = (10000.0 ** (-np.arange(16, dtype=np.float32) / 16)).astype(np.float32)
    ang = np.concatenate([row[:, None] * inv, col[:, None] * inv], axis=-1).astype(np.float32)
    rope = np.zeros((NT * 128, 64), np.float32)
    rope[:NCT * 128, 0:32] = 1.0
    rope[NCT * 128:, 0:32] = np.cos(ang)
    rope[NCT * 128:, 32:64] = np.sin(ang)
    return dict(identf=identf, identb=identf.astype(ml_dtypes.bfloat16), trif=trif, trib=trib,
                maskf=trif.astype(ml_dtypes.bfloat16), maskb=trib.astype(ml_dtypes.bfloat16),
                rope=rope.reshape(NT, 128, 64))


def make_in_maps(inputs, nb, T):
    c = _consts(T)
    f = lambda a: np.ascontiguousarray(np.asarray(a, dtype=np.float32))
    nwc = np.zeros((128, 32), np.float32)
    for l in range(2):
        for j, nm in enumerate(('norm1_w', 'norm2_w')):
            nwc[:, l * 16 + j * 8: l * 16 + j * 8 + 8] = f(inputs[nm])[l].reshape(8, 128).T
    CAP = T
    NJ = CAP // 512
    NG = (2 * T + NEXP * 511) // 512
    p = np.arange(128, dtype=np.float32)[:, None]
    mconst = np.concatenate([
        np.broadcast_to(np.arange(NEXP, dtype=np.float32)[None, :] * CAP, (128, NEXP)),
        np.broadcast_to(np.arange(NJ, dtype=np.float32)[None, :] * 512, (128, NJ)),
        np.broadcast_to(np.arange(NG, dtype=np.float32)[None, :], (128, NG)),
        np.arange(7, dtype=np.float32)[None, :] * 128 + p,
        np.arange(14, dtype=np.float32)[None, :] * 128 + p,
        np.arange(4, dtype=np.float32)[None, :] * 128 + p], axis=1).astype(np.float32)
    shared = dict(
        n2w=f(inputs['norm2_w']), mconst=np.ascontiguousarray(mconst),
        ada_w=f(inputs['ada_w']), ada_b=f(inputs['ada_b']), nwc=nwc, fnw=f(inputs['final_norm_w']).reshape(1, D),
        w_in=np.ascontiguousarray(f(inputs['w_in'])[:, :, _PERM]), gate_b=f(inputs['mlstm_gate_b']),
        mnw=f(inputs['mlstm_norm_w']), qnw=f(inputs['q_norm_w']), knw=f(inputs['k_norm_w']), w_out=f(inputs['w_out']),
        ffn_g=f(inputs['ffn_w_gate'])[0], ffn_u=f(inputs['ffn_w_up'])[0], ffn_d=f(inputs['ffn_w_down'])[0],
        router=f(inputs['moe_router'])[0], moe_g=f(inputs['moe_w_gate'])[0], moe_u=f(inputs['moe_w_up'])[0],
        moe_d=f(inputs['moe_w_down'])[0], **c)
    x = f(inputs['x'])
    ctx = f(inputs['ctx'])
    cc = f(inputs['c'])
    c_ctx = f(inputs['c_ctx'])
    maps = []
    for b in range(nb):
        cvec = np.concatenate([cc[b].reshape(8, 128).T, c_ctx.reshape(8, 128).T], axis=1)
        m = dict(shared)
        m['xin'] = np.ascontiguousarray(np.concatenate([ctx[b], x[b]], axis=0))
        m['cvec'] = np.ascontiguousarray(cvec)
        maps.append(m)
    return maps


def kernel(**inputs):
    x = np.asarray(inputs['x'])
    B, T, _ = x.shape
    nc = build(dict(T=T))
    maps = make_in_maps(inputs, B, T)
    res = run_bass_kernel_spmd(nc, maps, core_ids=list(range(B)))
    return np.stack([np.asarray(r['out'], dtype=np.float32) for r in res.results], axis=0)
```
